# Optimizing a Trainium2 kernel written in Bass

```python
import math
import jax
import jax.numpy as jnp
from jax import lax
import numpy as np

D_MODEL = 4096
BATCH = 4
SEQ = 4096
DEPTH = 1

GDN_HEAD_DIM = 128
GDN_HEADS = D_MODEL // 256
GDN_WIDTH = GDN_HEADS * GDN_HEAD_DIM
GDN_CHUNK = 64
CONV_WIDTH = 4
RWKV_HEAD_DIM = 64
RWKV_HEADS = D_MODEL // 128
RWKV_WIDTH = RWKV_HEADS * RWKV_HEAD_DIM
DECAY_LORA = 96
AAA_LORA = 96
GATE_LORA = 256
RWKV_GN_EPS = 64e-5
N_GROUPS = 8
EXPERTS_PER_GROUP = 8
N_EXPERTS = N_GROUPS * EXPERTS_PER_GROUP
TOP_K = 2
D_EXPERT = 512
MOE_BLOCK = 128
RMS_EPS = 1e-6
N_MOD = 6

GDN_CONV_COLS = 3 * GDN_WIDTH
OFF_GDN_Z = GDN_CONV_COLS
OFF_GDN_A = OFF_GDN_Z + GDN_WIDTH
OFF_GDN_B = OFF_GDN_A + GDN_HEADS
OFF_RWKV = OFF_GDN_B + GDN_HEADS
RWKV_SHIFT_COLS = 3 * RWKV_WIDTH + DECAY_LORA + AAA_LORA + GATE_LORA
OFF_GATES = OFF_RWKV + RWKV_SHIFT_COLS
IN_COLS = OFF_GATES + 2 * D_MODEL

kernel_name = 'hybrid_gdn_rwkv7_hmoe_block'


def rmsnorm(x, gain, eps=RMS_EPS):
    xf = x.astype(jnp.float32)
    y = xf * lax.rsqrt(jnp.mean(xf * xf, axis=-1, keepdims=True) + eps)
    return (y * gain.astype(jnp.float32)).astype(x.dtype)


def l2norm(x, eps=1e-6):
    xf = x.astype(jnp.float32)
    return xf * lax.rsqrt(jnp.sum(xf * xf, axis=-1, keepdims=True) + eps)


def causal_depthwise_conv(x, w):
    C = x.shape[-1]
    return lax.conv_general_dilated(
        x, w[:, None, :].astype(x.dtype), window_strides=(1,), padding=[(CONV_WIDTH - 1, 0)],
        dimension_numbers=('NWC', 'WIO', 'NWC'), feature_group_count=C)


def token_shift(x):
    return jnp.pad(x, ((0, 0), (1, 0), (0, 0)))[:, :-1]


def gated_delta_rule_chunked(q, k, v, g, beta):
    B, T, H, Dk = q.shape
    Dv = v.shape[-1]
    C = GDN_CHUNK
    n = T // C

    def blocks(t):
        return jnp.moveaxis(t.reshape((B, n, C, H) + t.shape[3:]), 3, 1)

    q, k, v, g, beta = blocks(q), blocks(k), blocks(v), blocks(g), blocks(beta)
    gc = jnp.cumsum(g, axis=-1)
    idx = jnp.arange(C)
    causal = idx[:, None] >= idx[None, :]
    strict = idx[:, None] > idx[None, :]
    decay = jnp.exp(jnp.where(causal, gc[..., :, None] - gc[..., None, :], -jnp.inf))
    kb = k * beta[..., None]
    L = jnp.where(strict, jnp.einsum('bhnid,bhnjd->bhnij', kb, k) * decay, 0.0)
    a_mat = jnp.eye(C, dtype=L.dtype) + L
    u = lax.linalg.triangular_solve(a_mat, v * beta[..., None], left_side=True, lower=True, unit_diagonal=True)
    w = lax.linalg.triangular_solve(a_mat, kb * jnp.exp(gc)[..., None], left_side=True, lower=True, unit_diagonal=True)
    attn = jnp.einsum('bhnid,bhnjd->bhnij', q, k) * decay
    g_last = gc[..., -1]
    q_dec = q * jnp.exp(gc)[..., None]
    k_dec = k * jnp.exp(g_last[..., None] - gc)[..., None]

    def step(S, inp):
        q_i, k_i, u_i, w_i, a_i, gl_i = inp
        v_new = u_i - jnp.einsum('bhcd,bhde->bhce', w_i, S)
        o = jnp.einsum('bhcd,bhde->bhce', q_i, S) + jnp.einsum('bhij,bhje->bhie', a_i, v_new)
        S = S * jnp.exp(gl_i)[..., None, None] + jnp.einsum('bhcd,bhce->bhde', k_i, v_new)
        return S, o

    xs = tuple(jnp.moveaxis(t, 2, 0) for t in (q_dec, k_dec, u, w, attn, g_last))
    _, o = lax.scan(step, jnp.zeros((B, H, Dk, Dv), jnp.float32), xs)
    return jnp.moveaxis(o, 0, 2).reshape(B, H, T, Dv).transpose(0, 2, 1, 3)


def rwkv7_recurrence(r, w, k, v, a, b):
    B, T, H, N = r.shape

    def step(S, inp):
        r_t, w_t, k_t, v_t, a_t, b_t = inp
        sa = jnp.einsum('bhvk,bhk->bhv', S, a_t)
        S = S * w_t[:, :, None, :] + sa[..., None] * b_t[:, :, None, :] + v_t[..., None] * k_t[:, :, None, :]
        return S, jnp.einsum('bhvk,bhk->bhv', S, r_t)

    xs = tuple(jnp.moveaxis(t, 1, 0) for t in (r, w, k, v, a, b))
    _, y = lax.scan(step, jnp.zeros((B, H, N, N), jnp.float32), xs)
    return jnp.moveaxis(y, 0, 1)


def hybrid_mixer(h, w_in, conv_w, gdn_a_log, gdn_dt_bias, gdn_onorm_g, rwkv_mu, rwkv_w0, rwkv_w_up,
                 rwkv_a0, rwkv_a_up, rwkv_g_up, rwkv_k_k, rwkv_k_a, rwkv_r_k, rwkv_ln_w, rwkv_ln_b,
                 w_gdn_o, w_rwkv_o, w_out):
    B, T, _ = h.shape
    f32 = jnp.float32
    z = h @ w_in

    qkv = jax.nn.silu(causal_depthwise_conv(z[..., :GDN_CONV_COLS], conv_w))
    q, k, v = jnp.split(qkv, 3, axis=-1)
    hd = (B, T, GDN_HEADS, GDN_HEAD_DIM)
    q = l2norm(q.reshape(hd)) * (GDN_HEAD_DIM ** -0.5)
    k = l2norm(k.reshape(hd))
    v = v.reshape(hd).astype(f32)
    gz = z[..., OFF_GDN_Z:OFF_GDN_A].reshape(hd).astype(f32)
    g = -jnp.exp(gdn_a_log.astype(f32)) * jax.nn.softplus(z[..., OFF_GDN_A:OFF_GDN_B].astype(f32) + gdn_dt_bias.astype(f32))
    beta = jax.nn.sigmoid(z[..., OFF_GDN_B:OFF_RWKV].astype(f32))
    o = gated_delta_rule_chunked(q, k, v, g, beta)
    o = o * lax.rsqrt(jnp.mean(o * o, axis=-1, keepdims=True) + RMS_EPS) * gdn_onorm_g.astype(f32) * jax.nn.silu(gz)
    h_a = o.reshape(B, T, GDN_WIDTH).astype(h.dtype) @ w_gdn_o

    W = RWKV_WIDTH
    zr = z[..., OFF_RWKV:OFF_GATES]
    zr = zr + (token_shift(zr) - zr) * rwkv_mu
    r, kr, vr = zr[..., :W], zr[..., W:2 * W], zr[..., 2 * W:3 * W]
    wd = zr[..., 3 * W:3 * W + DECAY_LORA]
    ad = zr[..., 3 * W + DECAY_LORA:3 * W + DECAY_LORA + AAA_LORA]
    gd = zr[..., 3 * W + DECAY_LORA + AAA_LORA:]
    w_log = -jax.nn.softplus(-(rwkv_w0 + jnp.tanh(wd) @ rwkv_w_up).astype(f32)) - 0.5
    decay = jnp.exp(-jnp.exp(w_log))
    a_lr = jax.nn.sigmoid((rwkv_a0 + ad @ rwkv_a_up).astype(f32))
    gate = (jax.nn.sigmoid(gd) @ rwkv_g_up).astype(f32)
    rh = (B, T, RWKV_HEADS, RWKV_HEAD_DIM)
    kk = l2norm((kr * rwkv_k_k).reshape(rh))
    k_eff = (kr.astype(f32) * (1 + (a_lr - 1) * rwkv_k_a.astype(f32))).reshape(rh)
    r_h = r.reshape(rh).astype(f32)
    v_h = vr.reshape(rh).astype(f32)
    y = rwkv7_recurrence(r_h, decay.reshape(rh), k_eff, v_h, -kk, kk * a_lr.reshape(rh))
    y_mu = jnp.mean(y, axis=-1, keepdims=True)
    y_var = jnp.mean(jnp.square(y - y_mu), axis=-1, keepdims=True)
    y = (y - y_mu) * lax.rsqrt(y_var + RWKV_GN_EPS)
    y = y * rwkv_ln_w.astype(f32).reshape(RWKV_HEADS, RWKV_HEAD_DIM) + rwkv_ln_b.astype(f32).reshape(RWKV_HEADS, RWKV_HEAD_DIM)
    y = y + jnp.sum(r_h * k_eff * rwkv_r_k.astype(f32), axis=-1, keepdims=True) * v_h
    y = y.reshape(B, T, W) * gate
    h_b = y.astype(h.dtype) @ w_rwkv_o

    gates = jax.nn.sigmoid(z[..., OFF_GATES:].astype(f32))
    m = gates[..., :D_MODEL] * h_a + gates[..., D_MODEL:] * h_b
    return m.astype(h.dtype) @ w_out


def hierarchical_moe(h, w_group, b_group, w_expert, b_expert, w1, w3, w2):
    B, T, D = h.shape
    N = B * T
    A = N * TOP_K
    f32 = jnp.float32
    hf = h.reshape(N, D)
    g_logits = (hf @ w_group).astype(f32) + b_group.astype(f32)
    g_sel = jnp.argmax(g_logits, axis=-1)
    g_prob = jnp.take_along_axis(jax.nn.softmax(g_logits, axis=-1), g_sel[:, None], axis=-1)
    e_logits = ((hf @ w_expert).astype(f32) + b_expert.astype(f32)).reshape(N, N_GROUPS, EXPERTS_PER_GROUP)
    e_logits = jnp.take_along_axis(e_logits, g_sel[:, None, None], axis=1)[:, 0]
    top_p, top_i = lax.top_k(jax.nn.softmax(e_logits, axis=-1), TOP_K)
    top_p = top_p / jnp.sum(top_p, axis=-1, keepdims=True)
    expert_id = (g_sel[:, None] * EXPERTS_PER_GROUP + top_i).reshape(A).astype(jnp.int32)
    weight = (g_prob * top_p).reshape(A)
    token_id = jnp.repeat(jnp.arange(N, dtype=jnp.int32), TOP_K)
    order = jnp.argsort(expert_id)
    e_sorted = expert_id[order]
    counts = jax.ops.segment_sum(jnp.ones((A,), jnp.int32), expert_id, num_segments=N_EXPERTS)
    padded = (counts + MOE_BLOCK - 1) // MOE_BLOCK * MOE_BLOCK
    padded_end = jnp.cumsum(padded)
    dest = (padded_end - padded)[e_sorted] + jnp.arange(A, dtype=jnp.int32) - (jnp.cumsum(counts) - counts)[e_sorted]
    P = (A + N_EXPERTS * (MOE_BLOCK - 1) + MOE_BLOCK - 1) // MOE_BLOCK * MOE_BLOCK
    n_blocks = P // MOE_BLOCK
    buf_tok = jnp.full((P,), N, jnp.int32).at[dest].set(token_id[order])
    buf_w = jnp.zeros((P,), f32).at[dest].set(weight[order])
    block_expert = jnp.minimum(
        jnp.searchsorted(padded_end, jnp.arange(n_blocks, dtype=jnp.int32) * MOE_BLOCK, side='right'),
        N_EXPERTS - 1)
    h_pad = jnp.concatenate([hf, jnp.zeros((1, D), hf.dtype)], axis=0)
    xb = h_pad[buf_tok].reshape(n_blocks, MOE_BLOCK, D)

    def expert_block(args):
        xblk, e = args
        return (jax.nn.silu(xblk @ w1[e]) * (xblk @ w3[e])) @ w2[e]

    yb = lax.map(expert_block, (xb, block_expert)).reshape(P, D)
    out = jax.ops.segment_sum(yb.astype(f32) * buf_w[:, None], buf_tok, num_segments=N + 1)[:N]
    return out.reshape(B, T, D).astype(h.dtype)


def setup_inputs(seed: int = 0) -> dict:
    key = jax.random.key(seed)
    ks = iter(jax.random.split(key, 48))
    f32 = jnp.float32
    L = DEPTH

    def nrm(shape, scale):
        return scale * jax.random.normal(next(ks), shape, f32)

    def unif(shape, lo, hi):
        return jax.random.uniform(next(ks), shape, f32, minval=lo, maxval=hi)

    dt = jnp.exp(unif((L, GDN_HEADS), math.log(1e-3), math.log(0.1)))
    return {
        'x': nrm((BATCH, SEQ, D_MODEL), 1.0),
        'c': nrm((BATCH, D_MODEL), 1.0),
        'w_ada': nrm((L, D_MODEL, N_MOD * D_MODEL), 0.5 * D_MODEL ** -0.5),
        'b_ada': nrm((L, N_MOD * D_MODEL), 0.02),
        'norm1_g': 1.0 + nrm((L, D_MODEL), 0.02),
        'w_in': nrm((L, D_MODEL, IN_COLS), D_MODEL ** -0.5),
        'conv_w': nrm((L, CONV_WIDTH, GDN_CONV_COLS), CONV_WIDTH ** -0.5),
        'gdn_a_log': jnp.log(unif((L, GDN_HEADS), 1.0, 16.0)),
        'gdn_dt_bias': dt + jnp.log(-jnp.expm1(-dt)),
        'gdn_onorm_g': 1.0 + nrm((L, GDN_HEAD_DIM), 0.02),
        'rwkv_mu': unif((L, RWKV_SHIFT_COLS), 0.0, 1.0),
        'rwkv_w0': unif((L, RWKV_WIDTH), -6.0, -1.0),
        'rwkv_w_up': nrm((L, DECAY_LORA, RWKV_WIDTH), 0.1 * DECAY_LORA ** -0.5),
        'rwkv_a0': nrm((L, RWKV_WIDTH), 0.1),
        'rwkv_a_up': nrm((L, AAA_LORA, RWKV_WIDTH), AAA_LORA ** -0.5),
        'rwkv_g_up': nrm((L, GATE_LORA, RWKV_WIDTH), GATE_LORA ** -0.5),
        'rwkv_k_k': 0.85 + nrm((L, RWKV_WIDTH), 0.02),
        'rwkv_k_a': 1.0 + nrm((L, RWKV_WIDTH), 0.02),
        'rwkv_r_k': nrm((L, RWKV_HEADS, RWKV_HEAD_DIM), 0.1),
        'rwkv_ln_w': 1.0 + nrm((L, RWKV_WIDTH), 0.02),
        'rwkv_ln_b': nrm((L, RWKV_WIDTH), 0.02),
        'w_gdn_o': nrm((L, GDN_WIDTH, D_MODEL), GDN_WIDTH ** -0.5),
        'w_rwkv_o': nrm((L, RWKV_WIDTH, D_MODEL), RWKV_WIDTH ** -0.5),
        'w_out': nrm((L, D_MODEL, D_MODEL), D_MODEL ** -0.5),
        'norm2_g': 1.0 + nrm((L, D_MODEL), 0.02),
        'w_group': nrm((L, D_MODEL, N_GROUPS), D_MODEL ** -0.5),
        'b_group': nrm((L, N_GROUPS), 0.01),
        'w_expert': nrm((L, D_MODEL, N_EXPERTS), D_MODEL ** -0.5),
        'b_expert': nrm((L, N_EXPERTS), 0.01),
        'w1': nrm((L, N_EXPERTS, D_MODEL, D_EXPERT), D_MODEL ** -0.5),
        'w3': nrm((L, N_EXPERTS, D_MODEL, D_EXPERT), D_MODEL ** -0.5),
        'w2': nrm((L, N_EXPERTS, D_EXPERT, D_MODEL), D_EXPERT ** -0.5),
        'norm_f_g': 1.0 + nrm((D_MODEL,), 0.02),
    }


def reference(x, c, w_ada, b_ada, norm1_g, w_in, conv_w, gdn_a_log, gdn_dt_bias, gdn_onorm_g, rwkv_mu,
              rwkv_w0, rwkv_w_up, rwkv_a0, rwkv_a_up, rwkv_g_up, rwkv_k_k, rwkv_k_a, rwkv_r_k, rwkv_ln_w,
              rwkv_ln_b, w_gdn_o, w_rwkv_o, w_out, norm2_g, w_group, b_group, w_expert, b_expert, w1, w3,
              w2, norm_f_g):
    for l in range(DEPTH):
        mod = jax.nn.silu(c) @ w_ada[l] + b_ada[l]
        sh1, sc1, g1, sh2, sc2, g2 = jnp.split(mod[:, None, :], N_MOD, axis=-1)
        h = rmsnorm(x, norm1_g[l]) * (1 + sc1) + sh1
        x = x + g1 * hybrid_mixer(h, w_in[l], conv_w[l], gdn_a_log[l], gdn_dt_bias[l], gdn_onorm_g[l],
                                  rwkv_mu[l], rwkv_w0[l], rwkv_w_up[l], rwkv_a0[l], rwkv_a_up[l],
                                  rwkv_g_up[l], rwkv_k_k[l], rwkv_k_a[l], rwkv_r_k[l], rwkv_ln_w[l],
                                  rwkv_ln_b[l], w_gdn_o[l], w_rwkv_o[l], w_out[l])
        h = rmsnorm(x, norm2_g[l]) * (1 + sc2) + sh2
        x = x + g2 * hierarchical_moe(h, w_group[l], b_group[l], w_expert[l], b_expert[l], w1[l], w3[l], w2[l])
    return rmsnorm(x, norm_f_g)
```

```python
import numpy as np
from contextlib import ExitStack
import concourse.bass as bass
import concourse.mybir as mybir

F32 = mybir.dt.float32
BF16 = mybir.dt.bfloat16
I32 = mybir.dt.int32
U8 = mybir.dt.uint8
AF = mybir.ActivationFunctionType
ALU = mybir.AluOpType
AX = mybir.AxisListType
DSZ = {F32: 4, BF16: 2, I32: 4, U8: 1}

GRAN = 512
SEM_LIMIT = 30000
DMA_SEM_LIMIT = 60000


class Buf:
    def __init__(self, arena, lo, nbytes, dtype, shape, name):
        self.arena, self.lo, self.hi, self.dtype, self.shape, self.name = arena, lo, lo + nbytes, dtype, shape, name
        ap = arena.t[:, lo:lo + nbytes]
        if dtype != U8:
            ap = ap.bitcast(dtype)
        P = shape[0]
        if P < 128:
            ap = ap[0:P]
        if len(shape) > 2:
            names = " ".join("d%d" % i for i in range(len(shape) - 1))
            kw = {"d%d" % i: shape[i + 1] for i in range(len(shape) - 1)}
            ap = ap.rearrange("p (%s) -> p %s" % (names, names), **kw)
        self.ap = ap

    def __getitem__(self, k):
        return self.ap[k]

    def grans(self):
        gr = 2048 if self.arena.id == "ps" else GRAN
        return [(self.arena.id, g) for g in range(self.lo // gr, (self.hi + gr - 1) // gr)]

    def sub(self, lo_el, n_el, shape=None):
        sz = DSZ[self.dtype]
        return Buf(self.arena, self.lo + lo_el * sz, n_el * sz, self.dtype, shape or [self.shape[0], n_el],
                   self.name + ".s")


class Arena:
    def __init__(self, t, nbytes, aid):
        self.t, self.nbytes, self.id, self.top, self.marks = t, nbytes, aid, 0, []

    def alloc(self, shape, dtype, name="b", align=64):
        if self.id == "ps":
            align = 2048
        n = int(np.prod(shape[1:])) * DSZ[dtype]
        lo = (self.top + align - 1) // align * align
        assert lo + n <= self.nbytes, "arena %s overflow: %s needs %d at %d / %d" % (self.id, name, n, lo, self.nbytes)
        self.top = lo + n
        return Buf(self, lo, n, dtype, list(shape), name)

    def mark(self):
        self.marks.append(self.top)

    def release(self):
        self.top = self.marks.pop()


class Key:
    def __init__(self, name):
        self.name = name

    def grans(self):
        return [("key", self.name)]


ENGS = ["pe", "act", "dve", "pool", "sp"]


class Sched:
    def __init__(self, nc, es, sbuf_bytes=190 * 1024):
        self.nc, self.es = nc, es
        self.sb = Arena(es.enter_context(nc.sbuf_tensor("arena", [128, sbuf_bytes], U8)), sbuf_bytes, "sb")
        self.ps = Arena(es.enter_context(nc.psum_tensor("psarena", [128, 16384], U8)), 16384, "ps")
        self.streams = {e: [] for e in ENGS}
        self.eng_sems = {e: [] for e in ENGS}
        self.eng_count = {e: 0 for e in ENGS}
        self.pending = {e: [] for e in ENGS}
        self.lastw = {}
        self.readers = {}
        self.waited = {e: {} for e in ENGS}
        self.dma_sems = {}
        self.nsem = 0
        self.nops = 0

    def _newsem(self, name):
        self.nsem += 1
        return self.es.enter_context(self.nc.semaphore("%s_%d" % (name, self.nsem)))

    @staticmethod
    def _excl(reads, writes):
        r2 = [r for r in reads if not (isinstance(r, Buf) and r.arena.id == "ps")]
        w2 = list(writes) + [r for r in reads if isinstance(r, Buf) and r.arena.id == "ps"]
        return r2, w2

    def _deps(self, reads, writes):
        reads, writes = self._excl(reads, writes)
        toks = {}

        def add(t):
            if t is None:
                return
            k = id(t[0])
            if k not in toks or toks[k][1] < t[1]:
                toks[k] = t
        for r in reads:
            for g in r.grans():
                add(self.lastw.get(g))
        for w in writes:
            for g in w.grans():
                add(self.lastw.get(g))
                for t in self.readers.get(g, {}).values():
                    add(t)
        return toks

    def _record(self, reads, writes, tok):
        reads, writes = self._excl(reads, writes)
        for r in reads:
            for g in r.grans():
                d = self.readers.setdefault(g, {})
                d[id(tok[0])] = tok
        for w in writes:
            for g in w.grans():
                self.lastw[g] = tok
                self.readers[g] = {}

    def _waits(self, stream, toks):
        out = []
        wd = self.waited[stream]
        for k, t in toks.items():
            if t[1] == 0:
                continue
            if len(t) > 2 and t[2] == stream and t[3] > self.eng_count[stream]:
                continue
            if wd.get(k, 0) >= t[1]:
                continue
            wd[k] = t[1]
            out.append(t)
        return out

    def op(self, eng, fn, reads=(), writes=(), inc=True):
        toks = self._deps(reads, writes)
        waits = self._waits(eng, toks)
        cnt = self.eng_count[eng]
        if cnt % SEM_LIMIT == 0 and (not self.eng_sems[eng] or cnt // SEM_LIMIT >= len(self.eng_sems[eng])):
            self.eng_sems[eng].append(self._newsem(eng))
        sem = self.eng_sems[eng][cnt // SEM_LIMIT]
        if inc:
            self.eng_count[eng] = cnt + 1
            tok = (sem, cnt % SEM_LIMIT + 1, eng, cnt + 1)
        else:
            tok = (sem, cnt % SEM_LIMIT + 1, eng, cnt + 1)
            self.pending[eng].append(1)
        if inc:
            self.pending[eng] = []
        self.streams[eng].append((fn, waits, (sem, 1) if inc else None))
        self._record(reads, writes, tok)
        self.nops += 1
        return tok

    def dma(self, queue, fns, reads=(), writes=(), semkey=None):
        if not isinstance(fns, (list, tuple)):
            fns = [fns]
        toks = self._deps(reads, writes)
        waits = self._waits(queue, toks)
        if semkey is None:
            semkey = (writes[0] if writes else reads[0]).grans()[0]
        if semkey not in self.dma_sems:
            self.dma_sems[semkey] = [self._newsem("dma"), 0]
        rec = self.dma_sems[semkey]
        if rec[1] + 16 * len(fns) > DMA_SEM_LIMIT:
            rec[0], rec[1] = self._newsem("dma"), 0
        for i, fn in enumerate(fns):
            self.streams[queue].append((fn, waits if i == 0 else [], (rec[0], 16)))
        rec[1] += 16 * len(fns)
        tok = (rec[0], rec[1])
        self._record(reads, writes, tok)
        self.nops += 1
        return tok

    def wait_all(self, eng):
        toks = {}
        for g, t in self.lastw.items():
            k = id(t[0])
            if k not in toks or toks[k][1] < t[1]:
                toks[k] = t
        waits = self._waits(eng, toks)
        self.streams[eng].append((None, waits, None))

    def emit(self):
        for e in ENGS:
            assert not self.pending[e], "engine %s has trailing inc=False ops" % e
        nc = self.nc
        with nc.Block() as block:
            def run(engobj, lst):
                for fn, waits, inc in lst:
                    for w in waits:
                        engobj.wait_ge(w[0], w[1])
                    if fn is None:
                        continue
                    ins = fn(engobj)
                    if inc is not None:
                        ins.then_inc(inc[0], inc[1])

            @block.tensor
            def _(t):
                run(t, self.streams["pe"])

            @block.scalar
            def _(t):
                run(t, self.streams["act"])

            @block.vector
            def _(t):
                run(t, self.streams["dve"])

            @block.gpsimd
            def _(t):
                run(t, self.streams["pool"])

            @block.sync
            def _(t):
                run(t, self.streams["sp"])


def simulate(self):
    pos = {e: 0 for e in ENGS}
    val = {}
    progress = True
    while progress:
        progress = False
        for e in ENGS:
            lst = self.streams[e]
            while pos[e] < len(lst):
                fn, waits, inc = lst[pos[e]]
                if all(val.get(id(w[0]), 0) >= w[1] for w in waits):
                    if inc is not None:
                        val[id(inc[0])] = val.get(id(inc[0]), 0) + inc[1]
                    pos[e] += 1
                    progress = True
                else:
                    break
    stuck = {e: (pos[e], len(self.streams[e])) for e in ENGS if pos[e] < len(self.streams[e])}
    for e, (p, n) in stuck.items():
        fn, waits, inc = self.streams[e][p]
        print("STUCK", e, p, n, [(val.get(id(w[0]), 0), w[1], w[2:] if len(w) > 2 else "dma") for w in waits])
    return not stuck


Sched.simulate = simulate

import numpy as np
from contextlib import ExitStack
from concourse.bass_utils import run_bass_kernel_spmd


class Cfg:
    def __init__(s, D, T, DE, pair=False):
        s.D, s.T, s.DE, s.pair = D, T, DE, pair
        s.KC = D // 128
        s.HG = D // 256
        s.GW = s.HG * 128
        s.HR = D // 128
        s.RW = s.HR * 64
        s.NP = s.RW // 128
        s.CONVC = 3 * s.GW
        s.OFF_Z = s.CONVC
        s.OFF_A = s.OFF_Z + s.GW
        s.OFF_B = s.OFF_A + s.HG
        s.OFF_R = s.OFF_B + s.HG
        s.RSC = 3 * s.RW + 448
        s.OFF_G = s.OFF_R + s.RSC
        s.INC = s.OFF_G + 2 * D
        s.NE, s.NG, s.EPG = 64, 8, 8
        s.NT = T // 128
        s.TT = min(512, T)
        s.NTT = T // s.TT
        s.FC = DE // 128
        s.TM = T // 2 if pair else T
        s.NBLK = (2 * s.TM + 64 * 127 + 127) // 128


class KB:
    def __init__(s, cfg, debug=()):
        s.cfg = cfg
        s.nc = bass.Bass("TRN2", target_bir_lowering=False)
        s.es = ExitStack()
        s.S = Sched(s.nc, s.es)
        s.d = {}
        s.debug = debug

    def breg(s, e, val):
        if not hasattr(s, "_bregs"):
            s._bregs = {}
        if val not in s._bregs:
            s._bregs[val] = e.to_reg(val)
        return s._bregs[val]

    def din(s, name, shape, dt=F32):
        s.d[name] = s.nc.dram_tensor(name, list(shape), dt, kind="ExternalInput").ap()
        return s.d[name]

    def dout(s, name, shape, dt=F32):
        s.d[name] = s.nc.dram_tensor(name, list(shape), dt, kind="ExternalOutput").ap()
        return s.d[name]

    def dscr(s, name, shape, dt=F32):
        s.d[name] = s.nc.dram_tensor(name, list(shape), dt, kind="Internal").ap()
        return s.d[name]

    def mm(s, out, lhsT, rhs, R, W, start=True, stop=True, inc=True):
        return s.S.op("pe", lambda e: e.matmul(out, lhsT, rhs, start=start, stop=stop), reads=R, writes=W, inc=inc)

    def tr(s, out, in_, ident, R, W, inc=True):
        return s.S.op("pe", lambda e: e.transpose(out, in_, ident), reads=R, writes=W, inc=inc)

    def act(s, out, in_, func, R, W, scale=1.0, bias=0.0, accum=None, eng="act"):
        if accum is not None:
            return s.S.op(eng, lambda e: e.activation(out=out, in_=in_, func=func, scale=scale, bias=bias, accum_out=accum), reads=R, writes=W)
        return s.S.op(eng, lambda e: e.activation(out=out, in_=in_, func=func, scale=scale, bias=bias), reads=R, writes=W)

    def ts(s, out, in0, s1, s2, op0, op1, R, W, eng="dve", accum=None):
        if op1 is None:
            return s.S.op(eng, lambda e: e.tensor_scalar(out=out, in0=in0, scalar1=s1, scalar2=None, op0=op0), reads=R, writes=W)
        if accum is not None:
            return s.S.op(eng, lambda e: e.tensor_scalar(out=out, in0=in0, scalar1=s1, scalar2=s2, op0=op0, op1=op1, accum_out=accum), reads=R, writes=W)
        return s.S.op(eng, lambda e: e.tensor_scalar(out=out, in0=in0, scalar1=s1, scalar2=s2, op0=op0, op1=op1), reads=R, writes=W)

    def tt(s, out, in0, in1, op, R, W, eng="dve"):
        return s.S.op(eng, lambda e: e.tensor_tensor(out=out, in0=in0, in1=in1, op=op), reads=R, writes=W)

    def stt(s, out, in0, scalar, in1, op0, op1, R, W, eng="dve"):
        return s.S.op(eng, lambda e: e.scalar_tensor_tensor(out=out, in0=in0, scalar=scalar, in1=in1, op0=op0, op1=op1), reads=R, writes=W)

    def cp(s, out, in_, R, W, eng="dve"):
        if eng == "act":
            return s.S.op("act", lambda e: e.activation(out=out, in_=in_, func=AF.Copy), reads=R, writes=W)
        return s.S.op(eng, lambda e: e.tensor_copy(out=out, in_=in_), reads=R, writes=W)

    def red(s, out, in_, op, R, W, eng="dve"):
        return s.S.op(eng, lambda e: e.tensor_reduce(out=out, in_=in_, axis=AX.X, op=op), reads=R, writes=W)

    def recip(s, out, in_, R, W):
        return s.S.op("dve", lambda e: e.reciprocal(out=out, in_=in_), reads=R, writes=W)

    def memset(s, ap, val, W, eng="pool"):
        return s.S.op(eng, lambda e: e.memset(ap, val), writes=W)

    def dma(s, q, out, in_, R, W, semkey=None):
        return s.S.dma(q, lambda e: e.dma_start(out=out, in_=in_), reads=R, writes=W, semkey=semkey)

    def rsqrt_col(s, col, R_W, eps, mul=1.0):
        s.ts(col, col, mul, eps, ALU.mult, ALU.add, [R_W], [R_W])
        s.recip(col, col, [R_W], [R_W])
        s.act(col, col, AF.Sqrt, [R_W], [R_W])


def make_consts():
    i = np.arange(128)
    c = {}
    c["ident"] = np.eye(128, dtype=np.float32)
    c["tril_s"] = (i[:, None] > i[None, :]).astype(np.float32)
    c["tril_i"] = (i[:, None] >= i[None, :]).astype(np.float32)
    c["triu_s"] = (i[:, None] < i[None, :]).astype(np.float32)
    c["triu_i"] = (i[:, None] <= i[None, :]).astype(np.float32)
    c["ones"] = np.ones((128, 128), np.float32)
    c["md16"] = ((i[:, None] // 16) == (i[None, :] // 16)).astype(np.float32)
    for s_ in (16, 32, 64):
        bi, bj = i[:, None] // s_, i[None, :] // s_
        c["m%d" % s_] = ((bi % 2 == 1) & (bj == bi - 1)).astype(np.float32)
    c["bones"] = ((i[:, None] // 64) == (i[None, :] // 64)).astype(np.float32)
    hs = np.zeros((128, 128), np.float32); hs[:64, 0] = 1; hs[64:, 1] = 1
    c["headsel"] = hs
    c["iota_f"] = np.broadcast_to(i[None, :], (128, 128)).astype(np.float32).copy()
    c["iota_p"] = np.broadcast_to(i[:, None], (128, 128)).astype(np.float32).copy()
    return c


def declare_inputs(kb):
    c = kb.cfg
    D, T = c.D, c.T
    kb.din("x", [T, D])
    kb.din("cT", [128, c.KC])
    kb.din("w_ada", [D, 6 * D])
    kb.din("b_ada", [1, 6 * D])
    kb.din("n1g", [128, c.KC])
    kb.din("n2g", [1, D])
    kb.din("nfg", [1, D])
    kb.din("w_in", [D, c.INC])
    kb.din("convT", [128, c.CONVC // 128, 4])
    kb.din("a_log", [1, c.HG])
    kb.din("dt_bias", [1, c.HG])
    kb.din("onorm_g", [1, 128])
    for nm in ["mu_rkv"]:
        kb.din(nm, [128, 3 * c.NP])
    kb.din("mu_wd", [96, 1]); kb.din("mu_ad", [96, 1]); kb.din("mu_gd", [128, 2])
    for nm in ["w0", "a0", "k_k", "k_a", "ln_w", "ln_b", "r_k"]:
        kb.din(nm, [128, c.NP])
    kb.din("w0_row", [1, c.RW]); kb.din("lnw_row", [1, c.RW]); kb.din("lnb_row", [1, c.RW])
    kb.din("w_up", [96, c.RW]); kb.din("a_up", [96, c.RW]); kb.din("g_up", [256, c.RW])
    kb.din("w_gdn_o", [c.GW, D]); kb.din("w_rwkv_o", [c.RW, D]); kb.din("w_out", [D, D])
    kb.din("wr", [D, 72]); kb.din("br", [1, 72])
    kb.din("w1", [64 * 128, (D // 128) * c.DE]); kb.din("w3", [64 * 128, (D // 128) * c.DE])
    kb.din("w2", [64 * 128, c.FC * D])
    for k, v in make_consts().items():
        kb.din(k, v.shape)
    kb.din("tokiota", [128, c.TM // 128], I32)
    kb.dout("out", [c.TM, D])
    kb.dscr("mod_d", [128, 6 * D])
    kb.dscr("hT_d", [D, T], BF16)
    kb.dscr("oy_d", [D, T], BF16)
    kb.dscr("xmid_d", [c.TM, D])
    kb.dscr("h2_d", [c.TM + 1, D], BF16)
    kb.dscr("yb_d", [c.NBLK * 128, D])
    kb.dscr("tokid_d", [c.NBLK * 128, 1], I32)


def host_inputs(cfg, inp, b, half=0):
    c = cfg
    D = c.D
    f = lambda a: np.ascontiguousarray(a, dtype=np.float32)
    fm = lambda v: f(np.asarray(v).reshape(-1, 128).T)
    m = {}
    m["x"] = f(inp["x"][b])
    m["cT"] = fm(inp["c"][b])
    m["w_ada"] = f(inp["w_ada"][0]); m["b_ada"] = f(inp["b_ada"][0][None, :])
    m["n1g"] = fm(inp["norm1_g"][0]); m["n2g"] = f(inp["norm2_g"][0][None, :]); m["nfg"] = f(inp["norm_f_g"][None, :])
    m["w_in"] = f(inp["w_in"][0])
    cw = np.asarray(inp["conv_w"][0])
    m["convT"] = f(cw.T.reshape(c.CONVC // 128, 128, 4).transpose(1, 0, 2))
    m["a_log"] = f(inp["gdn_a_log"][0][None, :]); m["dt_bias"] = f(inp["gdn_dt_bias"][0][None, :])
    m["onorm_g"] = f(inp["gdn_onorm_g"][0][None, :])
    mu = np.asarray(inp["rwkv_mu"][0])
    m["mu_rkv"] = fm(mu[:3 * c.RW])
    o = 3 * c.RW
    m["mu_wd"] = f(mu[o:o + 96][:, None]); m["mu_ad"] = f(mu[o + 96:o + 192][:, None]); m["mu_gd"] = fm(mu[o + 192:o + 448])
    m["w0"] = fm(inp["rwkv_w0"][0]); m["a0"] = fm(inp["rwkv_a0"][0]); m["k_k"] = fm(inp["rwkv_k_k"][0]); m["k_a"] = fm(inp["rwkv_k_a"][0])
    m["ln_w"] = fm(inp["rwkv_ln_w"][0]); m["ln_b"] = fm(inp["rwkv_ln_b"][0]); m["r_k"] = fm(np.asarray(inp["rwkv_r_k"][0]).reshape(-1))
    m["w0_row"] = f(inp["rwkv_w0"][0][None, :]); m["lnw_row"] = f(inp["rwkv_ln_w"][0][None, :]); m["lnb_row"] = f(inp["rwkv_ln_b"][0][None, :])
    m["w_up"] = f(inp["rwkv_w_up"][0]); m["a_up"] = f(inp["rwkv_a_up"][0]); m["g_up"] = f(inp["rwkv_g_up"][0])
    m["w_gdn_o"] = f(inp["w_gdn_o"][0]); m["w_rwkv_o"] = f(inp["w_rwkv_o"][0]); m["w_out"] = f(inp["w_out"][0])
    m["wr"] = f(np.concatenate([np.asarray(inp["w_group"][0]), np.asarray(inp["w_expert"][0])], axis=1))
    m["br"] = f(np.concatenate([np.asarray(inp["b_group"][0]), np.asarray(inp["b_expert"][0])])[None, :])
    m["w1"] = f(np.asarray(inp["w1"][0]).reshape(64 * 128, -1))
    m["w3"] = f(np.asarray(inp["w3"][0]).reshape(64 * 128, -1))
    m["w2"] = f(np.asarray(inp["w2"][0]).reshape(64 * 128, -1))
    m.update(make_consts())
    m["tokiota"] = (np.arange(c.TM // 128)[None, :] * 128 + np.arange(128)[:, None]).astype(np.int32)
    return m


def load_consts(kb):
    S = kb.S
    kb.C = {}
    for k in ["ident", "tril_s", "tril_i", "triu_s", "triu_i", "ones", "md16", "m16", "m32", "m64", "bones", "headsel", "iota_f", "iota_p"]:
        b = S.sb.alloc([128, 128], F32, k)
        kb.dma("sp", b[:], kb.d[k][:], [], [b])
        kb.C[k] = b
    for k in ["ident", "ones", "triu_i", "triu_s", "bones", "headsel"]:
        b = S.sb.alloc([128, 128], BF16, k + "_bf")
        kb.cp(b[:], kb.C[k][:], [kb.C[k]], [b], eng="pool")
        kb.C[k + "_bf"] = b


def phase_mod(kb):
    c, S, d = kb.cfg, kb.S, kb.d
    KC, D = c.KC, c.D
    S.sb.mark(); S.ps.mark()
    ct = S.sb.alloc([128, KC], F32, "ct")
    cs = S.sb.alloc([128, KC], F32, "cs")
    scr = S.sb.alloc([128, KC, 128], BF16, "scr")
    kb.dma("sp", ct[:], d["cT"][:], [], [ct])
    kb.act(cs[:], ct[:], AF.Silu, [ct], [cs])
    for kc in range(KC):
        kb.ts(scr[:, kc, :], kb.C["ones"][:], cs[:, kc:kc + 1], None, ALU.mult, None, [cs, kb.C["ones"]], [scr], eng="pool")
    MT = 512
    wa = [S.sb.alloc([128, KC, MT], BF16, "wa%d" % i) for i in range(2)]
    bb = [S.sb.alloc([128, MT], F32, "bb%d" % i) for i in range(2)]
    mo = [S.sb.alloc([128, MT], F32, "mo%d" % i) for i in range(2)]
    ps = [S.ps.alloc([128, MT], F32, "psm%d" % i) for i in range(2)]
    wsrc = d["w_ada"].rearrange("(kc p) n -> p kc n", p=128)
    LV = 9
    for m in range(6 * D // MT):
        i = m % 2
        kb.dma("pool", wa[i][:], wsrc[:, :, m * MT:(m + 1) * MT], [], [wa[i]])
        if LV < 2: continue
        kb.dma("sp", bb[i][:], d["b_ada"][0:1, m * MT:(m + 1) * MT].partition_broadcast(128), [], [bb[i]])
        if LV < 3: continue
        for kc in range(KC):
            kb.mm(ps[i][:], scr[:, kc, :], wa[i][:, kc, :], [scr, wa[i]], [ps[i]], start=(kc == 0), stop=(kc == KC - 1), inc=(kc == KC - 1))
        if LV < 4: continue
        kb.tt(mo[i][:], ps[i][:], bb[i][:], ALU.add, [ps[i], bb[i]], [mo[i]])
        if LV < 5: continue
        kb.dma("sp", d["mod_d"][:, m * MT:(m + 1) * MT], mo[i][:], [mo[i]], [kb.K_mod])
    S.sb.release(); S.ps.release()


def diag_extract(kb, dst, dstbuf, seg, tmpbig, tmp):
    c, d = kb.cfg, kb.d
    kb.dma("sp", tmpbig[:], d["mod_d"][:, seg * c.D:(seg + 1) * c.D], [kb.K_mod], [tmpbig])
    for kc in range(c.KC):
        kb.tt(tmp[:], tmpbig[:, kc * 128:(kc + 1) * 128], kb.C["ident"][:], ALU.mult, [tmpbig, kb.C["ident"]], [tmp])
        kb.red(dst[:, kc:kc + 1], tmp[:], ALU.add, [tmp], [dstbuf])


def phase_norm1(kb):
    c, S, d = kb.cfg, kb.S, kb.d
    KC, D, T, TT = c.KC, c.D, c.T, c.TT
    S.sb.mark(); S.ps.mark()
    A1 = S.sb.alloc([128, KC], F32, "A1"); B1 = S.sb.alloc([128, KC], F32, "B1"); g1n = S.sb.alloc([128, KC], F32, "g1n")
    S.sb.mark()
    big = S.sb.alloc([128, D], F32, "big"); tmp = S.sb.alloc([128, 128], F32, "tmp")
    diag_extract(kb, B1, B1, 0, big, tmp)
    diag_extract(kb, A1, A1, 1, big, tmp)
    kb.dma("sp", g1n[:], d["n1g"][:], [], [g1n])
    kb.stt(A1[:], A1[:], 1.0, g1n[:], ALU.add, ALU.mult, [A1, g1n], [A1])
    S.sb.release()
    xt = [S.sb.alloc([128, D], F32, "xt%d" % i) for i in range(2)]
    junk = S.sb.alloc([128, D], BF16, "junk")
    xn = [S.sb.alloc([128, D], BF16, "xn%d" % i) for i in range(2)]
    st = [S.sb.alloc([128, 2], F32, "st%d" % i) for i in range(2)]
    hTt = [S.sb.alloc([128, KC, TT], BF16, "hTt%d" % i) for i in range(2)]
    pst = [S.ps.alloc([128, 4, 128], BF16, "pst%d" % i) for i in range(4)]
    assert len({p.lo // 2048 for p in pst}) == 4
    hdst = d["hT_d"].rearrange("(kc p) t -> p kc t", p=128)
    G = min(4, KC)
    pi = 0
    for n in range(T // 128):
        i = n % 2
        hb = hTt[(n * 128 // TT) % 2]
        toff = (n * 128) % TT
        kb.dma("sp", xt[i][:], d["x"][n * 128:(n + 1) * 128, :], [], [xt[i]])
        kb.act(junk[:], xt[i][:], AF.Square, [xt[i]], [junk, st[i]], accum=st[i][:, 0:1])
        kb.rsqrt_col(st[i][:, 0:1], st[i], 1e-6, 1.0 / D)
        kb.ts(xn[i][:], xt[i][:], st[i][:, 0:1], None, ALU.mult, None, [xt[i], st[i]], [xn[i]])
        for g in range(KC // G):
            p = pst[pi % 4]; pi += 1
            for j in range(G):
                kc = g * G + j
                kb.tr(p[:, j, :], xn[i][:, kc * 128:(kc + 1) * 128], kb.C["ident_bf"][:], [xn[i], kb.C["ident_bf"]], [p], inc=(j == G - 1))
            for j in range(G):
                kc = g * G + j
                kb.act(hb[:, kc, toff:toff + 128], p[:, j, :], AF.Identity, [p, A1, B1], [hb], scale=A1[:, kc:kc + 1], bias=B1[:, kc:kc + 1],
                       eng="act")
        if toff + 128 == TT:
            t0 = n * 128 + 128 - TT
            kb.dma("sp", hdst[:, :, t0:t0 + TT], hb[:], [hb], [kb.K_hT])
    S.sb.release(); S.ps.release()


class Rot:
    def __init__(self, bufs):
        self.bufs, self.i = bufs, 0

    def get(self):
        b = self.bufs[self.i % len(self.bufs)]
        self.i += 1
        return b


def neumann(kb, grp, pt, NR=3):
    C = kb.C
    assert 2 * len(grp) <= len(pt.bufs)
    for m in grp:
        kb.tt(m["L2"][:], m["L"][:], C["md16"][:], ALU.mult, [m["L"], C["md16"]], [m["L2"]], eng="pool")
        kb.tt(m["U2"][:], m["U"][:], C["md16"][:], ALU.mult, [m["U"], C["md16"]], [m["U2"]], eng="pool")
        kb.tt(m["Y"][:], C["ident"][:], m["U2"][:], ALU.subtract, [C["ident"], m["U2"]], [m["Y"]], eng="pool")
    for r in range(NR):
        for m in grp:
            Lk, Uk = (m["L2"], m["U2"]) if r % 2 == 0 else (m["D"], m["DT"])
            p1 = pt.get()
            kb.mm(p1[:], Uk[:], Lk[:], [Uk, Lk], [p1])
            m["_p1"] = p1
            if r < NR - 1:
                p2 = pt.get()
                kb.mm(p2[:], Lk[:], Uk[:], [Lk, Uk], [p2])
                m["_p2"] = p2
        for m in grp:
            Ln, Un = (m["D"], m["DT"]) if r % 2 == 0 else (m["L2"], m["U2"])
            kb.cp(Ln[:], m["_p1"][:], [m["_p1"]], [Ln], eng="act")
            if r < NR - 1:
                kb.cp(Un[:], m["_p2"][:], [m["_p2"]], [Un], eng="dve")
        for m in grp:
            Ln = m["D"] if r % 2 == 0 else m["L2"]
            p3 = pt.get()
            kb.mm(p3[:], Ln[:], m["Y"][:], [Ln, m["Y"]], [p3])
            m["_p3"] = p3
        for m in grp:
            kb.tt(m["Y"][:], m["Y"][:], m["_p3"][:], ALU.add, [m["Y"], m["_p3"]], [m["Y"]])
    for s_ in (16, 32, 64):
        msk = C["m%d" % s_]
        for m in grp:
            kb.tt(m["L2"][:], m["L"][:], msk[:], ALU.mult, [m["L"], msk], [m["L2"]], eng="pool")
            p = pt.get()
            kb.tr(p[:], m["Y"][:], C["ident"][:], [m["Y"], C["ident"]], [p])
            m["_p1"] = p
            p = pt.get()
            kb.mm(p[:], m["L2"][:], m["Y"][:], [m["L2"], m["Y"]], [p])
            m["_p2"] = p
        for m in grp:
            kb.cp(m["D"][:], m["_p1"][:], [m["_p1"]], [m["D"]], eng="act")
            kb.cp(m["DT"][:], m["_p2"][:], [m["_p2"]], [m["DT"]], eng="dve")
        for m in grp:
            p = pt.get()
            kb.mm(p[:], m["D"][:], m["DT"][:], [m["D"], m["DT"]], [p])
            m["_p3"] = p
        for m in grp:
            kb.tt(m["Y"][:], m["Y"][:], m["_p3"][:], ALU.subtract, [m["Y"], m["_p3"]], [m["Y"]])


def phase_gdn_pre(kb, heads):
    c, S, d, C = kb.cfg, kb.S, kb.d, kb.C
    KC, T, TT, HG, NT = c.KC, c.T, c.TT, c.HG, c.NT
    P = {}
    for nm in ["g", "beta", "gc", "egc", "bege", "kdec", "egl"]:
        P[nm] = S.sb.alloc([128, HG, NT], F32, "gp_" + nm)
    S.sb.mark(); S.ps.mark()
    ab = S.sb.alloc([128, 2 * HG, NT], F32, "ab")
    wab = S.sb.alloc([128, KC, 2 * HG], BF16, "wab")
    hT = [S.sb.alloc([128, KC, TT], BF16, "hTs%d" % i) for i in range(2)]
    cb = S.sb.alloc([128, 3, HG], F32, "cb")
    pab = [S.ps.alloc([128, 2 * HG], F32, "pab%d" % i) for i in range(2)]
    pbig = S.ps.alloc([128, 512], F32, "pbig")
    kb.dma("pool", wab[:], d["w_in"].rearrange("(kc p) n -> p kc n", p=128)[:, :, c.OFF_A:c.OFF_A + 2 * HG], [], [wab])
    kb.dma("sp", cb[:, 0, :], d["dt_bias"][0:1, :].partition_broadcast(128), [], [cb])
    kb.dma("sp", cb[:, 1, :], d["a_log"][0:1, :].partition_broadcast(128), [], [cb])
    kb.act(cb[:, 2, :], cb[:, 1, :], AF.Exp, [cb], [cb])
    kb.ts(cb[:, 2, :], cb[:, 2, :], -1.0, None, ALU.mult, None, [cb], [cb])
    hsrc = d["hT_d"].rearrange("(kc p) t -> p kc t", p=128)
    for tt in range(T // TT):
        hb = hT[tt % 2]
        kb.dma("sp", hb[:], hsrc[:, :, tt * TT:(tt + 1) * TT], [kb.K_hT], [hb])
        for j in range(TT // 128):
            n = tt * (TT // 128) + j
            p = pab[n % 2]
            for kc in range(KC):
                kb.mm(p[:], hb[:, kc, j * 128:(j + 1) * 128], wab[:, kc, :], [hb, wab], [p], start=(kc == 0), stop=(kc == KC - 1), inc=(kc == KC - 1))
            kb.cp(ab[:, :, n], p[:], [p], [ab], eng="act")
    g, beta = P["g"], P["beta"]
    for h in range(HG):
        kb.act(g[:, h, :], ab[:, h, :], AF.Exp, [ab, cb], [g], bias=cb[:, 0, h:h + 1])
        kb.act(g[:, h, :], g[:, h, :], AF.Ln, [g], [g], bias=1.0)
        kb.ts(g[:, h, :], g[:, h, :], cb[:, 2, h:h + 1], None, ALU.mult, None, [g, cb], [g])
    kb.act(beta[:].rearrange("p h n -> p (h n)"), ab[:, HG:2 * HG, :].rearrange("p h n -> p (h n)"), AF.Sigmoid, [ab], [beta])
    N = HG * NT
    assert N <= 512
    gf = lambda b: b[:].rearrange("p h n -> p (h n)")
    kb.mm(pbig[:, 0:N], C["triu_i"][:], gf(g), [C["triu_i"], g], [pbig])
    kb.cp(gf(P["gc"]), pbig[:, 0:N], [pbig], [P["gc"]], eng="act")
    kb.mm(pbig[:, 0:N], C["ones"][:], gf(g), [C["ones"], g], [pbig])
    kb.act(gf(P["egl"]), pbig[:, 0:N], AF.Exp, [pbig], [P["egl"]])
    kb.tt(gf(P["kdec"]), pbig[:, 0:N], gf(P["gc"]), ALU.subtract, [pbig, P["gc"]], [P["kdec"]])
    kb.act(gf(P["kdec"]), gf(P["kdec"]), AF.Exp, [P["kdec"]], [P["kdec"]])
    kb.act(gf(P["egc"]), gf(P["gc"]), AF.Exp, [P["gc"]], [P["egc"]])
    kb.tt(gf(P["bege"]), gf(P["egc"]), gf(beta), ALU.mult, [P["egc"], beta], [P["bege"]])
    S.sb.release(); S.ps.release()
    return P


def inproj_groups(kb, cols, hT, wbufs, emit_evac, pbanks):
    c, d = kb.cfg, kb.d
    KC, T, TT = c.KC, c.T, c.TT
    wsrc = d["w_in"].rearrange("(kc p) n -> p kc n", p=128)
    for gi, co in enumerate(cols):
        kb.dma("pool", wbufs[gi][:], wsrc[:, :, co:co + 128], [], [wbufs[gi]])
    hsrc = d["hT_d"].rearrange("(kc p) t -> p kc t", p=128)
    pi = 0
    for tt in range(T // TT):
        hb = hT[tt % 2]
        kb.dma("sp", hb[:], hsrc[:, :, tt * TT:(tt + 1) * TT], [kb.K_hT], [hb])
        for gi in range(len(cols)):
            p = pbanks[pi % len(pbanks)]; pi += 1
            for kc in range(KC):
                kb.mm(p[:, 0:TT], wbufs[gi][:, kc, :], hb[:, kc, :], [wbufs[gi], hb], [p], start=(kc == 0), stop=(kc == KC - 1), inc=(kc == KC - 1))
            emit_evac(gi, tt, p)


def phase_gdn(kb, heads, P):
    c, S, d, C = kb.cfg, kb.S, kb.d, kb.C
    KC, T, TT, HG, NT = c.KC, c.T, c.TT, c.HG, c.NT
    S.sb.mark(); S.ps.mark()
    gon = S.sb.alloc([128, 128], F32, "gon")
    kb.dma("sp", gon[:], d["onorm_g"][0:1, :].partition_broadcast(128), [], [gon])
    qf = S.sb.alloc([128, T], BF16, "qf"); kf = S.sb.alloc([128, T], BF16, "kf")
    vf = S.sb.alloc([128, T], BF16, "vf"); gzf = S.sb.alloc([128, T], BF16, "gzf")
    of = S.sb.alloc([128, T], BF16, "of")
    Sst = S.sb.alloc([128, 128], F32, "Sst"); Sbf = S.sb.alloc([128, 128], BF16, "Sbf")
    for hi, h in enumerate(heads):
        S.sb.mark(); S.ps.mark()
        wb = [S.sb.alloc([128, KC, 128], BF16, "wg%d" % i) for i in range(4)]
        hT = [S.sb.alloc([128, KC, TT], BF16, "hTg%d" % i) for i in range(2)]
        zc = [S.sb.alloc([128, 3 + TT], F32, "zc%d" % i) for i in range(3)]
        cw = S.sb.alloc([128, 3, 4], F32, "cw")
        acc = [S.sb.alloc([128, TT], F32, "acc%d" % i) for i in range(2)]
        sq = S.sb.alloc([128, TT], BF16, "sq"); rn = S.sb.alloc([128, TT], F32, "rn")
        pb = [S.ps.alloc([128, 512], F32, "pg%d" % i) for i in range(5)]
        pn = [S.ps.alloc([128, 512], F32, "pn%d" % i) for i in range(2)]
        cols = [h * 128, c.GW + h * 128, 2 * c.GW + h * 128, c.OFF_Z + h * 128]
        for i in range(3):
            kb.dma("sp", cw[:, i, :], d["convT"][:, cols[i] // 128, :], [], [cw])
            kb.memset(zc[i][:, 0:3], 0.0, [zc[i]])
        dst = [qf, kf, vf]
        scale_q = 128.0 ** -0.5

        def evac(gi, tt, p):
            t0 = tt * TT
            if gi == 3:
                kb.act(gzf[:, t0:t0 + TT], p[:, 0:TT], AF.Silu, [p], [gzf])
                return
            z = zc[gi]
            kb.cp(z[:, 3:3 + TT], p[:, 0:TT], [p], [z], eng="act")
            a = acc[gi % 2]
            kb.ts(a[:], z[:, 3:3 + TT], cw[:, gi, 3:4], None, ALU.mult, None, [z, cw], [a])
            for i in range(3):
                kb.stt(a[:], z[:, i:i + TT], cw[:, gi, i:i + 1], a[:], ALU.mult, ALU.add, [z, cw, a], [a])
            kb.cp(z[:, 0:3], z[:, TT:TT + 3], [z], [z], eng="pool")
            if gi == 2:
                kb.act(vf[:, t0:t0 + TT], a[:], AF.Silu, [a], [vf])
                return
            kb.act(a[:], a[:], AF.Silu, [a], [a])
            kb.act(sq[:], a[:], AF.Square, [a], [sq])
            pp = pn[gi % 2]
            kb.mm(pp[:, 0:TT], C["ones_bf"][:], sq[:], [C["ones_bf"], sq], [pp])
            kb.ts(rn[:], pp[:, 0:TT], 1.0, 1e-6, ALU.mult, ALU.add, [pp], [rn])
            kb.recip(rn[:], rn[:], [rn], [rn])
            kb.act(rn[:], rn[:], AF.Sqrt, [rn], [rn])
            if gi == 0:
                kb.stt(dst[gi][:, t0:t0 + TT], a[:], scale_q, rn[:], ALU.mult, ALU.mult, [a, rn], [dst[gi]])
            else:
                kb.tt(dst[gi][:, t0:t0 + TT], a[:], rn[:], ALU.mult, [a, rn], [dst[gi]])
        GL = 9
        if GL >= 1: inproj_groups(kb, cols, hT, wb, evac, pb)
        S.sb.release(); S.ps.release()
        if GL < 2: return
        S.sb.mark(); S.ps.mark()
        ktm = S.sb.alloc([128, NT, 128], BF16, "ktm"); vbt = S.sb.alloc([128, NT, 128], BF16, "vbt")
        kbe = S.sb.alloc([128, NT, 128], BF16, "kbe")
        wT = S.sb.alloc([128, NT, 128], BF16, "wT"); u = S.sb.alloc([128, NT, 128], F32, "u")
        aT = S.sb.alloc([128, NT, 128], BF16, "aT")
        G = min(3, NT)
        grp = []
        for i in range(G):
            m = {k: S.sb.alloc([128, 128], F32, "%s%d" % (k, i)) for k in ["L", "U", "Y", "L2", "U2", "gb", "D", "DT"]}
            m["Ybf"] = S.sb.alloc([128, 128], BF16, "Ybf%d" % i)
            grp.append(m)
        pt = Rot([S.ps.alloc([128, 128], F32, "pt%d" % i) for i in range(6)])
        ptb = Rot([S.ps.alloc([128, 128], BF16, "ptb%d" % i) for i in range(2)])
        gcol = lambda nm, n: P[nm][:, h, n:n + 1]
        for n0 in range(0, NT, G):
            ns = list(range(n0, min(NT, n0 + G)))
            for i, n in enumerate(ns):
                m = grp[i]
                cs = slice(n * 128, (n + 1) * 128)
                p = ptb.get()
                kb.tr(p[:], kf[:, cs], C["ident_bf"][:], [kf, C["ident_bf"]], [p])
                kb.cp(ktm[:, n, :], p[:], [p], [ktm], eng="act")
                kb.ts(kbe[:, n, :], p[:], gcol("bege", n), None, ALU.mult, None, [p, P["bege"]], [kbe])
                p = ptb.get()
                kb.tr(p[:], vf[:, cs], C["ident_bf"][:], [vf, C["ident_bf"]], [p])
                kb.ts(vbt[:, n, :], p[:], gcol("beta", n), None, ALU.mult, None, [p, P["beta"]], [vbt])
                kb.ts(m["gb"][:], C["ones"][:], gcol("g", n), None, ALU.mult, None, [C["ones"], P["g"]], [m["gb"]], eng="pool")
                p = pt.get()
                kb.mm(p[:], m["gb"][:], C["triu_i"][:], [m["gb"], C["triu_i"]], [p])
                kb.ts(m["D"][:], p[:], gcol("gc", n), 0.0, ALU.subtract, ALU.max, [p, P["gc"]], [m["D"]])
                kb.ts(m["DT"][:], p[:], gcol("gc", n), 0.0, ALU.subtract, ALU.min, [p, P["gc"]], [m["DT"]])
                kb.act(m["D"][:], m["D"][:], AF.Exp, [m["D"]], [m["D"]], scale=-1.0)
                kb.act(m["DT"][:], m["DT"][:], AF.Exp, [m["DT"]], [m["DT"]])
                kb.tt(m["D"][:], m["D"][:], C["tril_s"][:], ALU.mult, [m["D"], C["tril_s"]], [m["D"]], eng="pool")
                kb.tt(m["DT"][:], m["DT"][:], C["triu_i"][:], ALU.mult, [m["DT"], C["triu_i"]], [m["DT"]], eng="pool")
                p = pt.get()
                kb.mm(p[:], kf[:, cs], kf[:, cs], [kf], [p])
                kb.stt(m["L"][:], p[:], gcol("beta", n), m["D"][:], ALU.mult, ALU.mult, [p, P["beta"], m["D"]], [m["L"]])
                p = pt.get()
                kb.tr(p[:], m["L"][:], C["ident"][:], [m["L"], C["ident"]], [p])
                kb.cp(m["U"][:], p[:], [p], [m["U"]], eng="act")
                p = pt.get()
                kb.mm(p[:], kf[:, cs], qf[:, cs], [kf, qf], [p])
                kb.tt(aT[:, n, :], p[:], m["DT"][:], ALU.mult, [p, m["DT"]], [aT])
            if GL < 3: continue
            neumann(kb, grp[:len(ns)], pt)
            if GL < 4: continue
            for i, n in enumerate(ns):
                m = grp[i]
                kb.cp(m["Ybf"][:], m["Y"][:], [m["Y"]], [m["Ybf"]], eng="pool")
                p = pt.get()
                kb.mm(p[:], kbe[:, n, :], m["Ybf"][:], [kbe, m["Ybf"]], [p])
                kb.cp(wT[:, n, :], p[:], [p], [wT], eng="act")
                p = pt.get()
                kb.mm(p[:], m["Ybf"][:], vbt[:, n, :], [m["Ybf"], vbt], [p])
                kb.cp(u[:, n, :], p[:], [p], [u], eng="dve")
        if GL < 5: return
        vn = S.sb.alloc([128, 128], F32, "vn"); vnb = S.sb.alloc([128, 128], BF16, "vnb"); vns = S.sb.alloc([128, 128], BF16, "vns")
        o1 = S.sb.alloc([128, 128], F32, "o1"); o = S.sb.alloc([128, 128], F32, "o"); onb = S.sb.alloc([128, 128], BF16, "onb")
        junk = S.sb.alloc([128, 128], BF16, "junkg"); st = S.sb.alloc([128, 2], F32, "stg")
        kb.memset(Sst[:], 0.0, [Sst]); kb.memset(Sbf[:], 0.0, [Sbf])
        for n in range(NT):
            cs = slice(n * 128, (n + 1) * 128)
            pw = pt.get(); pq = pt.get()
            kb.mm(pw[:], wT[:, n, :], Sbf[:], [wT, Sbf], [pw])
            kb.mm(pq[:], qf[:, cs], Sbf[:], [qf, Sbf], [pq])
            kb.tt(vn[:], u[:, n, :], pw[:], ALU.subtract, [u, pw], [vn])
            kb.cp(vnb[:], vn[:], [vn], [vnb], eng="act")
            kb.ts(vns[:], vn[:], gcol("kdec", n), None, ALU.mult, None, [vn, P["kdec"]], [vns])
            pa = pt.get(); psu = pt.get()
            kb.mm(pa[:], aT[:, n, :], vnb[:], [aT, vnb], [pa])
            kb.mm(psu[:], ktm[:, n, :], vns[:], [ktm, vns], [psu])
            kb.stt(Sst[:], Sst[:], gcol("egl", n), psu[:], ALU.mult, ALU.add, [Sst, P["egl"], psu], [Sst])
            kb.cp(Sbf[:], Sst[:], [Sst], [Sbf], eng="act")
            kb.cp(o1[:], pa[:], [pa], [o1], eng="act")
            kb.stt(o[:], pq[:], gcol("egc", n), o1[:], ALU.mult, ALU.add, [pq, P["egc"], o1], [o])
            kb.act(junk[:], o[:], AF.Square, [o], [junk, st], accum=st[:, 0:1])
            kb.rsqrt_col(st[:, 0:1], st, 1e-6, 1.0 / 128)
            kb.stt(onb[:], o[:], st[:, 0:1], gon[:], ALU.mult, ALU.mult, [o, st, gon], [onb])
            p = ptb.get()
            kb.tr(p[:], onb[:], C["ident_bf"][:], [onb, C["ident_bf"]], [p])
            kb.tt(of[:, cs], p[:], gzf[:, cs], ALU.mult, [p, gzf], [of])
        kb.dma("sp", d["oy_d"][h * 128:(h + 1) * 128, :], of[:], [of], [kb.K_oy])
        S.sb.release(); S.ps.release()
    S.sb.release(); S.ps.release()


def phase_rwkv_pre(kb):
    c, S, d, C = kb.cfg, kb.S, kb.d, kb.C
    KC, T, TT = c.KC, c.T, c.TT
    L = {}
    L["twd"] = S.sb.alloc([128, T], BF16, "twd"); L["ads"] = S.sb.alloc([128, T], BF16, "ads")
    L["sgd"] = S.sb.alloc([128, 2, T], BF16, "sgd")
    S.sb.mark(); S.ps.mark()
    base = c.OFF_R + 3 * c.RW
    groups = [(base, 96), (base + 96, 96), (base + 192, 128), (base + 320, 128)]
    wb = [S.sb.alloc([128, KC, 128], BF16, "wl%d" % i) for i in range(4)]
    hT = [S.sb.alloc([128, KC, TT], BF16, "hTl%d" % i) for i in range(2)]
    zr = [S.sb.alloc([128, 1 + TT], F32, "zl%d" % i) for i in range(4)]
    tmp = S.sb.alloc([128, TT], F32, "tl")
    mu = S.sb.alloc([128, 4], F32, "mul")
    pb = [S.ps.alloc([128, 512], F32, "pl%d" % i) for i in range(4)]
    kb.dma("sp", mu[0:96, 0:1], d["mu_wd"][:, :], [], [mu]); kb.dma("sp", mu[0:96, 1:2], d["mu_ad"][:, :], [], [mu])
    kb.dma("sp", mu[:, 2:4], d["mu_gd"][:, :], [], [mu])
    wsrc = d["w_in"].rearrange("(kc p) n -> p kc n", p=128)
    for gi, (co, w) in enumerate(groups):
        kb.dma("pool", wb[gi][:, :, 0:w], wsrc[:, :, co:co + w], [], [wb[gi]])
        kb.memset(zr[gi][:, 0:1], 0.0, [zr[gi]])
    hsrc = d["hT_d"].rearrange("(kc p) t -> p kc t", p=128)
    for tt in range(T // TT):
        hb = hT[tt % 2]
        t0 = tt * TT
        kb.dma("sp", hb[:], hsrc[:, :, t0:t0 + TT], [kb.K_hT], [hb])
        for gi, (co, w) in enumerate(groups):
            p = pb[gi]
            for kc in range(KC):
                kb.mm(p[0:w, 0:TT], wb[gi][:, kc, 0:w], hb[:, kc, :], [wb[gi], hb], [p], start=(kc == 0), stop=(kc == KC - 1), inc=(kc == KC - 1))
            z = zr[gi]
            kb.cp(z[0:w, 1:1 + TT], p[0:w, 0:TT], [p], [z], eng="act")
            kb.tt(tmp[0:w, :], z[0:w, 0:TT], z[0:w, 1:1 + TT], ALU.subtract, [z], [tmp])
            kb.stt(tmp[0:w, :], tmp[0:w, :], mu[0:w, gi:gi + 1], z[0:w, 1:1 + TT], ALU.mult, ALU.add, [tmp, mu, z], [tmp])
            kb.cp(z[0:w, 0:1], z[0:w, TT:TT + 1], [z], [z], eng="pool")
            if gi == 0:
                kb.act(L["twd"][0:96, t0:t0 + TT], tmp[0:96, :], AF.Tanh, [tmp], [L["twd"]])
            elif gi == 1:
                kb.cp(L["ads"][0:96, t0:t0 + TT], tmp[0:96, :], [tmp], [L["ads"]], eng="act")
            else:
                kb.act(L["sgd"][:, gi - 2, t0:t0 + TT], tmp[:], AF.Sigmoid, [tmp], [L["sgd"]])
    S.sb.release(); S.ps.release()
    return L


def phase_rwkv(kb, pairs, L):
    c, S, d, C = kb.cfg, kb.S, kb.d, kb.C
    KC, T, TT, NT, RW = c.KC, c.T, c.TT, c.NT, c.RW
    S.sb.mark(); S.ps.mark()
    rf = S.sb.alloc([128, T], F32, "rf"); kf = S.sb.alloc([128, T], F32, "kfr"); vf = S.sb.alloc([128, T], F32, "vfr")
    yf = S.sb.alloc([128, T], BF16, "yf")
    prmall = S.sb.alloc([128, 8, c.NP], F32, "prmall")
    for j, nm in enumerate(["w0", "a0", "k_k", "k_a", "r_k"]):
        kb.dma("sp", prmall[:, j, :], d[nm][:, :], [], [prmall])
    kb.dma("sp", prmall[:, 5:8, :], d["mu_rkv"].rearrange("p (j n) -> p j n", j=3), [], [prmall])
    prm_ = prmall
    bc = S.sb.alloc([128, 3, 128], F32, "bcr")
    wup = S.sb.alloc([128, 128], BF16, "wup"); aup = S.sb.alloc([128, 128], BF16, "aup"); gup = S.sb.alloc([128, 2, 128], BF16, "gup")
    H = S.sb.alloc([128, 64], F32, "Hst"); Hbf = S.sb.alloc([128, 64], BF16, "Hbf")
    for pr in pairs:
        ch0 = pr * 128
        class _P:
            def __getitem__(self, k):
                rows, cols = k
                return prmall[rows, cols.start, pr:pr + 1]
        prm = _P()
        for j, nm in enumerate(["w0_row", "lnw_row", "lnb_row"]):
            kb.dma("sp", bc[:, j, :], d[nm][0:1, ch0:ch0 + 128].partition_broadcast(128), [], [bc])
        kb.dma("pool", wup[0:96, :], d["w_up"][:, ch0:ch0 + 128], [], [wup])
        kb.dma("pool", aup[0:96, :], d["a_up"][:, ch0:ch0 + 128], [], [aup])
        kb.dma("pool", gup[:], d["g_up"].rearrange("(kc p) n -> p kc n", p=128)[:, :, ch0:ch0 + 128], [], [gup])
        S.sb.mark(); S.ps.mark()
        wb = [S.sb.alloc([128, KC, 128], BF16, "wr%d" % i) for i in range(3)]
        _h = S.sb.alloc([128, KC, TT], BF16, "hTr0")
        hT = [_h, _h]
        zr = [S.sb.alloc([128, 1 + TT], F32, "zr%d" % i) for i in range(3)]
        tmp = S.sb.alloc([128, TT], F32, "tr")
        pb = [S.ps.alloc([128, 512], F32, "pr%d" % i) for i in range(6)]
        cols = [c.OFF_R + j * RW + ch0 for j in range(3)]
        for i in range(3):
            kb.memset(zr[i][:, 0:1], 0.0, [zr[i]])
        dst = [rf, kf, vf]

        def evac(gi, tt, p):
            t0 = tt * TT
            z = zr[gi]
            kb.cp(z[:, 1:1 + TT], p[:, 0:TT], [p], [z], eng="act")
            kb.tt(tmp[:], z[:, 0:TT], z[:, 1:1 + TT], ALU.subtract, [z], [tmp])
            kb.stt(dst[gi][:, t0:t0 + TT], tmp[:], prm[:, 5 + gi:6 + gi], z[:, 1:1 + TT], ALU.mult, ALU.add, [tmp, prmall, z], [dst[gi]])
            kb.cp(z[:, 0:1], z[:, TT:TT + 1], [z], [z], eng="pool")
        inproj_groups(kb, cols, hT, wb, evac, pb)
        S.sb.release(); S.ps.release()
        S.sb.mark(); S.ps.mark()
        fA = {k: S.sb.alloc([128, TT], F32, "f_" + k) for k in ["alr", "G", "Gx", "t1", "t2", "kk", "ke"]}
        bA = {k: S.sb.alloc([128, TT], BF16, "b_" + k) for k in ["Rh", "Ah", "Kh", "Bh", "Kt", "Bt", "Pb", "vb"]}
        lwt = S.sb.alloc([128, 128], F32, "lwt")
        members = []
        for i in range(2):
            m = {k: S.sb.alloc([128, 128], F32, "r%s%d" % (k, i)) for k in ["L", "U", "Y", "L2", "U2", "D", "DT"]}
            for k in ["Ybf", "AakT", "ArbT", "ArkT"]:
                m[k] = S.sb.alloc([128, 128], BF16, "r%s%d" % (k, i))
            m["AKV"] = S.sb.alloc([128, 64], BF16, "rAKV%d" % i); m["U2v"] = S.sb.alloc([128, 64], F32, "rU2v%d" % i)
            m["Ub"] = S.sb.alloc([128, 64], BF16, "rUb%d" % i)
            members.append(m)
        Vtm = S.sb.alloc([128, 128], BF16, "Vtm"); Atm = S.sb.alloc([128, 128], BF16, "Atm")
        Bttm = S.sb.alloc([128, 128], BF16, "Bttm"); Kttm = S.sb.alloc([128, 128], BF16, "Kttm")
        WT = S.sb.alloc([128, 128], BF16, "WTr")
        Yp = S.sb.alloc([128, 128], F32, "Yp"); Yo = S.sb.alloc([128, 128], BF16, "Yo")
        st = S.sb.alloc([128, 8], F32, "str"); junk = S.sb.alloc([128, 128], BF16, "junkr")
        eGl = S.sb.alloc([128, NT], F32, "eGl")
        pt = Rot([S.ps.alloc([128, 128], F32, "qt%d" % i) for i in range(6)])
        ptb = Rot([S.ps.alloc([128, 128], BF16, "qtb%d" % i) for i in range(2)])
        kb.memset(H[:], 0.0, [H]); kb.memset(Hbf[:], 0.0, [Hbf])
        CPT = TT // 128
        for tt in range(T // TT):
            t0 = tt * TT
            ts_ = slice(t0, t0 + TT)
            p = pt.get()
            pbig = p
            for j in range(CPT):
                cs = slice(t0 + j * 128, t0 + (j + 1) * 128)
                js = slice(j * 128, (j + 1) * 128)
                n = tt * CPT + j
                p = pt.get()
                kb.mm(p[:], aup[0:96, :], L["ads"][0:96, cs], [aup, L["ads"]], [p])
                kb.act(fA["alr"][:, js], p[:], AF.Sigmoid, [p, prmall], [fA["alr"]], bias=prm[:, 1:2])
                p = pt.get()
                kb.mm(p[:], L["twd"][0:96, cs], wup[0:96, :], [L["twd"], wup], [p])
                kb.tt(lwt[:], p[:], bc[:, 0, :], ALU.add, [p, bc], [lwt])
                kb.act(lwt[:], lwt[:], AF.Sigmoid, [lwt], [lwt])
                kb.ts(lwt[:], lwt[:], -0.6065306597126334, None, ALU.mult, None, [lwt], [lwt])
                p = pt.get()
                kb.mm(p[:], lwt[:], C["triu_i"][:], [lwt, C["triu_i"]], [p])
                kb.cp(fA["G"][:, js], p[:], [p], [fA["G"]], eng="act")
                p = pt.get()
                kb.mm(p[:], lwt[:], C["triu_s"][:], [lwt, C["triu_s"]], [p])
                kb.cp(fA["Gx"][:, js], p[:], [p], [fA["Gx"]], eng="act")
                kb.act(fA["t2"][:, js], fA["G"][:, js], AF.Exp, [fA["G"]], [fA["t2"]], scale=-1.0, bias=fA["G"][:, j * 128 + 127:j * 128 + 128])
                kb.act(eGl[:, n:n + 1], fA["G"][:, j * 128 + 127:j * 128 + 128], AF.Exp, [fA["G"]], [eGl])
            alr, G, Gx, t1, t2, kk, ke = [fA[k] for k in ["alr", "G", "Gx", "t1", "t2", "kk", "ke"]]
            kb.ts(kk[:], kf[:, ts_], prm[:, 2:3], None, ALU.mult, None, [kf, prmall], [kk])
            kb.act(bA["Pb"][:], kk[:], AF.Square, [kk], [bA["Pb"]])
            for j in range(CPT):
                js = slice(j * 128, (j + 1) * 128)
                p = pt.get()
                kb.mm(p[:], C["bones_bf"][:], bA["Pb"][:, js], [C["bones_bf"], bA["Pb"]], [p])
                kb.ts(t1[:, js], p[:], 1.0, 1e-6, ALU.mult, ALU.add, [p], [t1])
            kb.recip(t1[:], t1[:], [t1], [t1])
            kb.act(t1[:], t1[:], AF.Sqrt, [t1], [t1])
            kb.tt(kk[:], kk[:], t1[:], ALU.mult, [kk, t1], [kk])
            kb.ts(ke[:], alr[:], -1.0, prm[:, 3:4], ALU.add, ALU.mult, [alr, prmall], [ke])
            kb.stt(ke[:], ke[:], 1.0, kf[:, ts_], ALU.add, ALU.mult, [ke, kf], [ke])
            kb.tt(bA["Kt"][:], ke[:], t2[:], ALU.mult, [ke, t2], [bA["Kt"]])
            kb.tt(t1[:], kk[:], alr[:], ALU.mult, [kk, alr], [t1])
            kb.tt(bA["Bt"][:], t1[:], t2[:], ALU.mult, [t1, t2], [bA["Bt"]])
            kb.stt(bA["Pb"][:], rf[:, ts_], prm[:, 4:5], ke[:], ALU.mult, ALU.mult, [rf, prmall, ke], [bA["Pb"]])
            kb.act(t2[:], G[:], AF.Exp, [G], [t2], scale=-1.0)
            kb.tt(bA["Kh"][:], ke[:], t2[:], ALU.mult, [ke, t2], [bA["Kh"]])
            kb.tt(bA["Bh"][:], t1[:], t2[:], ALU.mult, [t1, t2], [bA["Bh"]])
            kb.act(t2[:], G[:], AF.Exp, [G], [t2])
            kb.tt(bA["Rh"][:], rf[:, ts_], t2[:], ALU.mult, [rf, t2], [bA["Rh"]])
            kb.act(t2[:], Gx[:], AF.Exp, [Gx], [t2])
            kb.stt(bA["Ah"][:], kk[:], -1.0, t2[:], ALU.mult, ALU.mult, [kk, t2], [bA["Ah"]])
            kb.cp(bA["vb"][:], vf[:, ts_], [vf], [bA["vb"]], eng="pool")
            for j in range(CPT):
                n = tt * CPT + j
                js = slice(j * 128, (j + 1) * 128)
                cs = slice(t0 + j * 128, t0 + (j + 1) * 128)
                Rh, Ah, Kh, Bh, Kt, Bt, Pb, vb = [bA[k] for k in ["Rh", "Ah", "Kh", "Bh", "Kt", "Bt", "Pb", "vb"]]
                for (src, dstb) in [(vb, Vtm), (Ah, Atm), (Bt, Bttm), (Kt, Kttm)]:
                    p = ptb.get()
                    kb.tr(p[:], src[:, js], C["ident_bf"][:], [src, C["ident_bf"]], [p])
                    kb.cp(dstb[:], p[:], [p], [dstb], eng="act")
                for hh in range(2):
                    m = members[hh]
                    ps_ = slice(hh * 64, hh * 64 + 64)
                    p = pt.get()
                    kb.mm(p[:], Bh[ps_, js], Ah[ps_, js], [Bh, Ah], [p])
                    kb.stt(m["U"][:], p[:], -1.0, C["triu_s"][:], ALU.mult, ALU.mult, [p, C["triu_s"]], [m["U"]])
                    p = pt.get()
                    kb.mm(p[:], Ah[ps_, js], Bh[ps_, js], [Bh, Ah], [p])
                    kb.stt(m["L"][:], p[:], -1.0, C["tril_s"][:], ALU.mult, ALU.mult, [p, C["tril_s"]], [m["L"]])
                    p = pt.get()
                    kb.mm(p[:], Kh[ps_, js], Ah[ps_, js], [Kh, Ah], [p])
                    kb.tt(m["AakT"][:], p[:], C["triu_s"][:], ALU.mult, [p, C["triu_s"]], [m["AakT"]])
                    p = pt.get()
                    kb.mm(p[:], Bh[ps_, js], Rh[ps_, js], [Bh, Rh], [p])
                    kb.tt(m["ArbT"][:], p[:], C["triu_i"][:], ALU.mult, [p, C["triu_i"]], [m["ArbT"]])
                    p = pt.get()
                    kb.mm(p[:], Kh[ps_, js], Rh[ps_, js], [Kh, Rh], [p])
                    kb.tt(m["ArkT"][:], p[:], C["triu_i"][:], ALU.mult, [p, C["triu_i"]], [m["ArkT"]])
                neumann(kb, members, pt)
                for hh in range(2):
                    m = members[hh]
                    hs = slice(hh * 64, hh * 64 + 64)
                    kb.cp(m["Ybf"][:], m["Y"][:], [m["Y"]], [m["Ybf"]], eng="pool")
                    p = pt.get()
                    kb.mm(p[:, 0:64], m["AakT"][:], Vtm[:, hs], [m["AakT"], Vtm], [p])
                    kb.cp(m["AKV"][:], p[:, 0:64], [p], [m["AKV"]], eng="act")
                    p = pt.get()
                    kb.mm(p[:, 0:64], m["Ybf"][:], m["AKV"][:], [m["Ybf"], m["AKV"]], [p])
                    kb.cp(m["U2v"][:], p[:, 0:64], [p], [m["U2v"]], eng="dve")
                    p = pt.get()
                    kb.mm(p[:], Atm[:], m["Ybf"][:], [Atm, m["Ybf"]], [p])
                    kb.cp(WT[hs, :], p[hs, :], [p], [WT], eng="act")
                for hh in range(2):
                    m = members[hh]
                    hs = slice(hh * 64, hh * 64 + 64)
                    p = pt.get()
                    kb.mm(p[:, 0:64], WT[hs, :], Hbf[hs, :], [WT, Hbf], [p])
                    kb.tt(m["Ub"][:], p[:, 0:64], m["U2v"][:], ALU.add, [p, m["U2v"]], [m["Ub"]])
                    py = pt.get()
                    kb.mm(py[:, 0:64], Rh[hs, js], Hbf[hs, :], [Rh, Hbf], [py], start=True, stop=False, inc=False)
                    kb.mm(py[:, 0:64], m["ArkT"][:], Vtm[:, hs], [m["ArkT"], Vtm], [py], start=False, stop=False, inc=False)
                    kb.mm(py[:, 0:64], m["ArbT"][:], m["Ub"][:], [m["ArbT"], m["Ub"]], [py], start=False, stop=True)
                    kb.cp(Yp[:, hs], py[:, 0:64], [py], [Yp], eng="act")
                    ph = pt.get()
                    kb.mm(ph[:, 0:64], Kttm[:], Vtm[:, hs], [Kttm, Vtm], [ph], start=True, stop=False, inc=False)
                    kb.mm(ph[:, 0:64], Bttm[:], m["Ub"][:], [Bttm, m["Ub"]], [ph], start=False, stop=True)
                    kb.stt(H[hs, :], H[hs, :], eGl[hs, n:n + 1], ph[hs, 0:64], ALU.mult, ALU.add, [H, eGl, ph], [H])
                    kb.cp(Hbf[hs, :], H[hs, :], [H], [Hbf], eng="act")
                for hh in range(2):
                    hs = slice(hh * 64, hh * 64 + 64)
                    kb.red(st[:, 0:1], Yp[:, hs], ALU.add, [Yp], [st])
                    kb.act(junk[:, 0:64], Yp[:, hs], AF.Square, [Yp], [junk, st], accum=st[:, 1:2])
                    kb.ts(st[:, 0:2], st[:, 0:2], 1.0 / 64, None, ALU.mult, None, [st], [st])
                    kb.tt(st[:, 2:3], st[:, 0:1], st[:, 0:1], ALU.mult, [st], [st])
                    kb.tt(st[:, 2:3], st[:, 1:2], st[:, 2:3], ALU.subtract, [st], [st])
                    kb.rsqrt_col(st[:, 2:3], st, 64e-5, 1.0)
                    kb.ts(Yp[:, hs], Yp[:, hs], st[:, 0:1], st[:, 2:3], ALU.subtract, ALU.mult, [Yp, st], [Yp])
                kb.tt(Yp[:], Yp[:], bc[:, 1, :], ALU.mult, [Yp, bc], [Yp])
                kb.tt(Yp[:], Yp[:], bc[:, 2, :], ALU.add, [Yp, bc], [Yp])
                p = pt.get()
                kb.mm(p[:, 0:2], Pb[:, js], C["headsel_bf"][:, 0:2], [Pb, C["headsel_bf"]], [p])
                kb.cp(st[:, 4:6], p[:, 0:2], [p], [st], eng="act")
                for hh in range(2):
                    hs = slice(hh * 64, hh * 64 + 64)
                    kb.stt(Yp[:, hs], Vtm[:, hs], st[:, 4 + hh:5 + hh], Yp[:, hs], ALU.mult, ALU.add, [Vtm, st, Yp], [Yp])
                p = pt.get()
                kb.mm(p[:], L["sgd"][:, 0, cs], gup[:, 0, :], [L["sgd"], gup], [p], start=True, stop=False, inc=False)
                kb.mm(p[:], L["sgd"][:, 1, cs], gup[:, 1, :], [L["sgd"], gup], [p], start=False, stop=True)
                kb.tt(Yo[:], Yp[:], p[:], ALU.mult, [Yp, p], [Yo])
                p = ptb.get()
                kb.tr(p[:], Yo[:], C["ident_bf"][:], [Yo, C["ident_bf"]], [p])
                kb.cp(yf[:, cs], p[:], [p], [yf], eng="act")
        kb.dma("sp", d["oy_d"][c.GW + ch0:c.GW + ch0 + 128, :], yf[:], [yf], [kb.K_oy])
        S.sb.release(); S.ps.release()
    S.sb.release(); S.ps.release()


def phase_mix(kb, tok0):
    c, S, d, C = kb.cfg, kb.S, kb.d, kb.C
    KC, D, T, TT, TM = c.KC, c.D, c.T, c.TT, c.TM
    KH = KC // 2
    S.sb.mark(); S.ps.mark()
    mT = S.sb.alloc([128, KC, TT], BF16, "mT")
    g1bc = S.sb.alloc([128, D], F32, "g1bc")
    kb.dma("sp", g1bc[:], d["mod_d"][:, 2 * D:3 * D], [kb.K_mod], [g1bc])
    wsrc = d["w_in"].rearrange("(kc p) n -> p kc n", p=128)
    gosrc = d["w_gdn_o"].rearrange("(kc p) n -> p kc n", p=128)
    rosrc = d["w_rwkv_o"].rearrange("(kc p) n -> p kc n", p=128)
    wosrc = d["w_out"].rearrange("(kc p) n -> p kc n", p=128)
    hsrc = d["hT_d"].rearrange("(kc p) t -> p kc t", p=128)
    osrc = d["oy_d"].rearrange("(kc p) t -> p kc t", p=128)
    for tt in range(TM // TT):
        t0 = tok0 + tt * TT
        S.sb.mark(); S.ps.mark()
        hb = S.sb.alloc([128, KC, TT], BF16, "hTm"); ob = S.sb.alloc([128, KC, TT], BF16, "oTm")
        wga = [S.sb.alloc([128, KC, 128], BF16, "wga%d" % i) for i in range(2)]
        wgb = [S.sb.alloc([128, KC, 128], BF16, "wgb%d" % i) for i in range(2)]
        wgo = [S.sb.alloc([128, KH, 128], BF16, "wgo%d" % i) for i in range(2)]
        wro = [S.sb.alloc([128, KH, 128], BF16, "wro%d" % i) for i in range(2)]
        sa = S.sb.alloc([128, TT], F32, "sa"); sbb = S.sb.alloc([128, TT], F32, "sbb"); ta = S.sb.alloc([128, TT], F32, "ta")
        pp = [S.ps.alloc([128, 512], F32, "pm%d" % i) for i in range(8)]
        kb.dma("sp", hb[:], hsrc[:, :, t0:t0 + TT], [kb.K_hT], [hb])
        kb.dma("sp", ob[:], osrc[:, :, t0:t0 + TT], [kb.K_oy], [ob])
        for j in range(KC):
            i = j % 2
            kb.dma("pool", wga[i][:], wsrc[:, :, c.OFF_G + j * 128:c.OFF_G + (j + 1) * 128], [], [wga[i]])
            kb.dma("pool", wgb[i][:], wsrc[:, :, c.OFF_G + D + j * 128:c.OFF_G + D + (j + 1) * 128], [], [wgb[i]])
            kb.dma("pool", wgo[i][:], gosrc[:, :, j * 128:(j + 1) * 128], [], [wgo[i]])
            kb.dma("pool", wro[i][:], rosrc[:, :, j * 128:(j + 1) * 128], [], [wro[i]])
            pa, pb_, pc, pd = [pp[(4 * j + q) % 8] for q in range(4)]
            for kc in range(KC):
                kb.mm(pa[:, 0:TT], wga[i][:, kc, :], hb[:, kc, :], [wga[i], hb], [pa], start=(kc == 0), stop=(kc == KC - 1), inc=(kc == KC - 1))
            for kc in range(KC):
                kb.mm(pb_[:, 0:TT], wgb[i][:, kc, :], hb[:, kc, :], [wgb[i], hb], [pb_], start=(kc == 0), stop=(kc == KC - 1), inc=(kc == KC - 1))
            for kc in range(KH):
                kb.mm(pc[:, 0:TT], wgo[i][:, kc, :], ob[:, kc, :], [wgo[i], ob], [pc], start=(kc == 0), stop=(kc == KH - 1), inc=(kc == KH - 1))
            for kc in range(KH):
                kb.mm(pd[:, 0:TT], wro[i][:, kc, :], ob[:, KH + kc, :], [wro[i], ob], [pd], start=(kc == 0), stop=(kc == KH - 1), inc=(kc == KH - 1))
            kb.act(sa[:], pa[:, 0:TT], AF.Sigmoid, [pa], [sa])
            kb.act(sbb[:], pb_[:, 0:TT], AF.Sigmoid, [pb_], [sbb])
            kb.tt(ta[:], sa[:], pc[:, 0:TT], ALU.mult, [sa, pc], [ta])
            kb.tt(sbb[:], sbb[:], pd[:, 0:TT], ALU.mult, [sbb, pd], [sbb])
            kb.tt(mT[:, j, :], ta[:], sbb[:], ALU.add, [ta, sbb], [mT], eng="pool")
        S.sb.release(); S.ps.release()
        S.sb.mark(); S.ps.mark()
        NW = min(512, D)
        wo = [S.sb.alloc([128, KC, NW], BF16, "wo%d" % i) for i in range(2)]
        xt = [S.sb.alloc([128, D], F32, "xtm%d" % i) for i in range(TT // 128)]
        po = [S.ps.alloc([128, 512], F32, "po%d" % i) for i in range(4)]
        for i in range(TT // 128):
            kb.dma("sp", xt[i][:], d["x"][t0 + i * 128:t0 + (i + 1) * 128, :], [], [xt[i]])
        pi = 0
        for n in range(D // NW):
            w = wo[n % 2]
            kb.dma("pool", w[:], wosrc[:, :, n * NW:(n + 1) * NW], [], [w])
            for i in range(TT // 128):
                p = po[pi % 4]; pi += 1
                for kc in range(KC):
                    kb.mm(p[:, 0:NW], mT[:, kc, i * 128:(i + 1) * 128], w[:, kc, :], [mT, w], [p], start=(kc == 0), stop=(kc == KC - 1), inc=(kc == KC - 1))
                cs = slice(n * NW, (n + 1) * NW)
                kb.tt(ta_ := None, None, None, None, [], []) if False else None
                kb.S.op("dve", (lambda e, i=i, p=p, cs=cs: e.tensor_tensor(out=p[:, 0:NW], in0=p[:, 0:NW], in1=g1bc[:, cs], op=ALU.mult)), reads=[g1bc], writes=[p])
                kb.tt(xt[i][:, cs], xt[i][:, cs], p[:, 0:NW], ALU.add, [xt[i], p], [xt[i]])
        for i in range(TT // 128):
            r0 = tt * TT + i * 128
            kb.dma("sp", d["xmid_d"][r0:r0 + 128, :], xt[i][:], [xt[i]], [kb.K_xmid])
        S.sb.release(); S.ps.release()
    S.sb.release(); S.ps.release()


def phase_route(kb):
    c, S, d, C = kb.cfg, kb.S, kb.d, kb.C
    KC, D, TM, NBLK = c.KC, c.D, c.TM, c.NBLK
    NTm = TM // 128
    R = {}
    R["wts"] = S.sb.alloc([128, NTm, 2], F32, "wts")
    R["desti"] = S.sb.alloc([128, NTm, 2], I32, "desti")
    R["idxW"] = S.sb.alloc([128, NBLK], I32, "idxW")
    S.sb.mark(); S.ps.mark()
    M1 = S.sb.alloc([128, NTm, 64], F32, "M1"); M2 = S.sb.alloc([128, NTm, 64], F32, "M2")
    Msb = S.sb.alloc([128, NTm, 64], BF16, "Msb")
    destf = S.sb.alloc([128, NTm, 2], F32, "destf")
    A2 = S.sb.alloc([128, D], F32, "A2"); B2 = S.sb.alloc([128, D], F32, "B2")
    wr = S.sb.alloc([128, KC, 72], F32, "wr"); brb = S.sb.alloc([128, 72], F32, "brb")
    xm = [S.sb.alloc([128, D], F32, "xmr%d" % i) for i in range(2)]
    h2b = [S.sb.alloc([128, D], BF16, "h2b%d" % i) for i in range(2)]
    junk = S.sb.alloc([128, D], BF16, "junkq")
    hTc = [S.sb.alloc([128, 128], F32, "hTc%d" % i) for i in range(4)]
    lg = S.sb.alloc([128, 72], F32, "lg"); sm = S.sb.alloc([128, 64], F32, "sm"); st = S.sb.alloc([128, 16], F32, "stq")
    ptr = Rot([S.ps.alloc([128, 128], F32, "ptq%d" % i) for i in range(5)])
    pl = S.ps.alloc([128, 128], F32, "plq")
    kb.dma("sp", A2[:], d["mod_d"][:, 4 * D:5 * D], [kb.K_mod], [A2])
    kb.dma("sp", B2[:], d["n2g"][0:1, :].partition_broadcast(128), [], [B2])
    kb.stt(A2[:], A2[:], 1.0, B2[:], ALU.add, ALU.mult, [A2, B2], [A2])
    kb.dma("sp", B2[:], d["mod_d"][:, 3 * D:4 * D], [kb.K_mod], [B2])
    kb.dma("sp", wr[:], d["wr"].rearrange("(kc p) n -> p kc n", p=128), [], [wr])
    kb.dma("sp", brb[:], d["br"][0:1, :].partition_broadcast(128), [], [brb])
    for n in range(NTm):
        x_ = xm[n % 2]; hb = h2b[n % 2]
        kb.dma("sp", x_[:], d["xmid_d"][n * 128:(n + 1) * 128, :], [kb.K_xmid], [x_])
        kb.act(junk[:], x_[:], AF.Square, [x_], [junk, st], accum=st[:, 0:1])
        kb.rsqrt_col(st[:, 0:1], st, 1e-6, 1.0 / D)
        kb.stt(x_[:], x_[:], st[:, 0:1], A2[:], ALU.mult, ALU.mult, [x_, st, A2], [x_])
        kb.tt(x_[:], x_[:], B2[:], ALU.add, [x_, B2], [x_])
        kb.cp(hb[:], x_[:], [x_], [hb], eng="pool")
        kb.dma("sp", d["h2_d"][n * 128:(n + 1) * 128, :], hb[:], [hb], [kb.K_h2])
        for kc in range(KC):
            p = ptr.get(); hc = hTc[kc % 4]
            kb.tr(p[:], x_[:, kc * 128:(kc + 1) * 128], C["ident"][:], [x_, C["ident"]], [p])
            kb.cp(hc[:], p[:], [p], [hc], eng=("act" if kc % 2 == 0 else "dve"))
            kb.mm(pl[:, 0:72], hc[:], wr[:, kc, :], [hc, wr], [pl], start=(kc == 0), stop=(kc == KC - 1), inc=(kc == KC - 1))
        kb.tt(lg[:], pl[:, 0:72], brb[:], ALU.add, [pl, brb], [lg])
        kb.red(st[:, 1:2], lg[:, 0:8], ALU.max, [lg], [st])
        ohg = sm[:, 0:8]
        kb.ts(ohg, lg[:, 0:8], st[:, 1:2], None, ALU.is_equal, None, [lg, st], [sm])
        kb.ts(st[:, 2:3], st[:, 1:2], -1.0, None, ALU.mult, None, [st], [st])
        kb.act(sm[:, 8:16], lg[:, 0:8], AF.Exp, [lg, st], [sm, st], bias=st[:, 2:3], accum=st[:, 3:4])
        kb.recip(st[:, 3:4], st[:, 3:4], [st], [st])
        esel = sm[:, 16:24]
        kb.ts(esel, lg[:, 8:16], sm[:, 0:1], None, ALU.mult, None, [lg, sm], [sm])
        for g in range(1, 8):
            kb.stt(esel, lg[:, 8 + 8 * g:16 + 8 * g], sm[:, g:g + 1], esel, ALU.mult, ALU.add, [lg, sm], [sm])
        kb.red(st[:, 4:5], esel, ALU.max, [sm], [st])
        mk1 = sm[:, 24:32]; es2 = sm[:, 32:40]; mk2 = sm[:, 40:48]
        kb.ts(mk1, esel, st[:, 4:5], None, ALU.is_equal, None, [sm, st], [sm])
        kb.stt(es2, mk1, -1e30, esel, ALU.mult, ALU.add, [sm], [sm])
        kb.red(st[:, 5:6], es2, ALU.max, [sm], [st])
        kb.ts(mk2, es2, st[:, 5:6], None, ALU.is_equal, None, [sm, st], [sm])
        kb.tt(st[:, 6:7], st[:, 4:5], st[:, 5:6], ALU.subtract, [st], [st])
        kb.act(st[:, 6:7], st[:, 6:7], AF.Sigmoid, [st], [st])
        kb.ts(st[:, 7:8], st[:, 6:7], -1.0, 1.0, ALU.mult, ALU.add, [st], [st])
        kb.ts(R["wts"][:, n, :], st[:, 6:8], st[:, 3:4], None, ALU.mult, None, [st], [R["wts"]])
        for g in range(8):
            kb.ts(M1[:, n, 8 * g:8 * g + 8], mk1, sm[:, g:g + 1], None, ALU.mult, None, [sm], [M1], eng="pool")
            kb.ts(M2[:, n, 8 * g:8 * g + 8], mk2, sm[:, g:g + 1], None, ALU.mult, None, [sm], [M2], eng="pool")
        kb.tt(Msb[:, n, :], M1[:, n, :], M2[:, n, :], ALU.add, [M1, M2], [Msb], eng="pool")
    cnt = S.sb.alloc([128, 8], F32, "cnt")
    pc = ptr.get()
    for n in range(NTm):
        kb.mm(pc[0:64, 0:1], Msb[:, n, :], C["ones_bf"][:, 0:1], [Msb, C["ones_bf"]], [pc], start=(n == 0), stop=(n == NTm - 1), inc=(n == NTm - 1))
    kb.cp(cnt[0:64, 0:1], pc[0:64, 0:1], [pc], [cnt], eng="act")
    thr = S.sb.alloc([128, 128], F32, "thr")
    assert 2 * TM // 128 <= 128
    kb.ts(thr[0:64, :], C["iota_f"][0:64, :], 128.0, None, ALU.mult, None, [C["iota_f"]], [thr])
    kb.ts(thr[0:64, :], thr[0:64, :], cnt[0:64, 0:1], None, ALU.is_lt, None, [thr, cnt], [thr])
    kb.red(cnt[0:64, 2:3], thr[0:64, :], ALU.add, [thr], [cnt])
    nbrep = S.sb.alloc([128, 128], F32, "nbrep")
    kb.ts(nbrep[0:64, :], C["ones"][0:64, :], cnt[0:64, 2:3], None, ALU.mult, None, [C["ones"], cnt], [nbrep])
    p = ptr.get()
    kb.mm(p[:, 0:64], nbrep[0:64, :], C["triu_s"][0:64, 0:64], [nbrep, C["triu_s"]], [p])
    startrow = S.sb.alloc([128, 64], F32, "startrow")
    kb.ts(startrow[:], p[:, 0:64], 128.0, None, ALU.mult, None, [p], [startrow])
    p = ptr.get()
    kb.mm(p[0:64, 0:1], C["triu_i"][0:64, 0:64], cnt[0:64, 2:3], [C["triu_i"], cnt], [p])
    kb.cp(cnt[0:64, 3:4], p[0:64, 0:1], [p], [cnt], eng="act")
    Bm = S.sb.alloc([128, 128], F32, "Bm")
    kb.ts(Bm[0:64, 0:NBLK], C["iota_f"][0:64, 0:NBLK], cnt[0:64, 3:4], None, ALU.is_ge, None, [C["iota_f"], cnt], [Bm])
    p = ptr.get()
    kb.mm(p[:, 0:NBLK], C["ones"][0:64, :], Bm[0:64, 0:NBLK], [C["ones"], Bm], [p])
    idxf = S.sb.alloc([128, 128], F32, "idxf")
    kb.stt(idxf[:, 0:NBLK], p[:, 0:NBLK], 128.0, C["iota_p"][:, 0:NBLK], ALU.mult, ALU.add, [p, C["iota_p"]], [idxf])
    kb.cp(R["idxW"][:], idxf[:, 0:NBLK], [idxf], [R["idxW"]], eng="dve")
    carry = S.sb.alloc([128, 64], F32, "carry"); df = S.sb.alloc([128, 64], F32, "df"); tmp = S.sb.alloc([128, 64], F32, "tmpq")
    kb.cp(carry[:], startrow[:], [startrow], [carry], eng="pool")
    for n in range(NTm):
        p = ptr.get()
        kb.mm(p[:, 0:64], C["triu_s_bf"][:], Msb[:, n, :], [C["triu_s_bf"], Msb], [p])
        kb.tt(df[:], p[:, 0:64], carry[:], ALU.add, [p, carry], [df])
        kb.tt(tmp[:], df[:], M1[:, n, :], ALU.mult, [df, M1], [tmp])
        kb.red(destf[:, n, 0:1], tmp[:], ALU.add, [tmp], [destf])
        kb.tt(tmp[:], df[:], M2[:, n, :], ALU.mult, [df, M2], [tmp])
        kb.red(destf[:, n, 1:2], tmp[:], ALU.add, [tmp], [destf])
        p = ptr.get()
        kb.mm(p[:, 0:64], C["ones_bf"][:], Msb[:, n, :], [C["ones_bf"], Msb], [p])
        kb.tt(carry[:], carry[:], p[:, 0:64], ALU.add, [carry, p], [carry])
    kb.cp(R["desti"][:].rearrange("p n j -> p (n j)"), destf[:].rearrange("p n j -> p (n j)"), [destf], [R["desti"]], eng="dve")
    fill = S.sb.alloc([128, NBLK], I32, "fill"); tokv = S.sb.alloc([128, NTm], I32, "tokv")
    zrow = S.sb.alloc([128, D], BF16, "zrow")
    kb.memset(fill[:], TM, [fill]); kb.memset(zrow[0:1, :], 0.0, [zrow])
    kb.dma("sp", tokv[:], d["tokiota"][:, :], [], [tokv])
    kb.dma("sp", d["h2_d"][TM:TM + 1, :], zrow[0:1, :], [zrow], [kb.K_h2])
    kb.dma("sp", d["tokid_d"].rearrange("(p b) o -> p (b o)", p=128), fill[:], [fill], [kb.K_tokid])
    for n in range(NTm):
        for j in range(2):
            kb.S.dma("pool", (lambda e, n=n, j=j: e.indirect_dma_start(
                out=d["tokid_d"][:, :], out_offset=bass.IndirectOffsetOnAxis(ap=R["desti"][:, n, j:j + 1], axis=0),
                in_=tokv[:, n:n + 1], in_offset=None)),
                reads=[R["desti"], tokv, kb.K_tokid], writes=[kb.K_tokid2], semkey="tokscat")
    S.sb.release(); S.ps.release()
    return R


def phase_moe(kb, R):
    c, S, d, C = kb.cfg, kb.S, kb.d, kb.C
    KC, D, TM, NBLK, DE, FC = c.KC, c.D, c.TM, c.NBLK, c.DE, c.FC
    S.sb.mark(); S.ps.mark()
    w1t = S.sb.alloc([128, KC * DE], BF16, "w1t"); w3t = S.sb.alloc([128, KC * DE], BF16, "w3t"); w2t = S.sb.alloc([128, FC * D], BF16, "w2t")
    xb = [S.sb.alloc([128, D], BF16, "xb%d" % i) for i in range(2)]
    idb = [S.sb.alloc([128, 1], I32, "idb%d" % i) for i in range(2)]
    xbT = S.sb.alloc([128, KC, 128], BF16, "xbT"); aT = S.sb.alloc([128, FC, 128], BF16, "aT")
    s1 = S.sb.alloc([128, DE], F32, "s1"); ab = S.sb.alloc([128, DE], BF16, "ab_")
    yb = [S.sb.alloc([128, D], F32, "yb%d" % i) for i in range(2)]
    pst = Rot([S.ps.alloc([128, 4, 128], BF16, "pe%d" % i) for i in range(2)])
    ph = [S.ps.alloc([128, 512], F32, "ph%d" % i) for i in range(2)]
    py = Rot([S.ps.alloc([128, 512], F32, "py%d" % i) for i in range(4)])
    NW = min(512, D)
    for b in range(NBLK):
        i = b % 2
        kb.dma("sp", idb[i][:], d["tokid_d"][b * 128:(b + 1) * 128, :], [kb.K_tokid2], [idb[i]])
        kb.S.dma("pool", (lambda e, i=i: e.indirect_dma_start(out=xb[i][:], out_offset=None, in_=d["h2_d"][:, :],
                 in_offset=bass.IndirectOffsetOnAxis(ap=idb[i][:, 0:1], axis=0))),
                 reads=[idb[i], kb.K_h2], writes=[xb[i]])
        for (wt, nm) in [(w1t, "w1"), (w3t, "w3"), (w2t, "w2")]:
            kb.S.dma("pool", (lambda e, wt=wt, nm=nm, b=b: e.indirect_dma_start(out=wt[:], out_offset=None, in_=d[nm][:, :],
                     in_offset=bass.IndirectOffsetOnAxis(ap=R["idxW"][:, b:b + 1], axis=0), bounds_check=kb.breg(e, 64 * 128 - 1), oob_is_err=False)),
                     reads=[R["idxW"]], writes=[wt])
        G = min(4, KC)
        for g in range(KC // G):
            p = pst.get()
            for j in range(G):
                kc = g * G + j
                kb.tr(p[:, j, :], xb[i][:, kc:D:KC], C["ident_bf"][:], [xb[i], C["ident_bf"]], [p], inc=(j == G - 1))
            kb.cp(xbT[:, g * G:(g + 1) * G, :], p[:, 0:G, :], [p], [xbT], eng=("act" if g % 2 == 0 else "dve"))
        for kc in range(KC):
            kb.mm(ph[0][:, 0:DE], xbT[:, kc, :], w1t[:, kc * DE:(kc + 1) * DE], [xbT, w1t], [ph[0]], start=(kc == 0), stop=(kc == KC - 1), inc=(kc == KC - 1))
        for kc in range(KC):
            kb.mm(ph[1][:, 0:DE], xbT[:, kc, :], w3t[:, kc * DE:(kc + 1) * DE], [xbT, w3t], [ph[1]], start=(kc == 0), stop=(kc == KC - 1), inc=(kc == KC - 1))
        kb.act(s1[:], ph[0][:, 0:DE], AF.Silu, [ph[0]], [s1])
        kb.tt(ab[:], s1[:], ph[1][:, 0:DE], ALU.mult, [s1, ph[1]], [ab])
        p = pst.get()
        for fc in range(FC):
            kb.tr(p[:, fc, :], ab[:, fc:DE:FC], C["ident_bf"][:], [ab, C["ident_bf"]], [p], inc=(fc == FC - 1))
        kb.cp(aT[:], p[:, 0:FC, :], [p], [aT], eng="act")
        for n in range(D // NW):
            pq = py.get()
            for fc in range(FC):
                kb.mm(pq[:, 0:NW], aT[:, fc, :], w2t[:, fc * D + n * NW:fc * D + (n + 1) * NW], [aT, w2t], [pq], start=(fc == 0), stop=(fc == FC - 1), inc=(fc == FC - 1))
            kb.cp(yb[i][:, n * NW:(n + 1) * NW], pq[:, 0:NW], [pq], [yb[i]], eng=("act" if n % 2 == 0 else "dve"))
        kb.dma("sp", d["yb_d"][b * 128:(b + 1) * 128, :], yb[i][:], [yb[i]], [kb.K_yb])
    S.sb.release(); S.ps.release()


def phase_final(kb, R):
    c, S, d, C = kb.cfg, kb.S, kb.d, kb.C
    D, TM, NBLK = c.D, c.TM, c.NBLK
    S.sb.mark()
    g2 = S.sb.alloc([128, D], F32, "g2bc"); nf = S.sb.alloc([128, D], F32, "nfbc")
    r1 = [S.sb.alloc([128, D], F32, "r1_%d" % i) for i in range(2)]; r2 = [S.sb.alloc([128, D], F32, "r2_%d" % i) for i in range(2)]
    xm = [S.sb.alloc([128, D], F32, "xmf%d" % i) for i in range(2)]
    junk = S.sb.alloc([128, D], BF16, "junkf"); st = S.sb.alloc([128, 2], F32, "stf")
    kb.dma("sp", g2[:], d["mod_d"][:, 5 * D:6 * D], [kb.K_mod], [g2])
    kb.dma("sp", nf[:], d["nfg"][0:1, :].partition_broadcast(128), [], [nf])
    for n in range(TM // 128):
        i = n % 2
        for j, rb in enumerate([r1[i], r2[i]]):
            kb.S.dma("pool", (lambda e, rb=rb, n=n, j=j: e.indirect_dma_start(out=rb[:], out_offset=None, in_=d["yb_d"][:, :],
                     in_offset=bass.IndirectOffsetOnAxis(ap=R["desti"][:, n, j:j + 1], axis=0))),
                     reads=[R["desti"], kb.K_yb], writes=[rb])
        kb.dma("sp", xm[i][:], d["xmid_d"][n * 128:(n + 1) * 128, :], [kb.K_xmid], [xm[i]])
        kb.ts(r1[i][:], r1[i][:], R["wts"][:, n, 0:1], None, ALU.mult, None, [r1[i], R["wts"]], [r1[i]])
        kb.stt(r1[i][:], r2[i][:], R["wts"][:, n, 1:2], r1[i][:], ALU.mult, ALU.add, [r2[i], R["wts"], r1[i]], [r1[i]])
        kb.tt(r1[i][:], r1[i][:], g2[:], ALU.mult, [r1[i], g2], [r1[i]], eng="pool")
        kb.tt(xm[i][:], xm[i][:], r1[i][:], ALU.add, [xm[i], r1[i]], [xm[i]])
        kb.act(junk[:], xm[i][:], AF.Square, [xm[i]], [junk, st], accum=st[:, 0:1])
        kb.rsqrt_col(st[:, 0:1], st, 1e-6, 1.0 / D)
        kb.stt(xm[i][:], xm[i][:], st[:, 0:1], nf[:], ALU.mult, ALU.mult, [xm[i], st, nf], [xm[i]])
        kb.dma("sp", d["out"][n * 128:(n + 1) * 128, :], xm[i][:], [xm[i]], [kb.K_out])
    S.sb.release()


NCORES = 4


def build_program(cfg):
    kb = KB(cfg)
    declare_inputs(kb)
    for k in ["mod_d", "hT_d", "oy_d", "xmid_d", "h2", "tokid", "tokid2", "yb", "out"]:
        setattr(kb, "K_" + k.replace("_d", ""), Key(k))
    S = kb.S
    load_consts(kb)
    phase_mod(kb)
    phase_norm1(kb)
    S.sb.mark(); P = phase_gdn_pre(kb, list(range(cfg.HG))); phase_gdn(kb, list(range(cfg.HG)), P); S.sb.release()
    S.sb.mark(); L = phase_rwkv_pre(kb); phase_rwkv(kb, list(range(cfg.NP)), L); S.sb.release()
    phase_mix(kb, 0)
    S.sb.mark(); R = phase_route(kb); phase_moe(kb, R); phase_final(kb, R); S.sb.release()
    S.wait_all("sp")
    assert S.simulate()
    S.emit()
    return kb


def kernel(**inputs):
    x = np.asarray(inputs["x"])
    B, T, D = x.shape
    DE = np.asarray(inputs["w1"]).shape[-1]
    cfg = Cfg(D, T, DE)
    kb = build_program(cfg)
    ncores = min(NCORES, B) if B < NCORES else NCORES
    in_maps = [host_inputs(cfg, inputs, b) for b in range(B)]
    res = run_bass_kernel_spmd(kb.nc, in_maps, core_ids=list(range(B)))
    out = np.stack([np.asarray(res.results[b]["out"]) for b in range(B)], axis=0)
    return out.astype(np.float32)
```

```python
import numpy as np
from contextlib import ExitStack
import concourse.bass as bass
import concourse.mybir as mybir

F32 = mybir.dt.float32
BF16 = mybir.dt.bfloat16
I32 = mybir.dt.int32
U8 = mybir.dt.uint8
AF = mybir.ActivationFunctionType
ALU = mybir.AluOpType
AX = mybir.AxisListType
DSZ = {F32: 4, BF16: 2, I32: 4, U8: 1}

GRAN = 512
SEM_LIMIT = 30000
DMA_SEM_LIMIT = 60000


class Buf:
    def __init__(self, arena, lo, nbytes, dtype, shape, name):
        self.arena, self.lo, self.hi, self.dtype, self.shape, self.name = arena, lo, lo + nbytes, dtype, shape, name
        ap = arena.t[:, lo:lo + nbytes]
        if dtype != U8:
            ap = ap.bitcast(dtype)
        P = shape[0]
        if P < 128:
            ap = ap[0:P]
        if len(shape) > 2:
            names = " ".join("d%d" % i for i in range(len(shape) - 1))
            kw = {"d%d" % i: shape[i + 1] for i in range(len(shape) - 1)}
            ap = ap.rearrange("p (%s) -> p %s" % (names, names), **kw)
        self.ap = ap

    def __getitem__(self, k):
        return self.ap[k]

    def grans(self):
        gr = 2048 if self.arena.id == "ps" else GRAN
        return [(self.arena.id, g) for g in range(self.lo // gr, (self.hi + gr - 1) // gr)]

    def sub(self, lo_el, n_el, shape=None):
        sz = DSZ[self.dtype]
        return Buf(self.arena, self.lo + lo_el * sz, n_el * sz, self.dtype, shape or [self.shape[0], n_el],
                   self.name + ".s")


class Arena:
    def __init__(self, t, nbytes, aid):
        self.t, self.nbytes, self.id, self.top, self.marks = t, nbytes, aid, 0, []

    def alloc(self, shape, dtype, name="b", align=64):
        if self.id == "ps":
            align = 2048
        n = int(np.prod(shape[1:])) * DSZ[dtype]
        lo = (self.top + align - 1) // align * align
        assert lo + n <= self.nbytes, "arena %s overflow: %s needs %d at %d / %d" % (self.id, name, n, lo, self.nbytes)
        self.top = lo + n
        return Buf(self, lo, n, dtype, list(shape), name)

    def mark(self):
        self.marks.append(self.top)

    def release(self):
        self.top = self.marks.pop()


class Key:
    def __init__(self, name):
        self.name = name

    def grans(self):
        return [("key", self.name)]


ENGS = ["pe", "act", "dve", "pool", "sp"]


class Sched:
    def __init__(self, nc, es, sbuf_bytes=190 * 1024):
        self.nc, self.es = nc, es
        self.sb = Arena(es.enter_context(nc.sbuf_tensor("arena", [128, sbuf_bytes], U8)), sbuf_bytes, "sb")
        self.ps = Arena(es.enter_context(nc.psum_tensor("psarena", [128, 16384], U8)), 16384, "ps")
        self.streams = {e: [] for e in ENGS}
        self.eng_sems = {e: [] for e in ENGS}
        self.eng_count = {e: 0 for e in ENGS}
        self.pending = {e: [] for e in ENGS}
        self.lastw = {}
        self.readers = {}
        self.waited = {e: {} for e in ENGS}
        self.dma_sems = {}
        self.nsem = 0
        self.nops = 0

    def _newsem(self, name):
        self.nsem += 1
        return self.es.enter_context(self.nc.semaphore("%s_%d" % (name, self.nsem)))

    @staticmethod
    def _excl(reads, writes):
        r2 = [r for r in reads if not (isinstance(r, Buf) and r.arena.id == "ps")]
        w2 = list(writes) + [r for r in reads if isinstance(r, Buf) and r.arena.id == "ps"]
        return r2, w2

    def _deps(self, reads, writes):
        reads, writes = self._excl(reads, writes)
        toks = {}

        def add(t):
            if t is None:
                return
            k = id(t[0])
            if k not in toks or toks[k][1] < t[1]:
                toks[k] = t
        for r in reads:
            for g in r.grans():
                add(self.lastw.get(g))
        for w in writes:
            for g in w.grans():
                add(self.lastw.get(g))
                for t in self.readers.get(g, {}).values():
                    add(t)
        return toks

    def _record(self, reads, writes, tok):
        reads, writes = self._excl(reads, writes)
        for r in reads:
            for g in r.grans():
                d = self.readers.setdefault(g, {})
                d[id(tok[0])] = tok
        for w in writes:
            for g in w.grans():
                self.lastw[g] = tok
                self.readers[g] = {}

    def _waits(self, stream, toks):
        out = []
        wd = self.waited[stream]
        for k, t in toks.items():
            if t[1] == 0:
                continue
            if len(t) > 2 and t[2] == stream and t[3] > self.eng_count[stream]:
                continue
            if wd.get(k, 0) >= t[1]:
                continue
            wd[k] = t[1]
            out.append(t)
        return out

    def op(self, eng, fn, reads=(), writes=(), inc=True):
        toks = self._deps(reads, writes)
        waits = self._waits(eng, toks)
        cnt = self.eng_count[eng]
        if cnt % SEM_LIMIT == 0 and (not self.eng_sems[eng] or cnt // SEM_LIMIT >= len(self.eng_sems[eng])):
            self.eng_sems[eng].append(self._newsem(eng))
        sem = self.eng_sems[eng][cnt // SEM_LIMIT]
        if inc:
            self.eng_count[eng] = cnt + 1
            tok = (sem, cnt % SEM_LIMIT + 1, eng, cnt + 1)
        else:
            tok = (sem, cnt % SEM_LIMIT + 1, eng, cnt + 1)
            self.pending[eng].append(1)
        if inc:
            self.pending[eng] = []
        self.streams[eng].append((fn, waits, (sem, 1) if inc else None))
        self._record(reads, writes, tok)
        self.nops += 1
        return tok

    def dma(self, queue, fns, reads=(), writes=(), semkey=None):
        if not isinstance(fns, (list, tuple)):
            fns = [fns]
        toks = self._deps(reads, writes)
        waits = self._waits(queue, toks)
        if semkey is None:
            semkey = (writes[0] if writes else reads[0]).grans()[0]
        if semkey not in self.dma_sems:
            self.dma_sems[semkey] = [self._newsem("dma"), 0]
        rec = self.dma_sems[semkey]
        if rec[1] + 16 * len(fns) > DMA_SEM_LIMIT:
            rec[0], rec[1] = self._newsem("dma"), 0
        for i, fn in enumerate(fns):
            self.streams[queue].append((fn, waits if i == 0 else [], (rec[0], 16)))
        rec[1] += 16 * len(fns)
        tok = (rec[0], rec[1])
        self._record(reads, writes, tok)
        self.nops += 1
        return tok

    def wait_all(self, eng):
        toks = {}
        for g, t in self.lastw.items():
            k = id(t[0])
            if k not in toks or toks[k][1] < t[1]:
                toks[k] = t
        waits = self._waits(eng, toks)
        self.streams[eng].append((None, waits, None))

    def emit(self):
        for e in ENGS:
            assert not self.pending[e], "engine %s has trailing inc=False ops" % e
        nc = self.nc
        with nc.Block() as block:
            def run(engobj, lst):
                for fn, waits, inc in lst:
                    for w in waits:
                        engobj.wait_ge(w[0], w[1])
                    if fn is None:
                        continue
                    ins = fn(engobj)
                    if inc is not None:
                        ins.then_inc(inc[0], inc[1])

            @block.tensor
            def _(t):
                run(t, self.streams["pe"])

            @block.scalar
            def _(t):
                run(t, self.streams["act"])

            @block.vector
            def _(t):
                run(t, self.streams["dve"])

            @block.gpsimd
            def _(t):
                run(t, self.streams["pool"])

            @block.sync
            def _(t):
                run(t, self.streams["sp"])


def simulate(self):
    pos = {e: 0 for e in ENGS}
    val = {}
    progress = True
    while progress:
        progress = False
        for e in ENGS:
            lst = self.streams[e]
            while pos[e] < len(lst):
                fn, waits, inc = lst[pos[e]]
                if all(val.get(id(w[0]), 0) >= w[1] for w in waits):
                    if inc is not None:
                        val[id(inc[0])] = val.get(id(inc[0]), 0) + inc[1]
                    pos[e] += 1
                    progress = True
                else:
                    break
    stuck = {e: (pos[e], len(self.streams[e])) for e in ENGS if pos[e] < len(self.streams[e])}
    for e, (p, n) in stuck.items():
        fn, waits, inc = self.streams[e][p]
        print("STUCK", e, p, n, [(val.get(id(w[0]), 0), w[1], w[2:] if len(w) > 2 else "dma") for w in waits])
    return not stuck


Sched.simulate = simulate

import numpy as np
from contextlib import ExitStack
from concourse.bass_utils import run_bass_kernel_spmd


class Cfg:
    def __init__(s, D, T, DE, pair=False):
        s.D, s.T, s.DE, s.pair = D, T, DE, pair
        s.KC = D // 128
        s.HG = D // 256
        s.GW = s.HG * 128
        s.HR = D // 128
        s.RW = s.HR * 64
        s.NP = s.RW // 128
        s.CONVC = 3 * s.GW
        s.OFF_Z = s.CONVC
        s.OFF_A = s.OFF_Z + s.GW
        s.OFF_B = s.OFF_A + s.HG
        s.OFF_R = s.OFF_B + s.HG
        s.RSC = 3 * s.RW + 448
        s.OFF_G = s.OFF_R + s.RSC
        s.INC = s.OFF_G + 2 * D
        s.NE, s.NG, s.EPG = 64, 8, 8
        s.NT = T // 128
        s.TT = min(512, T)
        s.NTT = T // s.TT
        s.FC = DE // 128
        s.TM = T // 2 if pair else T
        s.NBLK = (2 * s.TM + 64 * 127 + 127) // 128


class KB:
    def __init__(s, cfg, debug=()):
        s.cfg = cfg
        s.nc = bass.Bass("TRN2", target_bir_lowering=False)
        s.es = ExitStack()
        s.S = Sched(s.nc, s.es)
        s.d = {}
        s.debug = debug

    def breg(s, e, val):
        if not hasattr(s, "_bregs"):
            s._bregs = {}
        if val not in s._bregs:
            s._bregs[val] = e.to_reg(val)
        return s._bregs[val]

    def din(s, name, shape, dt=F32):
        s.d[name] = s.nc.dram_tensor(name, list(shape), dt, kind="ExternalInput").ap()
        return s.d[name]

    def dout(s, name, shape, dt=F32):
        s.d[name] = s.nc.dram_tensor(name, list(shape), dt, kind="ExternalOutput").ap()
        return s.d[name]

    def dscr(s, name, shape, dt=F32):
        s.d[name] = s.nc.dram_tensor(name, list(shape), dt, kind="Internal").ap()
        return s.d[name]

    def mm(s, out, lhsT, rhs, R, W, start=True, stop=True, inc=True):
        return s.S.op("pe", lambda e: e.matmul(out, lhsT, rhs, start=start, stop=stop), reads=R, writes=W, inc=inc)

    def tr(s, out, in_, ident, R, W, inc=True):
        return s.S.op("pe", lambda e: e.transpose(out, in_, ident), reads=R, writes=W, inc=inc)

    def act(s, out, in_, func, R, W, scale=1.0, bias=0.0, accum=None, eng="act"):
        if accum is not None:
            return s.S.op(eng, lambda e: e.activation(out=out, in_=in_, func=func, scale=scale, bias=bias, accum_out=accum), reads=R, writes=W)
        return s.S.op(eng, lambda e: e.activation(out=out, in_=in_, func=func, scale=scale, bias=bias), reads=R, writes=W)

    def ts(s, out, in0, s1, s2, op0, op1, R, W, eng="dve", accum=None):
        if op1 is None:
            return s.S.op(eng, lambda e: e.tensor_scalar(out=out, in0=in0, scalar1=s1, scalar2=None, op0=op0), reads=R, writes=W)
        if accum is not None:
            return s.S.op(eng, lambda e: e.tensor_scalar(out=out, in0=in0, scalar1=s1, scalar2=s2, op0=op0, op1=op1, accum_out=accum), reads=R, writes=W)
        return s.S.op(eng, lambda e: e.tensor_scalar(out=out, in0=in0, scalar1=s1, scalar2=s2, op0=op0, op1=op1), reads=R, writes=W)

    def tt(s, out, in0, in1, op, R, W, eng="dve"):
        return s.S.op(eng, lambda e: e.tensor_tensor(out=out, in0=in0, in1=in1, op=op), reads=R, writes=W)

    def stt(s, out, in0, scalar, in1, op0, op1, R, W, eng="dve"):
        return s.S.op(eng, lambda e: e.scalar_tensor_tensor(out=out, in0=in0, scalar=scalar, in1=in1, op0=op0, op1=op1), reads=R, writes=W)

    def cp(s, out, in_, R, W, eng="dve"):
        if eng == "act":
            return s.S.op("act", lambda e: e.activation(out=out, in_=in_, func=AF.Copy), reads=R, writes=W)
        return s.S.op(eng, lambda e: e.tensor_copy(out=out, in_=in_), reads=R, writes=W)

    def red(s, out, in_, op, R, W, eng="dve"):
        return s.S.op(eng, lambda e: e.tensor_reduce(out=out, in_=in_, axis=AX.X, op=op), reads=R, writes=W)

    def recip(s, out, in_, R, W):
        return s.S.op("dve", lambda e: e.reciprocal(out=out, in_=in_), reads=R, writes=W)

    def memset(s, ap, val, W, eng="pool"):
        return s.S.op(eng, lambda e: e.memset(ap, val), writes=W)

    def dma(s, q, out, in_, R, W, semkey=None):
        return s.S.dma(q, lambda e: e.dma_start(out=out, in_=in_), reads=R, writes=W, semkey=semkey)

    def rsqrt_col(s, col, R_W, eps, mul=1.0):
        s.ts(col, col, mul, eps, ALU.mult, ALU.add, [R_W], [R_W])
        s.recip(col, col, [R_W], [R_W])
        s.act(col, col, AF.Sqrt, [R_W], [R_W])


def make_consts():
    i = np.arange(128)
    c = {}
    c["ident"] = np.eye(128, dtype=np.float32)
    c["tril_s"] = (i[:, None] > i[None, :]).astype(np.float32)
    c["tril_i"] = (i[:, None] >= i[None, :]).astype(np.float32)
    c["triu_s"] = (i[:, None] < i[None, :]).astype(np.float32)
    c["triu_i"] = (i[:, None] <= i[None, :]).astype(np.float32)
    c["ones"] = np.ones((128, 128), np.float32)
    c["md16"] = ((i[:, None] // 16) == (i[None, :] // 16)).astype(np.float32)
    for s_ in (16, 32, 64):
        bi, bj = i[:, None] // s_, i[None, :] // s_
        c["m%d" % s_] = ((bi % 2 == 1) & (bj == bi - 1)).astype(np.float32)
    c["bones"] = ((i[:, None] // 64) == (i[None, :] // 64)).astype(np.float32)
    hs = np.zeros((128, 128), np.float32); hs[:64, 0] = 1; hs[64:, 1] = 1
    c["headsel"] = hs
    c["iota_f"] = np.broadcast_to(i[None, :], (128, 128)).astype(np.float32).copy()
    c["iota_p"] = np.broadcast_to(i[:, None], (128, 128)).astype(np.float32).copy()
    return c


def declare_inputs(kb):
    c = kb.cfg
    D, T = c.D, c.T
    kb.din("x", [T, D])
    kb.din("cT", [128, c.KC])
    kb.din("w_ada", [D, 6 * D])
    kb.din("b_ada", [1, 6 * D])
    kb.din("n1g", [128, c.KC])
    kb.din("n2g", [1, D])
    kb.din("nfg", [1, D])
    kb.din("w_in", [D, c.INC])
    kb.din("convT", [128, c.CONVC // 128, 4])
    kb.din("a_log", [1, c.HG])
    kb.din("dt_bias", [1, c.HG])
    kb.din("onorm_g", [1, 128])
    for nm in ["mu_rkv"]:
        kb.din(nm, [128, 3 * c.NP])
    kb.din("mu_wd", [96, 1]); kb.din("mu_ad", [96, 1]); kb.din("mu_gd", [128, 2])
    for nm in ["w0", "a0", "k_k", "k_a", "ln_w", "ln_b", "r_k"]:
        kb.din(nm, [128, c.NP])
    kb.din("w0_row", [1, c.RW]); kb.din("lnw_row", [1, c.RW]); kb.din("lnb_row", [1, c.RW])
    kb.din("w_up", [96, c.RW]); kb.din("a_up", [96, c.RW]); kb.din("g_up", [256, c.RW])
    kb.din("w_gdn_o", [c.GW, D]); kb.din("w_rwkv_o", [c.RW, D]); kb.din("w_out", [D, D])
    kb.din("wr", [D, 72]); kb.din("br", [1, 72])
    kb.din("w1", [64 * 128, (D // 128) * c.DE]); kb.din("w3", [64 * 128, (D // 128) * c.DE])
    kb.din("w2", [64 * 128, c.FC * D])
    for k, v in make_consts().items():
        kb.din(k, v.shape)
    kb.din("tokiota", [128, c.TM // 128], I32)
    kb.dout("out", [c.TM, D])
    kb.dscr("mod_d", [128, 6 * D])
    kb.dscr("hT_d", [D, T], BF16)
    kb.dscr("oy_d", [D, T], BF16)
    kb.dscr("xmid_d", [c.TM, D])
    NWc = min(512, D)
    kb.dscr("wga_c", [c.KC, 128, c.KC * 128], BF16); kb.dscr("wgb_c", [c.KC, 128, c.KC * 128], BF16)
    kb.dscr("wgo_c", [c.KC, 128, (c.KC // 2) * 128], BF16); kb.dscr("wro_c", [c.KC, 128, (c.KC // 2) * 128], BF16)
    kb.dscr("wo_c", [D // NWc, 128, c.KC * NWc], BF16)
    kb.dscr("h2_d", [c.TM + 1, D], BF16)
    kb.dscr("yb_d", [c.NBLK * 128, D])
    kb.dscr("tokid_d", [c.NBLK * 128, 1], I32)


def host_inputs(cfg, inp, b, half=0):
    c = cfg
    D = c.D
    f = lambda a: np.ascontiguousarray(a, dtype=np.float32)
    fm = lambda v: f(np.asarray(v).reshape(-1, 128).T)
    m = {}
    m["x"] = f(inp["x"][b])
    m["cT"] = fm(inp["c"][b])
    m["w_ada"] = f(inp["w_ada"][0]); m["b_ada"] = f(inp["b_ada"][0][None, :])
    m["n1g"] = fm(inp["norm1_g"][0]); m["n2g"] = f(inp["norm2_g"][0][None, :]); m["nfg"] = f(inp["norm_f_g"][None, :])
    m["w_in"] = f(inp["w_in"][0])
    cw = np.asarray(inp["conv_w"][0])
    m["convT"] = f(cw.T.reshape(c.CONVC // 128, 128, 4).transpose(1, 0, 2))
    m["a_log"] = f(inp["gdn_a_log"][0][None, :]); m["dt_bias"] = f(inp["gdn_dt_bias"][0][None, :])
    m["onorm_g"] = f(inp["gdn_onorm_g"][0][None, :])
    mu = np.asarray(inp["rwkv_mu"][0])
    m["mu_rkv"] = fm(mu[:3 * c.RW])
    o = 3 * c.RW
    m["mu_wd"] = f(mu[o:o + 96][:, None]); m["mu_ad"] = f(mu[o + 96:o + 192][:, None]); m["mu_gd"] = fm(mu[o + 192:o + 448])
    m["w0"] = fm(inp["rwkv_w0"][0]); m["a0"] = fm(inp["rwkv_a0"][0]); m["k_k"] = fm(inp["rwkv_k_k"][0]); m["k_a"] = fm(inp["rwkv_k_a"][0])
    m["ln_w"] = fm(inp["rwkv_ln_w"][0]); m["ln_b"] = fm(inp["rwkv_ln_b"][0]); m["r_k"] = fm(np.asarray(inp["rwkv_r_k"][0]).reshape(-1))
    m["w0_row"] = f(inp["rwkv_w0"][0][None, :]); m["lnw_row"] = f(inp["rwkv_ln_w"][0][None, :]); m["lnb_row"] = f(inp["rwkv_ln_b"][0][None, :])
    m["w_up"] = f(inp["rwkv_w_up"][0]); m["a_up"] = f(inp["rwkv_a_up"][0]); m["g_up"] = f(inp["rwkv_g_up"][0])
    m["w_gdn_o"] = f(inp["w_gdn_o"][0]); m["w_rwkv_o"] = f(inp["w_rwkv_o"][0]); m["w_out"] = f(inp["w_out"][0])
    m["wr"] = f(np.concatenate([np.asarray(inp["w_group"][0]), np.asarray(inp["w_expert"][0])], axis=1))
    m["br"] = f(np.concatenate([np.asarray(inp["b_group"][0]), np.asarray(inp["b_expert"][0])])[None, :])
    m["w1"] = f(np.asarray(inp["w1"][0]).reshape(64 * 128, -1))
    m["w3"] = f(np.asarray(inp["w3"][0]).reshape(64 * 128, -1))
    m["w2"] = f(np.asarray(inp["w2"][0]).reshape(64 * 128, -1))
    m.update(make_consts())
    m["tokiota"] = (np.arange(c.TM // 128)[None, :] * 128 + np.arange(128)[:, None]).astype(np.int32)
    return m


def load_consts(kb):
    S = kb.S
    kb.C = {}
    for k in ["ident", "tril_s", "tril_i", "triu_s", "triu_i", "ones", "md16", "m16", "m32", "m64", "bones", "headsel", "iota_f", "iota_p"]:
        b = S.sb.alloc([128, 128], F32, k)
        kb.dma("sp", b[:], kb.d[k][:], [], [b])
        kb.C[k] = b
    for k in ["ident", "ones", "triu_i", "triu_s", "bones", "headsel"]:
        b = S.sb.alloc([128, 128], BF16, k + "_bf")
        kb.cp(b[:], kb.C[k][:], [kb.C[k]], [b], eng="pool")
        kb.C[k + "_bf"] = b


def phase_mod(kb):
    c, S, d = kb.cfg, kb.S, kb.d
    KC, D = c.KC, c.D
    S.sb.mark(); S.ps.mark()
    ct = S.sb.alloc([128, KC], F32, "ct")
    cs = S.sb.alloc([128, KC], F32, "cs")
    scr = S.sb.alloc([128, KC, 128], BF16, "scr")
    kb.dma("sp", ct[:], d["cT"][:], [], [ct])
    kb.act(cs[:], ct[:], AF.Silu, [ct], [cs])
    for kc in range(KC):
        kb.ts(scr[:, kc, :], kb.C["ones"][:], cs[:, kc:kc + 1], None, ALU.mult, None, [cs, kb.C["ones"]], [scr], eng="pool")
    MT = 512
    wa = [S.sb.alloc([128, KC, MT], BF16, "wa%d" % i) for i in range(2)]
    bb = [S.sb.alloc([128, MT], F32, "bb%d" % i) for i in range(2)]
    mo = [S.sb.alloc([128, MT], F32, "mo%d" % i) for i in range(2)]
    ps = [S.ps.alloc([128, MT], F32, "psm%d" % i) for i in range(2)]
    wsrc = d["w_ada"].rearrange("(kc p) n -> p kc n", p=128)
    LV = 9
    for m in range(6 * D // MT):
        i = m % 2
        kb.dma("pool", wa[i][:], wsrc[:, :, m * MT:(m + 1) * MT], [], [wa[i]])
        if LV < 2: continue
        kb.dma("sp", bb[i][:], d["b_ada"][0:1, m * MT:(m + 1) * MT].partition_broadcast(128), [], [bb[i]])
        if LV < 3: continue
        for kc in range(KC):
            kb.mm(ps[i][:], scr[:, kc, :], wa[i][:, kc, :], [scr, wa[i]], [ps[i]], start=(kc == 0), stop=(kc == KC - 1), inc=(kc == KC - 1))
        if LV < 4: continue
        kb.tt(mo[i][:], ps[i][:], bb[i][:], ALU.add, [ps[i], bb[i]], [mo[i]])
        if LV < 5: continue
        kb.dma("sp", d["mod_d"][:, m * MT:(m + 1) * MT], mo[i][:], [mo[i]], [kb.K_mod])
    S.sb.release(); S.ps.release()


def diag_extract(kb, dst, dstbuf, seg, tmpbig, tmp):
    c, d = kb.cfg, kb.d
    kb.dma("sp", tmpbig[:], d["mod_d"][:, seg * c.D:(seg + 1) * c.D], [kb.K_mod], [tmpbig])
    for kc in range(c.KC):
        kb.tt(tmp[:], tmpbig[:, kc * 128:(kc + 1) * 128], kb.C["ident"][:], ALU.mult, [tmpbig, kb.C["ident"]], [tmp])
        kb.red(dst[:, kc:kc + 1], tmp[:], ALU.add, [tmp], [dstbuf])


def phase_norm1(kb):
    c, S, d = kb.cfg, kb.S, kb.d
    KC, D, T, TT = c.KC, c.D, c.T, c.TT
    S.sb.mark(); S.ps.mark()
    A1 = S.sb.alloc([128, KC], F32, "A1"); B1 = S.sb.alloc([128, KC], F32, "B1"); g1n = S.sb.alloc([128, KC], F32, "g1n")
    S.sb.mark()
    big = S.sb.alloc([128, D], F32, "big"); tmp = S.sb.alloc([128, 128], F32, "tmp")
    diag_extract(kb, B1, B1, 0, big, tmp)
    diag_extract(kb, A1, A1, 1, big, tmp)
    kb.dma("sp", g1n[:], d["n1g"][:], [], [g1n])
    kb.stt(A1[:], A1[:], 1.0, g1n[:], ALU.add, ALU.mult, [A1, g1n], [A1])
    S.sb.release()
    xt = [S.sb.alloc([128, D], F32, "xt%d" % i) for i in range(2)]
    junk = S.sb.alloc([128, D], BF16, "junk")
    xn = [S.sb.alloc([128, D], BF16, "xn%d" % i) for i in range(2)]
    st = [S.sb.alloc([128, 2], F32, "st%d" % i) for i in range(2)]
    hTt = [S.sb.alloc([128, KC, TT], BF16, "hTt%d" % i) for i in range(2)]
    pst = [S.ps.alloc([128, 4, 128], BF16, "pst%d" % i) for i in range(4)]
    assert len({p.lo // 2048 for p in pst}) == 4
    hdst = d["hT_d"].rearrange("(kc p) t -> p kc t", p=128)
    G = min(4, KC)
    pi = 0
    for n in range(T // 128):
        i = n % 2
        hb = hTt[(n * 128 // TT) % 2]
        toff = (n * 128) % TT
        kb.dma("sp", xt[i][:], d["x"][n * 128:(n + 1) * 128, :], [], [xt[i]])
        kb.act(junk[:], xt[i][:], AF.Square, [xt[i]], [junk, st[i]], accum=st[i][:, 0:1])
        kb.rsqrt_col(st[i][:, 0:1], st[i], 1e-6, 1.0 / D)
        kb.ts(xn[i][:], xt[i][:], st[i][:, 0:1], None, ALU.mult, None, [xt[i], st[i]], [xn[i]])
        for g in range(KC // G):
            p = pst[pi % 4]; pi += 1
            for j in range(G):
                kc = g * G + j
                kb.tr(p[:, j, :], xn[i][:, kc * 128:(kc + 1) * 128], kb.C["ident_bf"][:], [xn[i], kb.C["ident_bf"]], [p], inc=(j == G - 1))
            for j in range(G):
                kc = g * G + j
                kb.act(hb[:, kc, toff:toff + 128], p[:, j, :], AF.Identity, [p, A1, B1], [hb], scale=A1[:, kc:kc + 1], bias=B1[:, kc:kc + 1],
                       eng="act")
        if toff + 128 == TT:
            t0 = n * 128 + 128 - TT
            kb.dma("sp", hdst[:, :, t0:t0 + TT], hb[:], [hb], [kb.K_hT])
    S.sb.release(); S.ps.release()


class Rot:
    def __init__(self, bufs):
        self.bufs, self.i = bufs, 0

    def get(self):
        b = self.bufs[self.i % len(self.bufs)]
        self.i += 1
        return b


def neumann(kb, grp, pt, NR=3):
    C = kb.C
    for m in grp:
        kb.tt(m["L2"][:], m["L"][:], C["md16"][:], ALU.mult, [m["L"], C["md16"]], [m["L2"]], eng="pool")
        kb.tt(m["U2"][:], m["U"][:], C["md16"][:], ALU.mult, [m["U"], C["md16"]], [m["U2"]], eng="pool")
        kb.tt(m["Y"][:], C["ident"][:], m["U2"][:], ALU.subtract, [C["ident"], m["U2"]], [m["Y"]], eng="pool")
    for r in range(NR):
        for m in grp:
            Lk, Uk = (m["L2"], m["U2"]) if r % 2 == 0 else (m["D"], m["DT"])
            Ln, Un = (m["D"], m["DT"]) if r % 2 == 0 else (m["L2"], m["U2"])
            p1 = pt.get()
            kb.mm(p1[:], Uk[:], Lk[:], [Uk, Lk], [p1])
            kb.cp(Ln[:], p1[:], [p1], [Ln], eng="act")
            if r < NR - 1:
                p2 = pt.get()
                kb.mm(p2[:], Lk[:], Uk[:], [Lk, Uk], [p2])
                kb.cp(Un[:], p2[:], [p2], [Un], eng="dve")
        for m in grp:
            Ln = m["D"] if r % 2 == 0 else m["L2"]
            p3 = pt.get()
            kb.mm(p3[:], Ln[:], m["Y"][:], [Ln, m["Y"]], [p3])
            kb.tt(m["Y"][:], m["Y"][:], p3[:], ALU.add, [m["Y"], p3], [m["Y"]])
    for s_ in (16, 32, 64):
        msk = C["m%d" % s_]
        for m in grp:
            kb.tt(m["L2"][:], m["L"][:], msk[:], ALU.mult, [m["L"], msk], [m["L2"]], eng="pool")
            p = pt.get()
            kb.tr(p[:], m["Y"][:], C["ident"][:], [m["Y"], C["ident"]], [p])
            kb.cp(m["D"][:], p[:], [p], [m["D"]], eng="act")
            p = pt.get()
            kb.mm(p[:], m["L2"][:], m["Y"][:], [m["L2"], m["Y"]], [p])
            kb.cp(m["DT"][:], p[:], [p], [m["DT"]], eng="dve")
        for m in grp:
            p = pt.get()
            kb.mm(p[:], m["D"][:], m["DT"][:], [m["D"], m["DT"]], [p])
            kb.tt(m["Y"][:], m["Y"][:], p[:], ALU.subtract, [m["Y"], p], [m["Y"]])


def phase_gdn_pre(kb, heads):
    c, S, d, C = kb.cfg, kb.S, kb.d, kb.C
    KC, T, TT, HG, NT = c.KC, c.T, c.TT, c.HG, c.NT
    P = {}
    for nm in ["g", "beta", "gc", "egc", "bege", "kdec", "egl"]:
        P[nm] = S.sb.alloc([128, HG, NT], F32, "gp_" + nm)
    S.sb.mark(); S.ps.mark()
    ab = S.sb.alloc([128, 2 * HG, NT], F32, "ab")
    wab = S.sb.alloc([128, KC, 2 * HG], BF16, "wab")
    hT = [S.sb.alloc([128, KC, TT], BF16, "hTs%d" % i) for i in range(2)]
    cb = S.sb.alloc([128, 3, HG], F32, "cb")
    pab = [S.ps.alloc([128, 2 * HG], F32, "pab%d" % i) for i in range(2)]
    pbig = S.ps.alloc([128, 512], F32, "pbig")
    kb.dma("pool", wab[:], d["w_in"].rearrange("(kc p) n -> p kc n", p=128)[:, :, c.OFF_A:c.OFF_A + 2 * HG], [], [wab])
    kb.dma("sp", cb[:, 0, :], d["dt_bias"][0:1, :].partition_broadcast(128), [], [cb])
    kb.dma("sp", cb[:, 1, :], d["a_log"][0:1, :].partition_broadcast(128), [], [cb])
    kb.act(cb[:, 2, :], cb[:, 1, :], AF.Exp, [cb], [cb])
    kb.ts(cb[:, 2, :], cb[:, 2, :], -1.0, None, ALU.mult, None, [cb], [cb])
    hsrc = d["hT_d"].rearrange("(kc p) t -> p kc t", p=128)
    for tt in range(T // TT):
        hb = hT[tt % 2]
        kb.dma("sp", hb[:], hsrc[:, :, tt * TT:(tt + 1) * TT], [kb.K_hT], [hb])
        for j in range(TT // 128):
            n = tt * (TT // 128) + j
            p = pab[n % 2]
            for kc in range(KC):
                kb.mm(p[:], hb[:, kc, j * 128:(j + 1) * 128], wab[:, kc, :], [hb, wab], [p], start=(kc == 0), stop=(kc == KC - 1), inc=(kc == KC - 1))
            kb.cp(ab[:, :, n], p[:], [p], [ab], eng="act")
    g, beta = P["g"], P["beta"]
    for h in range(HG):
        kb.act(g[:, h, :], ab[:, h, :], AF.Exp, [ab, cb], [g], bias=cb[:, 0, h:h + 1])
        kb.act(g[:, h, :], g[:, h, :], AF.Ln, [g], [g], bias=1.0)
        kb.ts(g[:, h, :], g[:, h, :], cb[:, 2, h:h + 1], None, ALU.mult, None, [g, cb], [g])
    kb.act(beta[:].rearrange("p h n -> p (h n)"), ab[:, HG:2 * HG, :].rearrange("p h n -> p (h n)"), AF.Sigmoid, [ab], [beta])
    N = HG * NT
    assert N <= 512
    gf = lambda b: b[:].rearrange("p h n -> p (h n)")
    kb.mm(pbig[:, 0:N], C["triu_i"][:], gf(g), [C["triu_i"], g], [pbig])
    kb.cp(gf(P["gc"]), pbig[:, 0:N], [pbig], [P["gc"]], eng="act")
    kb.mm(pbig[:, 0:N], C["ones"][:], gf(g), [C["ones"], g], [pbig])
    kb.act(gf(P["egl"]), pbig[:, 0:N], AF.Exp, [pbig], [P["egl"]])
    kb.tt(gf(P["kdec"]), pbig[:, 0:N], gf(P["gc"]), ALU.subtract, [pbig, P["gc"]], [P["kdec"]])
    kb.act(gf(P["kdec"]), gf(P["kdec"]), AF.Exp, [P["kdec"]], [P["kdec"]])
    kb.act(gf(P["egc"]), gf(P["gc"]), AF.Exp, [P["gc"]], [P["egc"]])
    kb.tt(gf(P["bege"]), gf(P["egc"]), gf(beta), ALU.mult, [P["egc"], beta], [P["bege"]])
    S.sb.release(); S.ps.release()
    return P


def inproj_groups(kb, cols, hT, wbufs, emit_evac, pbanks):
    c, d = kb.cfg, kb.d
    KC, T, TT = c.KC, c.T, c.TT
    wsrc = d["w_in"].rearrange("(kc p) n -> p kc n", p=128)
    for gi, co in enumerate(cols):
        kb.dma("pool", wbufs[gi][:], wsrc[:, :, co:co + 128], [], [wbufs[gi]])
    hsrc = d["hT_d"].rearrange("(kc p) t -> p kc t", p=128)
    pi = 0
    for tt in range(T // TT):
        hb = hT[tt % 2]
        kb.dma("sp", hb[:], hsrc[:, :, tt * TT:(tt + 1) * TT], [kb.K_hT], [hb])
        for gi in range(len(cols)):
            p = pbanks[pi % len(pbanks)]; pi += 1
            for kc in range(KC):
                kb.mm(p[:, 0:TT], wbufs[gi][:, kc, :], hb[:, kc, :], [wbufs[gi], hb], [p], start=(kc == 0), stop=(kc == KC - 1), inc=(kc == KC - 1))
            emit_evac(gi, tt, p)


def phase_gdn(kb, heads, P):
    c, S, d, C = kb.cfg, kb.S, kb.d, kb.C
    KC, T, TT, HG, NT = c.KC, c.T, c.TT, c.HG, c.NT
    S.sb.mark(); S.ps.mark()
    gon = S.sb.alloc([128, 128], F32, "gon")
    kb.dma("sp", gon[:], d["onorm_g"][0:1, :].partition_broadcast(128), [], [gon])
    qf = S.sb.alloc([128, T], BF16, "qf"); kf = S.sb.alloc([128, T], BF16, "kf")
    vf = S.sb.alloc([128, T], BF16, "vf"); gzf = S.sb.alloc([128, T], BF16, "gzf")
    of = S.sb.alloc([128, T], BF16, "of")
    Sst = S.sb.alloc([128, 128], F32, "Sst"); Sbf = S.sb.alloc([128, 128], BF16, "Sbf")
    for hi, h in enumerate(heads):
        S.sb.mark(); S.ps.mark()
        wb = [S.sb.alloc([128, KC, 128], BF16, "wg%d" % i) for i in range(4)]
        hT = [S.sb.alloc([128, KC, TT], BF16, "hTg%d" % i) for i in range(2)]
        zc = [S.sb.alloc([128, 3 + TT], F32, "zc%d" % i) for i in range(3)]
        cw = S.sb.alloc([128, 3, 4], F32, "cw")
        acc = [S.sb.alloc([128, TT], F32, "acc%d" % i) for i in range(2)]
        sq = S.sb.alloc([128, TT], BF16, "sq"); rn = S.sb.alloc([128, TT], F32, "rn")
        pb = [S.ps.alloc([128, 512], F32, "pg%d" % i) for i in range(5)]
        pn = [S.ps.alloc([128, 512], F32, "pn%d" % i) for i in range(2)]
        cols = [h * 128, c.GW + h * 128, 2 * c.GW + h * 128, c.OFF_Z + h * 128]
        for i in range(3):
            kb.dma("sp", cw[:, i, :], d["convT"][:, cols[i] // 128, :], [], [cw])
            kb.memset(zc[i][:, 0:3], 0.0, [zc[i]])
        dst = [qf, kf, vf]
        scale_q = 128.0 ** -0.5

        def evac(gi, tt, p):
            t0 = tt * TT
            if gi == 3:
                kb.act(gzf[:, t0:t0 + TT], p[:, 0:TT], AF.Silu, [p], [gzf])
                return
            z = zc[gi]
            kb.cp(z[:, 3:3 + TT], p[:, 0:TT], [p], [z], eng="act")
            a = acc[gi % 2]
            kb.ts(a[:], z[:, 3:3 + TT], cw[:, gi, 3:4], None, ALU.mult, None, [z, cw], [a])
            for i in range(3):
                kb.stt(a[:], z[:, i:i + TT], cw[:, gi, i:i + 1], a[:], ALU.mult, ALU.add, [z, cw, a], [a])
            kb.cp(z[:, 0:3], z[:, TT:TT + 3], [z], [z], eng="pool")
            if gi == 2:
                kb.act(vf[:, t0:t0 + TT], a[:], AF.Silu, [a], [vf])
                return
            kb.act(a[:], a[:], AF.Silu, [a], [a])
            kb.act(sq[:], a[:], AF.Square, [a], [sq])
            pp = pn[gi % 2]
            kb.mm(pp[:, 0:TT], C["ones_bf"][:], sq[:], [C["ones_bf"], sq], [pp])
            kb.ts(rn[:], pp[:, 0:TT], 1.0, 1e-6, ALU.mult, ALU.add, [pp], [rn])
            kb.recip(rn[:], rn[:], [rn], [rn])
            kb.act(rn[:], rn[:], AF.Sqrt, [rn], [rn])
            if gi == 0:
                kb.stt(dst[gi][:, t0:t0 + TT], a[:], scale_q, rn[:], ALU.mult, ALU.mult, [a, rn], [dst[gi]])
            else:
                kb.tt(dst[gi][:, t0:t0 + TT], a[:], rn[:], ALU.mult, [a, rn], [dst[gi]])
        GL = 9
        if GL >= 1: inproj_groups(kb, cols, hT, wb, evac, pb)
        S.sb.release(); S.ps.release()
        if GL < 2: return
        S.sb.mark(); S.ps.mark()
        ktm = S.sb.alloc([128, NT, 128], BF16, "ktm"); vbt = S.sb.alloc([128, NT, 128], BF16, "vbt")
        kbe = S.sb.alloc([128, NT, 128], BF16, "kbe")
        wT = S.sb.alloc([128, NT, 128], BF16, "wT"); u = S.sb.alloc([128, NT, 128], F32, "u")
        aT = S.sb.alloc([128, NT, 128], BF16, "aT")
        G = min(8, NT)
        grp = []
        for i in range(G):
            m = {k: S.sb.alloc([128, 128], F32, "%s%d" % (k, i)) for k in ["L", "U", "Y", "L2", "U2", "gb", "D", "DT"]}
            m["Ybf"] = S.sb.alloc([128, 128], BF16, "Ybf%d" % i)
            grp.append(m)
        pt = Rot([S.ps.alloc([128, 128], F32, "pt%d" % i) for i in range(6)])
        ptb = Rot([S.ps.alloc([128, 128], BF16, "ptb%d" % i) for i in range(2)])
        gcol = lambda nm, n: P[nm][:, h, n:n + 1]
        for n0 in range(0, NT, G):
            ns = list(range(n0, min(NT, n0 + G)))
            for i, n in enumerate(ns):
                m = grp[i]
                cs = slice(n * 128, (n + 1) * 128)
                p = ptb.get()
                kb.tr(p[:], kf[:, cs], C["ident_bf"][:], [kf, C["ident_bf"]], [p])
                kb.cp(ktm[:, n, :], p[:], [p], [ktm], eng="act")
                kb.ts(kbe[:, n, :], p[:], gcol("bege", n), None, ALU.mult, None, [p, P["bege"]], [kbe])
                p = ptb.get()
                kb.tr(p[:], vf[:, cs], C["ident_bf"][:], [vf, C["ident_bf"]], [p])
                kb.ts(vbt[:, n, :], p[:], gcol("beta", n), None, ALU.mult, None, [p, P["beta"]], [vbt])
                kb.ts(m["gb"][:], C["ones"][:], gcol("g", n), None, ALU.mult, None, [C["ones"], P["g"]], [m["gb"]], eng="pool")
                p = pt.get()
                kb.mm(p[:], m["gb"][:], C["triu_i"][:], [m["gb"], C["triu_i"]], [p])
                kb.ts(m["D"][:], p[:], gcol("gc", n), 0.0, ALU.subtract, ALU.max, [p, P["gc"]], [m["D"]])
                kb.ts(m["DT"][:], p[:], gcol("gc", n), 0.0, ALU.subtract, ALU.min, [p, P["gc"]], [m["DT"]])
                kb.act(m["D"][:], m["D"][:], AF.Exp, [m["D"]], [m["D"]], scale=-1.0)
                kb.act(m["DT"][:], m["DT"][:], AF.Exp, [m["DT"]], [m["DT"]])
                kb.tt(m["D"][:], m["D"][:], C["tril_s"][:], ALU.mult, [m["D"], C["tril_s"]], [m["D"]], eng="pool")
                kb.tt(m["DT"][:], m["DT"][:], C["triu_i"][:], ALU.mult, [m["DT"], C["triu_i"]], [m["DT"]], eng="pool")
                p = pt.get()
                kb.mm(p[:], kf[:, cs], kf[:, cs], [kf], [p])
                kb.stt(m["L"][:], p[:], gcol("beta", n), m["D"][:], ALU.mult, ALU.mult, [p, P["beta"], m["D"]], [m["L"]])
                p = pt.get()
                kb.tr(p[:], m["L"][:], C["ident"][:], [m["L"], C["ident"]], [p])
                kb.cp(m["U"][:], p[:], [p], [m["U"]], eng="act")
                p = pt.get()
                kb.mm(p[:], kf[:, cs], qf[:, cs], [kf, qf], [p])
                kb.tt(aT[:, n, :], p[:], m["DT"][:], ALU.mult, [p, m["DT"]], [aT])
            if GL < 3: continue
            neumann(kb, grp[:len(ns)], pt)
            if GL < 4: continue
            for i, n in enumerate(ns):
                m = grp[i]
                kb.cp(m["Ybf"][:], m["Y"][:], [m["Y"]], [m["Ybf"]], eng="pool")
                p = pt.get()
                kb.mm(p[:], kbe[:, n, :], m["Ybf"][:], [kbe, m["Ybf"]], [p])
                kb.cp(wT[:, n, :], p[:], [p], [wT], eng="act")
                p = pt.get()
                kb.mm(p[:], m["Ybf"][:], vbt[:, n, :], [m["Ybf"], vbt], [p])
                kb.cp(u[:, n, :], p[:], [p], [u], eng="dve")
        if GL < 5: return
        vn = S.sb.alloc([128, 128], F32, "vn"); vnb = S.sb.alloc([128, 128], BF16, "vnb"); vns = S.sb.alloc([128, 128], BF16, "vns")
        o1 = S.sb.alloc([128, 128], F32, "o1"); o = S.sb.alloc([128, 128], F32, "o"); onb = S.sb.alloc([128, 128], BF16, "onb")
        junk = S.sb.alloc([128, 128], BF16, "junkg"); st = S.sb.alloc([128, 2], F32, "stg")
        kb.memset(Sst[:], 0.0, [Sst]); kb.memset(Sbf[:], 0.0, [Sbf])
        for n in range(NT):
            cs = slice(n * 128, (n + 1) * 128)
            pw = pt.get(); pq = pt.get()
            kb.mm(pw[:], wT[:, n, :], Sbf[:], [wT, Sbf], [pw])
            kb.mm(pq[:], qf[:, cs], Sbf[:], [qf, Sbf], [pq])
            kb.tt(vn[:], u[:, n, :], pw[:], ALU.subtract, [u, pw], [vn])
            kb.cp(vnb[:], vn[:], [vn], [vnb], eng="act")
            kb.ts(vns[:], vn[:], gcol("kdec", n), None, ALU.mult, None, [vn, P["kdec"]], [vns])
            pa = pt.get(); psu = pt.get()
            kb.mm(pa[:], aT[:, n, :], vnb[:], [aT, vnb], [pa])
            kb.mm(psu[:], ktm[:, n, :], vns[:], [ktm, vns], [psu])
            kb.stt(Sst[:], Sst[:], gcol("egl", n), psu[:], ALU.mult, ALU.add, [Sst, P["egl"], psu], [Sst])
            kb.cp(Sbf[:], Sst[:], [Sst], [Sbf], eng="act")
            kb.cp(o1[:], pa[:], [pa], [o1], eng="act")
            kb.stt(o[:], pq[:], gcol("egc", n), o1[:], ALU.mult, ALU.add, [pq, P["egc"], o1], [o])
            kb.act(junk[:], o[:], AF.Square, [o], [junk, st], accum=st[:, 0:1])
            kb.rsqrt_col(st[:, 0:1], st, 1e-6, 1.0 / 128)
            kb.stt(onb[:], o[:], st[:, 0:1], gon[:], ALU.mult, ALU.mult, [o, st, gon], [onb])
            p = ptb.get()
            kb.tr(p[:], onb[:], C["ident_bf"][:], [onb, C["ident_bf"]], [p])
            kb.tt(of[:, cs], p[:], gzf[:, cs], ALU.mult, [p, gzf], [of])
        kb.dma("sp", d["oy_d"][h * 128:(h + 1) * 128, :], of[:], [of], [kb.K_oy])
        S.sb.release(); S.ps.release()
    S.sb.release(); S.ps.release()


def phase_rwkv_pre(kb):
    c, S, d, C = kb.cfg, kb.S, kb.d, kb.C
    KC, T, TT = c.KC, c.T, c.TT
    L = {}
    L["twd"] = S.sb.alloc([128, T], BF16, "twd"); L["ads"] = S.sb.alloc([128, T], BF16, "ads")
    L["sgd"] = S.sb.alloc([128, 2, T], BF16, "sgd")
    S.sb.mark(); S.ps.mark()
    base = c.OFF_R + 3 * c.RW
    groups = [(base, 96), (base + 96, 96), (base + 192, 128), (base + 320, 128)]
    wb = [S.sb.alloc([128, KC, 128], BF16, "wl%d" % i) for i in range(4)]
    hT = [S.sb.alloc([128, KC, TT], BF16, "hTl%d" % i) for i in range(2)]
    zr = [S.sb.alloc([128, 1 + TT], F32, "zl%d" % i) for i in range(4)]
    tmp = S.sb.alloc([128, TT], F32, "tl")
    mu = S.sb.alloc([128, 4], F32, "mul")
    pb = [S.ps.alloc([128, 512], F32, "pl%d" % i) for i in range(4)]
    kb.dma("sp", mu[0:96, 0:1], d["mu_wd"][:, :], [], [mu]); kb.dma("sp", mu[0:96, 1:2], d["mu_ad"][:, :], [], [mu])
    kb.dma("sp", mu[:, 2:4], d["mu_gd"][:, :], [], [mu])
    wsrc = d["w_in"].rearrange("(kc p) n -> p kc n", p=128)
    for gi, (co, w) in enumerate(groups):
        kb.dma("pool", wb[gi][:, :, 0:w], wsrc[:, :, co:co + w], [], [wb[gi]])
        kb.memset(zr[gi][:, 0:1], 0.0, [zr[gi]])
    hsrc = d["hT_d"].rearrange("(kc p) t -> p kc t", p=128)
    for tt in range(T // TT):
        hb = hT[tt % 2]
        t0 = tt * TT
        kb.dma("sp", hb[:], hsrc[:, :, t0:t0 + TT], [kb.K_hT], [hb])
        for gi, (co, w) in enumerate(groups):
            p = pb[gi]
            for kc in range(KC):
                kb.mm(p[0:w, 0:TT], wb[gi][:, kc, 0:w], hb[:, kc, :], [wb[gi], hb], [p], start=(kc == 0), stop=(kc == KC - 1), inc=(kc == KC - 1))
            z = zr[gi]
            kb.cp(z[0:w, 1:1 + TT], p[0:w, 0:TT], [p], [z], eng="act")
            kb.tt(tmp[0:w, :], z[0:w, 0:TT], z[0:w, 1:1 + TT], ALU.subtract, [z], [tmp])
            kb.stt(tmp[0:w, :], tmp[0:w, :], mu[0:w, gi:gi + 1], z[0:w, 1:1 + TT], ALU.mult, ALU.add, [tmp, mu, z], [tmp])
            kb.cp(z[0:w, 0:1], z[0:w, TT:TT + 1], [z], [z], eng="pool")
            if gi == 0:
                kb.act(L["twd"][0:96, t0:t0 + TT], tmp[0:96, :], AF.Tanh, [tmp], [L["twd"]])
            elif gi == 1:
                kb.cp(L["ads"][0:96, t0:t0 + TT], tmp[0:96, :], [tmp], [L["ads"]], eng="act")
            else:
                kb.act(L["sgd"][:, gi - 2, t0:t0 + TT], tmp[:], AF.Sigmoid, [tmp], [L["sgd"]])
    S.sb.release(); S.ps.release()
    return L


def phase_rwkv(kb, pairs, L):
    c, S, d, C = kb.cfg, kb.S, kb.d, kb.C
    KC, T, TT, NT, RW = c.KC, c.T, c.TT, c.NT, c.RW
    S.sb.mark(); S.ps.mark()
    rf = S.sb.alloc([128, T], F32, "rf"); kf = S.sb.alloc([128, T], F32, "kfr"); vf = S.sb.alloc([128, T], F32, "vfr")
    yf = S.sb.alloc([128, T], BF16, "yf")
    prmall = S.sb.alloc([128, 8, c.NP], F32, "prmall")
    for j, nm in enumerate(["w0", "a0", "k_k", "k_a", "r_k"]):
        kb.dma("sp", prmall[:, j, :], d[nm][:, :], [], [prmall])
    kb.dma("sp", prmall[:, 5:8, :], d["mu_rkv"].rearrange("p (j n) -> p j n", j=3), [], [prmall])
    prm_ = prmall
    bc = S.sb.alloc([128, 3, 128], F32, "bcr")
    wup = S.sb.alloc([128, 128], BF16, "wup"); aup = S.sb.alloc([128, 128], BF16, "aup"); gup = S.sb.alloc([128, 2, 128], BF16, "gup")
    H = S.sb.alloc([128, 64], F32, "Hst"); Hbf = S.sb.alloc([128, 64], BF16, "Hbf")
    for pr in pairs:
        ch0 = pr * 128
        class _P:
            def __getitem__(self, k):
                rows, cols = k
                return prmall[rows, cols.start, pr:pr + 1]
        prm = _P()
        for j, nm in enumerate(["w0_row", "lnw_row", "lnb_row"]):
            kb.dma("sp", bc[:, j, :], d[nm][0:1, ch0:ch0 + 128].partition_broadcast(128), [], [bc])
        kb.dma("pool", wup[0:96, :], d["w_up"][:, ch0:ch0 + 128], [], [wup])
        kb.dma("pool", aup[0:96, :], d["a_up"][:, ch0:ch0 + 128], [], [aup])
        kb.dma("pool", gup[:], d["g_up"].rearrange("(kc p) n -> p kc n", p=128)[:, :, ch0:ch0 + 128], [], [gup])
        S.sb.mark(); S.ps.mark()
        wb = [S.sb.alloc([128, KC, 128], BF16, "wr%d" % i) for i in range(3)]
        _h = S.sb.alloc([128, KC, TT], BF16, "hTr0")
        hT = [_h, _h]
        zr = [S.sb.alloc([128, 1 + TT], F32, "zr%d" % i) for i in range(3)]
        tmp = S.sb.alloc([128, TT], F32, "tr")
        pb = [S.ps.alloc([128, 512], F32, "pr%d" % i) for i in range(6)]
        cols = [c.OFF_R + j * RW + ch0 for j in range(3)]
        for i in range(3):
            kb.memset(zr[i][:, 0:1], 0.0, [zr[i]])
        dst = [rf, kf, vf]

        def evac(gi, tt, p):
            t0 = tt * TT
            z = zr[gi]
            kb.cp(z[:, 1:1 + TT], p[:, 0:TT], [p], [z], eng="act")
            kb.tt(tmp[:], z[:, 0:TT], z[:, 1:1 + TT], ALU.subtract, [z], [tmp])
            kb.stt(dst[gi][:, t0:t0 + TT], tmp[:], prm[:, 5 + gi:6 + gi], z[:, 1:1 + TT], ALU.mult, ALU.add, [tmp, prmall, z], [dst[gi]])
            kb.cp(z[:, 0:1], z[:, TT:TT + 1], [z], [z], eng="pool")
        inproj_groups(kb, cols, hT, wb, evac, pb)
        S.sb.release(); S.ps.release()
        S.sb.mark(); S.ps.mark()
        fA = {k: S.sb.alloc([128, TT], F32, "f_" + k) for k in ["alr", "G", "Gx", "t1", "t2", "kk", "ke"]}
        bA = {k: S.sb.alloc([128, TT], BF16, "b_" + k) for k in ["Rh", "Ah", "Kh", "Bh", "Kt", "Bt", "Pb", "vb"]}
        lwt = S.sb.alloc([128, 128], F32, "lwt")
        CPT = TT // 128
        members = []
        for j_ in range(CPT):
            row = []
            for i in range(2):
                m = {k: S.sb.alloc([128, 128], F32, "r%s%d_%d" % (k, i, j_)) for k in ["L", "U", "Y", "L2", "U2", "D", "DT"]}
                for k in ["Ybf", "AakT", "ArbT", "ArkT"]:
                    m[k] = S.sb.alloc([128, 128], BF16, "r%s%d_%d" % (k, i, j_))
                m["AKV"] = S.sb.alloc([128, 64], BF16, "rAKV%d_%d" % (i, j_)); m["U2v"] = S.sb.alloc([128, 64], F32, "rU2v%d_%d" % (i, j_))
                m["Ub"] = S.sb.alloc([128, 64], BF16, "rUb%d_%d" % (i, j_))
                row.append(m)
            members.append(row)
        VtmL = [S.sb.alloc([128, 128], BF16, "Vtm%d" % j_) for j_ in range(CPT)]
        AtmL = [S.sb.alloc([128, 128], BF16, "Atm%d" % j_) for j_ in range(CPT)]
        BttmL = [S.sb.alloc([128, 128], BF16, "Bttm%d" % j_) for j_ in range(CPT)]
        KttmL = [S.sb.alloc([128, 128], BF16, "Kttm%d" % j_) for j_ in range(CPT)]
        WTL = [S.sb.alloc([128, 128], BF16, "WTr%d" % j_) for j_ in range(CPT)]
        Yp = S.sb.alloc([128, 128], F32, "Yp"); Yo = S.sb.alloc([128, 128], BF16, "Yo")
        st = S.sb.alloc([128, 8], F32, "str"); junk = S.sb.alloc([128, 128], BF16, "junkr")
        eGl = S.sb.alloc([128, NT], F32, "eGl")
        pt = Rot([S.ps.alloc([128, 128], F32, "qt%d" % i) for i in range(6)])
        ptb = Rot([S.ps.alloc([128, 128], BF16, "qtb%d" % i) for i in range(2)])
        kb.memset(H[:], 0.0, [H]); kb.memset(Hbf[:], 0.0, [Hbf])
        CPT = TT // 128
        for tt in range(T // TT):
            t0 = tt * TT
            ts_ = slice(t0, t0 + TT)
            p = pt.get()
            pbig = p
            for j in range(CPT):
                cs = slice(t0 + j * 128, t0 + (j + 1) * 128)
                js = slice(j * 128, (j + 1) * 128)
                n = tt * CPT + j
                p = pt.get()
                kb.mm(p[:], aup[0:96, :], L["ads"][0:96, cs], [aup, L["ads"]], [p])
                kb.act(fA["alr"][:, js], p[:], AF.Sigmoid, [p, prmall], [fA["alr"]], bias=prm[:, 1:2])
                p = pt.get()
                kb.mm(p[:], L["twd"][0:96, cs], wup[0:96, :], [L["twd"], wup], [p])
                kb.tt(lwt[:], p[:], bc[:, 0, :], ALU.add, [p, bc], [lwt])
                kb.act(lwt[:], lwt[:], AF.Sigmoid, [lwt], [lwt])
                kb.ts(lwt[:], lwt[:], -0.6065306597126334, None, ALU.mult, None, [lwt], [lwt])
                p = pt.get()
                kb.mm(p[:], lwt[:], C["triu_i"][:], [lwt, C["triu_i"]], [p])
                kb.cp(fA["G"][:, js], p[:], [p], [fA["G"]], eng="act")
                p = pt.get()
                kb.mm(p[:], lwt[:], C["triu_s"][:], [lwt, C["triu_s"]], [p])
                kb.cp(fA["Gx"][:, js], p[:], [p], [fA["Gx"]], eng="act")
                kb.act(fA["t2"][:, js], fA["G"][:, js], AF.Exp, [fA["G"]], [fA["t2"]], scale=-1.0, bias=fA["G"][:, j * 128 + 127:j * 128 + 128])
                kb.act(eGl[:, n:n + 1], fA["G"][:, j * 128 + 127:j * 128 + 128], AF.Exp, [fA["G"]], [eGl])
            alr, G, Gx, t1, t2, kk, ke = [fA[k] for k in ["alr", "G", "Gx", "t1", "t2", "kk", "ke"]]
            kb.ts(kk[:], kf[:, ts_], prm[:, 2:3], None, ALU.mult, None, [kf, prmall], [kk])
            kb.act(bA["Pb"][:], kk[:], AF.Square, [kk], [bA["Pb"]])
            for j in range(CPT):
                js = slice(j * 128, (j + 1) * 128)
                p = pt.get()
                kb.mm(p[:], C["bones_bf"][:], bA["Pb"][:, js], [C["bones_bf"], bA["Pb"]], [p])
                kb.ts(t1[:, js], p[:], 1.0, 1e-6, ALU.mult, ALU.add, [p], [t1])
            kb.recip(t1[:], t1[:], [t1], [t1])
            kb.act(t1[:], t1[:], AF.Sqrt, [t1], [t1])
            kb.tt(kk[:], kk[:], t1[:], ALU.mult, [kk, t1], [kk])
            kb.ts(ke[:], alr[:], -1.0, prm[:, 3:4], ALU.add, ALU.mult, [alr, prmall], [ke])
            kb.stt(ke[:], ke[:], 1.0, kf[:, ts_], ALU.add, ALU.mult, [ke, kf], [ke])
            kb.tt(bA["Kt"][:], ke[:], t2[:], ALU.mult, [ke, t2], [bA["Kt"]])
            kb.tt(t1[:], kk[:], alr[:], ALU.mult, [kk, alr], [t1])
            kb.tt(bA["Bt"][:], t1[:], t2[:], ALU.mult, [t1, t2], [bA["Bt"]])
            kb.stt(bA["Pb"][:], rf[:, ts_], prm[:, 4:5], ke[:], ALU.mult, ALU.mult, [rf, prmall, ke], [bA["Pb"]])
            kb.act(t2[:], G[:], AF.Exp, [G], [t2], scale=-1.0)
            kb.tt(bA["Kh"][:], ke[:], t2[:], ALU.mult, [ke, t2], [bA["Kh"]])
            kb.tt(bA["Bh"][:], t1[:], t2[:], ALU.mult, [t1, t2], [bA["Bh"]])
            kb.act(t2[:], G[:], AF.Exp, [G], [t2])
            kb.tt(bA["Rh"][:], rf[:, ts_], t2[:], ALU.mult, [rf, t2], [bA["Rh"]])
            kb.act(t2[:], Gx[:], AF.Exp, [Gx], [t2])
            kb.stt(bA["Ah"][:], kk[:], -1.0, t2[:], ALU.mult, ALU.mult, [kk, t2], [bA["Ah"]])
            kb.cp(bA["vb"][:], vf[:, ts_], [vf], [bA["vb"]], eng="pool")
            for j in range(CPT):
                n = tt * CPT + j
                js = slice(j * 128, (j + 1) * 128)
                cs = slice(t0 + j * 128, t0 + (j + 1) * 128)
                Rh, Ah, Kh, Bh, Kt, Bt, Pb, vb = [bA[k] for k in ["Rh", "Ah", "Kh", "Bh", "Kt", "Bt", "Pb", "vb"]]
                Vtm, Atm, Bttm, Kttm, WT = VtmL[j], AtmL[j], BttmL[j], KttmL[j], WTL[j]
                mem_j = members[j]
                for (src, dstb) in [(vb, Vtm), (Ah, Atm), (Bt, Bttm), (Kt, Kttm)]:
                    p = ptb.get()
                    kb.tr(p[:], src[:, js], C["ident_bf"][:], [src, C["ident_bf"]], [p])
                    kb.cp(dstb[:], p[:], [p], [dstb], eng="act")
                for hh in range(2):
                    m = mem_j[hh]
                    ps_ = slice(hh * 64, hh * 64 + 64)
                    p = pt.get()
                    kb.mm(p[:], Bh[ps_, js], Ah[ps_, js], [Bh, Ah], [p])
                    kb.stt(m["U"][:], p[:], -1.0, C["triu_s"][:], ALU.mult, ALU.mult, [p, C["triu_s"]], [m["U"]])
                    p = pt.get()
                    kb.mm(p[:], Ah[ps_, js], Bh[ps_, js], [Bh, Ah], [p])
                    kb.stt(m["L"][:], p[:], -1.0, C["tril_s"][:], ALU.mult, ALU.mult, [p, C["tril_s"]], [m["L"]])
                    p = pt.get()
                    kb.mm(p[:], Kh[ps_, js], Ah[ps_, js], [Kh, Ah], [p])
                    kb.tt(m["AakT"][:], p[:], C["triu_s"][:], ALU.mult, [p, C["triu_s"]], [m["AakT"]])
                    p = pt.get()
                    kb.mm(p[:], Bh[ps_, js], Rh[ps_, js], [Bh, Rh], [p])
                    kb.tt(m["ArbT"][:], p[:], C["triu_i"][:], ALU.mult, [p, C["triu_i"]], [m["ArbT"]])
                    p = pt.get()
                    kb.mm(p[:], Kh[ps_, js], Rh[ps_, js], [Kh, Rh], [p])
                    kb.tt(m["ArkT"][:], p[:], C["triu_i"][:], ALU.mult, [p, C["triu_i"]], [m["ArkT"]])
            neumann(kb, [m_ for row_ in members for m_ in row_], pt)
            for j in range(CPT):
                n = tt * CPT + j
                js = slice(j * 128, (j + 1) * 128)
                cs = slice(t0 + j * 128, t0 + (j + 1) * 128)
                Rh, Ah, Kh, Bh, Kt, Bt, Pb, vb = [bA[k] for k in ["Rh", "Ah", "Kh", "Bh", "Kt", "Bt", "Pb", "vb"]]
                Vtm, Atm, Bttm, Kttm, WT = VtmL[j], AtmL[j], BttmL[j], KttmL[j], WTL[j]
                mem_j = members[j]
                for hh in range(2):
                    m = mem_j[hh]
                    hs = slice(hh * 64, hh * 64 + 64)
                    kb.cp(m["Ybf"][:], m["Y"][:], [m["Y"]], [m["Ybf"]], eng="pool")
                    p = pt.get()
                    kb.mm(p[:, 0:64], m["AakT"][:], Vtm[:, hs], [m["AakT"], Vtm], [p])
                    kb.cp(m["AKV"][:], p[:, 0:64], [p], [m["AKV"]], eng="act")
                    p = pt.get()
                    kb.mm(p[:, 0:64], m["Ybf"][:], m["AKV"][:], [m["Ybf"], m["AKV"]], [p])
                    kb.cp(m["U2v"][:], p[:, 0:64], [p], [m["U2v"]], eng="dve")
                    p = pt.get()
                    kb.mm(p[:], Atm[:], m["Ybf"][:], [Atm, m["Ybf"]], [p])
                    kb.cp(WT[hs, :], p[hs, :], [p], [WT], eng="act")
            for j in range(CPT):
                n = tt * CPT + j
                js = slice(j * 128, (j + 1) * 128)
                cs = slice(t0 + j * 128, t0 + (j + 1) * 128)
                Rh, Ah, Kh, Bh, Kt, Bt, Pb, vb = [bA[k] for k in ["Rh", "Ah", "Kh", "Bh", "Kt", "Bt", "Pb", "vb"]]
                Vtm, Atm, Bttm, Kttm, WT = VtmL[j], AtmL[j], BttmL[j], KttmL[j], WTL[j]
                mem_j = members[j]
                for hh in range(2):
                    m = mem_j[hh]
                    hs = slice(hh * 64, hh * 64 + 64)
                    p = pt.get()
                    kb.mm(p[:, 0:64], WT[hs, :], Hbf[hs, :], [WT, Hbf], [p])
                    kb.tt(m["Ub"][:], p[:, 0:64], m["U2v"][:], ALU.add, [p, m["U2v"]], [m["Ub"]])
                    py = pt.get()
                    kb.mm(py[:, 0:64], Rh[hs, js], Hbf[hs, :], [Rh, Hbf], [py], start=True, stop=False, inc=False)
                    kb.mm(py[:, 0:64], m["ArkT"][:], Vtm[:, hs], [m["ArkT"], Vtm], [py], start=False, stop=False, inc=False)
                    kb.mm(py[:, 0:64], m["ArbT"][:], m["Ub"][:], [m["ArbT"], m["Ub"]], [py], start=False, stop=True)
                    kb.cp(Yp[:, hs], py[:, 0:64], [py], [Yp], eng="act")
                    ph = pt.get()
                    kb.mm(ph[:, 0:64], Kttm[:], Vtm[:, hs], [Kttm, Vtm], [ph], start=True, stop=False, inc=False)
                    kb.mm(ph[:, 0:64], Bttm[:], m["Ub"][:], [Bttm, m["Ub"]], [ph], start=False, stop=True)
                    kb.stt(H[hs, :], H[hs, :], eGl[hs, n:n + 1], ph[hs, 0:64], ALU.mult, ALU.add, [H, eGl, ph], [H])
                    kb.cp(Hbf[hs, :], H[hs, :], [H], [Hbf], eng="act")
                for hh in range(2):
                    hs = slice(hh * 64, hh * 64 + 64)
                    kb.red(st[:, 0:1], Yp[:, hs], ALU.add, [Yp], [st])
                    kb.act(junk[:, 0:64], Yp[:, hs], AF.Square, [Yp], [junk, st], accum=st[:, 1:2])
                    kb.ts(st[:, 0:2], st[:, 0:2], 1.0 / 64, None, ALU.mult, None, [st], [st])
                    kb.tt(st[:, 2:3], st[:, 0:1], st[:, 0:1], ALU.mult, [st], [st])
                    kb.tt(st[:, 2:3], st[:, 1:2], st[:, 2:3], ALU.subtract, [st], [st])
                    kb.rsqrt_col(st[:, 2:3], st, 64e-5, 1.0)
                    kb.ts(Yp[:, hs], Yp[:, hs], st[:, 0:1], st[:, 2:3], ALU.subtract, ALU.mult, [Yp, st], [Yp])
                kb.tt(Yp[:], Yp[:], bc[:, 1, :], ALU.mult, [Yp, bc], [Yp])
                kb.tt(Yp[:], Yp[:], bc[:, 2, :], ALU.add, [Yp, bc], [Yp])
                p = pt.get()
                kb.mm(p[:, 0:2], Pb[:, js], C["headsel_bf"][:, 0:2], [Pb, C["headsel_bf"]], [p])
                kb.cp(st[:, 4:6], p[:, 0:2], [p], [st], eng="act")
                for hh in range(2):
                    hs = slice(hh * 64, hh * 64 + 64)
                    kb.stt(Yp[:, hs], Vtm[:, hs], st[:, 4 + hh:5 + hh], Yp[:, hs], ALU.mult, ALU.add, [Vtm, st, Yp], [Yp])
                p = pt.get()
                kb.mm(p[:], L["sgd"][:, 0, cs], gup[:, 0, :], [L["sgd"], gup], [p], start=True, stop=False, inc=False)
                kb.mm(p[:], L["sgd"][:, 1, cs], gup[:, 1, :], [L["sgd"], gup], [p], start=False, stop=True)
                kb.tt(Yo[:], Yp[:], p[:], ALU.mult, [Yp, p], [Yo])
                p = ptb.get()
                kb.tr(p[:], Yo[:], C["ident_bf"][:], [Yo, C["ident_bf"]], [p])
                kb.cp(yf[:, cs], p[:], [p], [yf], eng="act")
        kb.dma("sp", d["oy_d"][c.GW + ch0:c.GW + ch0 + 128, :], yf[:], [yf], [kb.K_oy])
        S.sb.release(); S.ps.release()
    S.sb.release(); S.ps.release()


def phase_mix(kb, tok0):
    c, S, d, C = kb.cfg, kb.S, kb.d, kb.C
    KC, D, T, TT, TM = c.KC, c.D, c.T, c.TT, c.TM
    KH = KC // 2
    S.sb.mark(); S.ps.mark()
    mT = S.sb.alloc([128, KC, TT], BF16, "mT")
    g1bc = S.sb.alloc([128, D], F32, "g1bc")
    kb.dma("sp", g1bc[:], d["mod_d"][:, 2 * D:3 * D], [kb.K_mod], [g1bc])
    wsrc = d["w_in"].rearrange("(kc p) n -> p kc n", p=128)
    gosrc = d["w_gdn_o"].rearrange("(kc p) n -> p kc n", p=128)
    rosrc = d["w_rwkv_o"].rearrange("(kc p) n -> p kc n", p=128)
    wosrc = d["w_out"].rearrange("(kc p) n -> p kc n", p=128)
    hsrc = d["hT_d"].rearrange("(kc p) t -> p kc t", p=128)
    osrc = d["oy_d"].rearrange("(kc p) t -> p kc t", p=128)
    K_wc = Key("wcache")
    NW = min(512, D)
    for j in range(KC):
        kb.S.dma("pool", [
            (lambda e, j=j: e.dma_start(out=d["wga_c"][j].rearrange("p (kc n) -> p kc n", kc=KC), in_=wsrc[:, :, c.OFF_G + j * 128:c.OFF_G + (j + 1) * 128])),
            (lambda e, j=j: e.dma_start(out=d["wgb_c"][j].rearrange("p (kc n) -> p kc n", kc=KC), in_=wsrc[:, :, c.OFF_G + D + j * 128:c.OFF_G + D + (j + 1) * 128])),
            (lambda e, j=j: e.dma_start(out=d["wgo_c"][j].rearrange("p (kc n) -> p kc n", kc=KH), in_=gosrc[:, :, j * 128:(j + 1) * 128])),
            (lambda e, j=j: e.dma_start(out=d["wro_c"][j].rearrange("p (kc n) -> p kc n", kc=KH), in_=rosrc[:, :, j * 128:(j + 1) * 128])),
        ], reads=[], writes=[K_wc], semkey="wcache")
    for n in range(D // NW):
        kb.S.dma("pool", (lambda e, n=n: e.dma_start(out=d["wo_c"][n].rearrange("p (kc n) -> p kc n", kc=KC), in_=wosrc[:, :, n * NW:(n + 1) * NW])),
                 reads=[], writes=[K_wc], semkey="wcache")
    for tt in range(TM // TT):
        t0 = tok0 + tt * TT
        S.sb.mark(); S.ps.mark()
        hb = S.sb.alloc([128, KC, TT], BF16, "hTm"); ob = S.sb.alloc([128, KC, TT], BF16, "oTm")
        wga = [S.sb.alloc([128, KC, 128], BF16, "wga%d" % i) for i in range(2)]
        wgb = [S.sb.alloc([128, KC, 128], BF16, "wgb%d" % i) for i in range(2)]
        wgo = [S.sb.alloc([128, KH, 128], BF16, "wgo%d" % i) for i in range(2)]
        wro = [S.sb.alloc([128, KH, 128], BF16, "wro%d" % i) for i in range(2)]
        sa = S.sb.alloc([128, TT], F32, "sa"); sbb = S.sb.alloc([128, TT], F32, "sbb"); ta = S.sb.alloc([128, TT], F32, "ta")
        pp = [S.ps.alloc([128, 512], F32, "pm%d" % i) for i in range(8)]
        kb.dma("sp", hb[:], hsrc[:, :, t0:t0 + TT], [kb.K_hT], [hb])
        kb.dma("sp", ob[:], osrc[:, :, t0:t0 + TT], [kb.K_oy], [ob])
        for j in range(KC):
            i = j % 2
            kb.dma("sp", wga[i][:], d["wga_c"][j].rearrange("p (kc n) -> p kc n", kc=KC), [K_wc], [wga[i]])
            kb.dma("sp", wgb[i][:], d["wgb_c"][j].rearrange("p (kc n) -> p kc n", kc=KC), [K_wc], [wgb[i]])
            kb.dma("sp", wgo[i][:], d["wgo_c"][j].rearrange("p (kc n) -> p kc n", kc=KH), [K_wc], [wgo[i]])
            kb.dma("sp", wro[i][:], d["wro_c"][j].rearrange("p (kc n) -> p kc n", kc=KH), [K_wc], [wro[i]])
            pa, pb_, pc, pd = [pp[(4 * j + q) % 8] for q in range(4)]
            for kc in range(KC):
                kb.mm(pa[:, 0:TT], wga[i][:, kc, :], hb[:, kc, :], [wga[i], hb], [pa], start=(kc == 0), stop=(kc == KC - 1), inc=(kc == KC - 1))
            for kc in range(KC):
                kb.mm(pb_[:, 0:TT], wgb[i][:, kc, :], hb[:, kc, :], [wgb[i], hb], [pb_], start=(kc == 0), stop=(kc == KC - 1), inc=(kc == KC - 1))
            for kc in range(KH):
                kb.mm(pc[:, 0:TT], wgo[i][:, kc, :], ob[:, kc, :], [wgo[i], ob], [pc], start=(kc == 0), stop=(kc == KH - 1), inc=(kc == KH - 1))
            for kc in range(KH):
                kb.mm(pd[:, 0:TT], wro[i][:, kc, :], ob[:, KH + kc, :], [wro[i], ob], [pd], start=(kc == 0), stop=(kc == KH - 1), inc=(kc == KH - 1))
            kb.act(sa[:], pa[:, 0:TT], AF.Sigmoid, [pa], [sa])
            kb.act(sbb[:], pb_[:, 0:TT], AF.Sigmoid, [pb_], [sbb])
            kb.tt(ta[:], sa[:], pc[:, 0:TT], ALU.mult, [sa, pc], [ta])
            kb.tt(sbb[:], sbb[:], pd[:, 0:TT], ALU.mult, [sbb, pd], [sbb])
            kb.tt(mT[:, j, :], ta[:], sbb[:], ALU.add, [ta, sbb], [mT], eng="pool")
        S.sb.release(); S.ps.release()
        S.sb.mark(); S.ps.mark()
        NW = min(512, D)
        wo = [S.sb.alloc([128, KC, NW], BF16, "wo%d" % i) for i in range(2)]
        xt = [S.sb.alloc([128, D], F32, "xtm%d" % i) for i in range(TT // 128)]
        po = [S.ps.alloc([128, 512], F32, "po%d" % i) for i in range(4)]
        for i in range(TT // 128):
            kb.dma("sp", xt[i][:], d["x"][t0 + i * 128:t0 + (i + 1) * 128, :], [], [xt[i]])
        pi = 0
        for n in range(D // NW):
            w = wo[n % 2]
            kb.dma("sp", w[:], d["wo_c"][n].rearrange("p (kc n) -> p kc n", kc=KC), [K_wc], [w])
            for i in range(TT // 128):
                p = po[pi % 4]; pi += 1
                for kc in range(KC):
                    kb.mm(p[:, 0:NW], mT[:, kc, i * 128:(i + 1) * 128], w[:, kc, :], [mT, w], [p], start=(kc == 0), stop=(kc == KC - 1), inc=(kc == KC - 1))
                cs = slice(n * NW, (n + 1) * NW)
                kb.tt(ta_ := None, None, None, None, [], []) if False else None
                kb.S.op("dve", (lambda e, i=i, p=p, cs=cs: e.tensor_tensor(out=p[:, 0:NW], in0=p[:, 0:NW], in1=g1bc[:, cs], op=ALU.mult)), reads=[g1bc], writes=[p])
                kb.tt(xt[i][:, cs], xt[i][:, cs], p[:, 0:NW], ALU.add, [xt[i], p], [xt[i]])
        for i in range(TT // 128):
            r0 = tt * TT + i * 128
            kb.dma("sp", d["xmid_d"][r0:r0 + 128, :], xt[i][:], [xt[i]], [kb.K_xmid])
        S.sb.release(); S.ps.release()
    S.sb.release(); S.ps.release()


def phase_route(kb):
    c, S, d, C = kb.cfg, kb.S, kb.d, kb.C
    KC, D, TM, NBLK = c.KC, c.D, c.TM, c.NBLK
    NTm = TM // 128
    R = {}
    R["wts"] = S.sb.alloc([128, NTm, 2], F32, "wts")
    R["desti"] = S.sb.alloc([128, NTm, 2], I32, "desti")
    R["idxW"] = S.sb.alloc([128, NBLK], I32, "idxW")
    S.sb.mark(); S.ps.mark()
    M1 = S.sb.alloc([128, NTm, 64], F32, "M1"); M2 = S.sb.alloc([128, NTm, 64], F32, "M2")
    Msb = S.sb.alloc([128, NTm, 64], BF16, "Msb")
    destf = S.sb.alloc([128, NTm, 2], F32, "destf")
    A2 = S.sb.alloc([128, D], F32, "A2"); B2 = S.sb.alloc([128, D], F32, "B2")
    wr = S.sb.alloc([128, KC, 72], F32, "wr"); brb = S.sb.alloc([128, 72], F32, "brb")
    xm = [S.sb.alloc([128, D], F32, "xmr%d" % i) for i in range(2)]
    h2b = [S.sb.alloc([128, D], BF16, "h2b%d" % i) for i in range(2)]
    junk = S.sb.alloc([128, D], BF16, "junkq")
    hTc = [S.sb.alloc([128, 128], F32, "hTc%d" % i) for i in range(4)]
    lg = S.sb.alloc([128, 72], F32, "lg"); sm = S.sb.alloc([128, 64], F32, "sm"); st = S.sb.alloc([128, 16], F32, "stq")
    ptr = Rot([S.ps.alloc([128, 128], F32, "ptq%d" % i) for i in range(5)])
    pl = S.ps.alloc([128, 128], F32, "plq")
    kb.dma("sp", A2[:], d["mod_d"][:, 4 * D:5 * D], [kb.K_mod], [A2])
    kb.dma("sp", B2[:], d["n2g"][0:1, :].partition_broadcast(128), [], [B2])
    kb.stt(A2[:], A2[:], 1.0, B2[:], ALU.add, ALU.mult, [A2, B2], [A2])
    kb.dma("sp", B2[:], d["mod_d"][:, 3 * D:4 * D], [kb.K_mod], [B2])
    kb.dma("sp", wr[:], d["wr"].rearrange("(kc p) n -> p kc n", p=128), [], [wr])
    kb.dma("sp", brb[:], d["br"][0:1, :].partition_broadcast(128), [], [brb])
    for n in range(NTm):
        x_ = xm[n % 2]; hb = h2b[n % 2]
        kb.dma("sp", x_[:], d["xmid_d"][n * 128:(n + 1) * 128, :], [kb.K_xmid], [x_])
        kb.act(junk[:], x_[:], AF.Square, [x_], [junk, st], accum=st[:, 0:1])
        kb.rsqrt_col(st[:, 0:1], st, 1e-6, 1.0 / D)
        kb.stt(x_[:], x_[:], st[:, 0:1], A2[:], ALU.mult, ALU.mult, [x_, st, A2], [x_])
        kb.tt(x_[:], x_[:], B2[:], ALU.add, [x_, B2], [x_])
        kb.cp(hb[:], x_[:], [x_], [hb], eng="pool")
        kb.dma("sp", d["h2_d"][n * 128:(n + 1) * 128, :], hb[:], [hb], [kb.K_h2])
        for kc in range(KC):
            p = ptr.get(); hc = hTc[kc % 4]
            kb.tr(p[:], x_[:, kc * 128:(kc + 1) * 128], C["ident"][:], [x_, C["ident"]], [p])
            kb.cp(hc[:], p[:], [p], [hc], eng=("act" if kc % 2 == 0 else "dve"))
            kb.mm(pl[:, 0:72], hc[:], wr[:, kc, :], [hc, wr], [pl], start=(kc == 0), stop=(kc == KC - 1), inc=(kc == KC - 1))
        kb.tt(lg[:], pl[:, 0:72], brb[:], ALU.add, [pl, brb], [lg])
        kb.red(st[:, 1:2], lg[:, 0:8], ALU.max, [lg], [st])
        ohg = sm[:, 0:8]
        kb.ts(ohg, lg[:, 0:8], st[:, 1:2], None, ALU.is_equal, None, [lg, st], [sm])
        kb.ts(st[:, 2:3], st[:, 1:2], -1.0, None, ALU.mult, None, [st], [st])
        kb.act(sm[:, 8:16], lg[:, 0:8], AF.Exp, [lg, st], [sm, st], bias=st[:, 2:3], accum=st[:, 3:4])
        kb.recip(st[:, 3:4], st[:, 3:4], [st], [st])
        esel = sm[:, 16:24]
        kb.ts(esel, lg[:, 8:16], sm[:, 0:1], None, ALU.mult, None, [lg, sm], [sm])
        for g in range(1, 8):
            kb.stt(esel, lg[:, 8 + 8 * g:16 + 8 * g], sm[:, g:g + 1], esel, ALU.mult, ALU.add, [lg, sm], [sm])
        kb.red(st[:, 4:5], esel, ALU.max, [sm], [st])
        mk1 = sm[:, 24:32]; es2 = sm[:, 32:40]; mk2 = sm[:, 40:48]
        kb.ts(mk1, esel, st[:, 4:5], None, ALU.is_equal, None, [sm, st], [sm])
        kb.stt(es2, mk1, -1e30, esel, ALU.mult, ALU.add, [sm], [sm])
        kb.red(st[:, 5:6], es2, ALU.max, [sm], [st])
        kb.ts(mk2, es2, st[:, 5:6], None, ALU.is_equal, None, [sm, st], [sm])
        kb.tt(st[:, 6:7], st[:, 4:5], st[:, 5:6], ALU.subtract, [st], [st])
        kb.act(st[:, 6:7], st[:, 6:7], AF.Sigmoid, [st], [st])
        kb.ts(st[:, 7:8], st[:, 6:7], -1.0, 1.0, ALU.mult, ALU.add, [st], [st])
        kb.ts(R["wts"][:, n, :], st[:, 6:8], st[:, 3:4], None, ALU.mult, None, [st], [R["wts"]])
        for g in range(8):
            kb.ts(M1[:, n, 8 * g:8 * g + 8], mk1, sm[:, g:g + 1], None, ALU.mult, None, [sm], [M1], eng="pool")
            kb.ts(M2[:, n, 8 * g:8 * g + 8], mk2, sm[:, g:g + 1], None, ALU.mult, None, [sm], [M2], eng="pool")
        kb.tt(Msb[:, n, :], M1[:, n, :], M2[:, n, :], ALU.add, [M1, M2], [Msb], eng="pool")
    cnt = S.sb.alloc([128, 8], F32, "cnt")
    pc = ptr.get()
    for n in range(NTm):
        kb.mm(pc[0:64, 0:1], Msb[:, n, :], C["ones_bf"][:, 0:1], [Msb, C["ones_bf"]], [pc], start=(n == 0), stop=(n == NTm - 1), inc=(n == NTm - 1))
    kb.cp(cnt[0:64, 0:1], pc[0:64, 0:1], [pc], [cnt], eng="act")
    thr = S.sb.alloc([128, 128], F32, "thr")
    assert 2 * TM // 128 <= 128
    kb.ts(thr[0:64, :], C["iota_f"][0:64, :], 128.0, None, ALU.mult, None, [C["iota_f"]], [thr])
    kb.ts(thr[0:64, :], thr[0:64, :], cnt[0:64, 0:1], None, ALU.is_lt, None, [thr, cnt], [thr])
    kb.red(cnt[0:64, 2:3], thr[0:64, :], ALU.add, [thr], [cnt])
    nbrep = S.sb.alloc([128, 128], F32, "nbrep")
    kb.ts(nbrep[0:64, :], C["ones"][0:64, :], cnt[0:64, 2:3], None, ALU.mult, None, [C["ones"], cnt], [nbrep])
    p = ptr.get()
    kb.mm(p[:, 0:64], nbrep[0:64, :], C["triu_s"][0:64, 0:64], [nbrep, C["triu_s"]], [p])
    startrow = S.sb.alloc([128, 64], F32, "startrow")
    kb.ts(startrow[:], p[:, 0:64], 128.0, None, ALU.mult, None, [p], [startrow])
    p = ptr.get()
    kb.mm(p[0:64, 0:1], C["triu_i"][0:64, 0:64], cnt[0:64, 2:3], [C["triu_i"], cnt], [p])
    kb.cp(cnt[0:64, 3:4], p[0:64, 0:1], [p], [cnt], eng="act")
    Bm = S.sb.alloc([128, 128], F32, "Bm")
    kb.ts(Bm[0:64, 0:NBLK], C["iota_f"][0:64, 0:NBLK], cnt[0:64, 3:4], None, ALU.is_ge, None, [C["iota_f"], cnt], [Bm])
    p = ptr.get()
    kb.mm(p[:, 0:NBLK], C["ones"][0:64, :], Bm[0:64, 0:NBLK], [C["ones"], Bm], [p])
    idxf = S.sb.alloc([128, 128], F32, "idxf")
    kb.stt(idxf[:, 0:NBLK], p[:, 0:NBLK], 128.0, C["iota_p"][:, 0:NBLK], ALU.mult, ALU.add, [p, C["iota_p"]], [idxf])
    kb.cp(R["idxW"][:], idxf[:, 0:NBLK], [idxf], [R["idxW"]], eng="dve")
    carry = S.sb.alloc([128, 64], F32, "carry"); df = S.sb.alloc([128, 64], F32, "df"); tmp = S.sb.alloc([128, 64], F32, "tmpq")
    kb.cp(carry[:], startrow[:], [startrow], [carry], eng="pool")
    for n in range(NTm):
        p = ptr.get()
        kb.mm(p[:, 0:64], C["triu_s_bf"][:], Msb[:, n, :], [C["triu_s_bf"], Msb], [p])
        kb.tt(df[:], p[:, 0:64], carry[:], ALU.add, [p, carry], [df])
        kb.tt(tmp[:], df[:], M1[:, n, :], ALU.mult, [df, M1], [tmp])
        kb.red(destf[:, n, 0:1], tmp[:], ALU.add, [tmp], [destf])
        kb.tt(tmp[:], df[:], M2[:, n, :], ALU.mult, [df, M2], [tmp])
        kb.red(destf[:, n, 1:2], tmp[:], ALU.add, [tmp], [destf])
        p = ptr.get()
        kb.mm(p[:, 0:64], C["ones_bf"][:], Msb[:, n, :], [C["ones_bf"], Msb], [p])
        kb.tt(carry[:], carry[:], p[:, 0:64], ALU.add, [carry, p], [carry])
    kb.cp(R["desti"][:].rearrange("p n j -> p (n j)"), destf[:].rearrange("p n j -> p (n j)"), [destf], [R["desti"]], eng="dve")
    fill = S.sb.alloc([128, NBLK], I32, "fill"); tokv = S.sb.alloc([128, NTm], I32, "tokv")
    zrow = S.sb.alloc([128, D], BF16, "zrow")
    kb.memset(fill[:], TM, [fill]); kb.memset(zrow[0:1, :], 0.0, [zrow])
    kb.dma("sp", tokv[:], d["tokiota"][:, :], [], [tokv])
    kb.dma("sp", d["h2_d"][TM:TM + 1, :], zrow[0:1, :], [zrow], [kb.K_h2])
    kb.dma("sp", d["tokid_d"].rearrange("(p b) o -> p (b o)", p=128), fill[:], [fill], [kb.K_tokid])
    for n in range(NTm):
        for j in range(2):
            kb.S.dma("pool", (lambda e, n=n, j=j: e.indirect_dma_start(
                out=d["tokid_d"][:, :], out_offset=bass.IndirectOffsetOnAxis(ap=R["desti"][:, n, j:j + 1], axis=0),
                in_=tokv[:, n:n + 1], in_offset=None)),
                reads=[R["desti"], tokv, kb.K_tokid], writes=[kb.K_tokid2], semkey="tokscat")
    S.sb.release(); S.ps.release()
    return R


def phase_moe(kb, R):
    c, S, d, C = kb.cfg, kb.S, kb.d, kb.C
    KC, D, TM, NBLK, DE, FC = c.KC, c.D, c.TM, c.NBLK, c.DE, c.FC
    S.sb.mark(); S.ps.mark()
    w1t = S.sb.alloc([128, KC * DE], BF16, "w1t"); w3t = S.sb.alloc([128, KC * DE], BF16, "w3t"); w2t = S.sb.alloc([128, FC * D], BF16, "w2t")
    xb = [S.sb.alloc([128, D], BF16, "xb%d" % i) for i in range(2)]
    idb = [S.sb.alloc([128, 1], I32, "idb%d" % i) for i in range(2)]
    xbT = S.sb.alloc([128, KC, 128], BF16, "xbT"); aT = S.sb.alloc([128, FC, 128], BF16, "aT")
    s1 = S.sb.alloc([128, DE], F32, "s1"); ab = S.sb.alloc([128, DE], BF16, "ab_")
    yb = [S.sb.alloc([128, D], F32, "yb%d" % i) for i in range(2)]
    pst = Rot([S.ps.alloc([128, 4, 128], BF16, "pe%d" % i) for i in range(2)])
    ph = [S.ps.alloc([128, 512], F32, "ph%d" % i) for i in range(2)]
    py = Rot([S.ps.alloc([128, 512], F32, "py%d" % i) for i in range(4)])
    NW = min(512, D)
    for b in range(NBLK):
        i = b % 2
        kb.dma("sp", idb[i][:], d["tokid_d"][b * 128:(b + 1) * 128, :], [kb.K_tokid2], [idb[i]])
        kb.S.dma("pool", (lambda e, i=i: e.indirect_dma_start(out=xb[i][:], out_offset=None, in_=d["h2_d"][:, :],
                 in_offset=bass.IndirectOffsetOnAxis(ap=idb[i][:, 0:1], axis=0))),
                 reads=[idb[i], kb.K_h2], writes=[xb[i]])
        for (wt, nm) in [(w1t, "w1"), (w3t, "w3"), (w2t, "w2")]:
            kb.S.dma("pool", (lambda e, wt=wt, nm=nm, b=b: e.indirect_dma_start(out=wt[:], out_offset=None, in_=d[nm][:, :],
                     in_offset=bass.IndirectOffsetOnAxis(ap=R["idxW"][:, b:b + 1], axis=0), bounds_check=kb.breg(e, 64 * 128 - 1), oob_is_err=False)),
                     reads=[R["idxW"]], writes=[wt])
        G = min(4, KC)
        for g in range(KC // G):
            p = pst.get()
            for j in range(G):
                kc = g * G + j
                kb.tr(p[:, j, :], xb[i][:, kc:D:KC], C["ident_bf"][:], [xb[i], C["ident_bf"]], [p], inc=(j == G - 1))
            kb.cp(xbT[:, g * G:(g + 1) * G, :], p[:, 0:G, :], [p], [xbT], eng=("act" if g % 2 == 0 else "dve"))
        for kc in range(KC):
            kb.mm(ph[0][:, 0:DE], xbT[:, kc, :], w1t[:, kc * DE:(kc + 1) * DE], [xbT, w1t], [ph[0]], start=(kc == 0), stop=(kc == KC - 1), inc=(kc == KC - 1))
        for kc in range(KC):
            kb.mm(ph[1][:, 0:DE], xbT[:, kc, :], w3t[:, kc * DE:(kc + 1) * DE], [xbT, w3t], [ph[1]], start=(kc == 0), stop=(kc == KC - 1), inc=(kc == KC - 1))
        kb.act(s1[:], ph[0][:, 0:DE], AF.Silu, [ph[0]], [s1])
        kb.tt(ab[:], s1[:], ph[1][:, 0:DE], ALU.mult, [s1, ph[1]], [ab])
        p = pst.get()
        for fc in range(FC):
            kb.tr(p[:, fc, :], ab[:, fc:DE:FC], C["ident_bf"][:], [ab, C["ident_bf"]], [p], inc=(fc == FC - 1))
        kb.cp(aT[:], p[:, 0:FC, :], [p], [aT], eng="act")
        for n in range(D // NW):
            pq = py.get()
            for fc in range(FC):
                kb.mm(pq[:, 0:NW], aT[:, fc, :], w2t[:, fc * D + n * NW:fc * D + (n + 1) * NW], [aT, w2t], [pq], start=(fc == 0), stop=(fc == FC - 1), inc=(fc == FC - 1))
            kb.cp(yb[i][:, n * NW:(n + 1) * NW], pq[:, 0:NW], [pq], [yb[i]], eng=("act" if n % 2 == 0 else "dve"))
        kb.dma("sp", d["yb_d"][b * 128:(b + 1) * 128, :], yb[i][:], [yb[i]], [kb.K_yb])
    S.sb.release(); S.ps.release()


def phase_final(kb, R):
    c, S, d, C = kb.cfg, kb.S, kb.d, kb.C
    D, TM, NBLK = c.D, c.TM, c.NBLK
    S.sb.mark()
    g2 = S.sb.alloc([128, D], F32, "g2bc"); nf = S.sb.alloc([128, D], F32, "nfbc")
    r1 = [S.sb.alloc([128, D], F32, "r1_%d" % i) for i in range(2)]; r2 = [S.sb.alloc([128, D], F32, "r2_%d" % i) for i in range(2)]
    xm = [S.sb.alloc([128, D], F32, "xmf%d" % i) for i in range(2)]
    junk = S.sb.alloc([128, D], BF16, "junkf"); st = S.sb.alloc([128, 2], F32, "stf")
    kb.dma("sp", g2[:], d["mod_d"][:, 5 * D:6 * D], [kb.K_mod], [g2])
    kb.dma("sp", nf[:], d["nfg"][0:1, :].partition_broadcast(128), [], [nf])
    for n in range(TM // 128):
        i = n % 2
        for j, rb in enumerate([r1[i], r2[i]]):
            kb.S.dma("pool", (lambda e, rb=rb, n=n, j=j: e.indirect_dma_start(out=rb[:], out_offset=None, in_=d["yb_d"][:, :],
                     in_offset=bass.IndirectOffsetOnAxis(ap=R["desti"][:, n, j:j + 1], axis=0))),
                     reads=[R["desti"], kb.K_yb], writes=[rb])
        kb.dma("sp", xm[i][:], d["xmid_d"][n * 128:(n + 1) * 128, :], [kb.K_xmid], [xm[i]])
        kb.ts(r1[i][:], r1[i][:], R["wts"][:, n, 0:1], None, ALU.mult, None, [r1[i], R["wts"]], [r1[i]])
        kb.stt(r1[i][:], r2[i][:], R["wts"][:, n, 1:2], r1[i][:], ALU.mult, ALU.add, [r2[i], R["wts"], r1[i]], [r1[i]])
        kb.tt(r1[i][:], r1[i][:], g2[:], ALU.mult, [r1[i], g2], [r1[i]], eng="pool")
        kb.tt(xm[i][:], xm[i][:], r1[i][:], ALU.add, [xm[i], r1[i]], [xm[i]])
        kb.act(junk[:], xm[i][:], AF.Square, [xm[i]], [junk, st], accum=st[:, 0:1])
        kb.rsqrt_col(st[:, 0:1], st, 1e-6, 1.0 / D)
        kb.stt(xm[i][:], xm[i][:], st[:, 0:1], nf[:], ALU.mult, ALU.mult, [xm[i], st, nf], [xm[i]])
        kb.dma("sp", d["out"][n * 128:(n + 1) * 128, :], xm[i][:], [xm[i]], [kb.K_out])
    S.sb.release()


NCORES = 4


def build_program(cfg):
    kb = KB(cfg)
    declare_inputs(kb)
    for k in ["mod_d", "hT_d", "oy_d", "xmid_d", "h2", "tokid", "tokid2", "yb", "out"]:
        setattr(kb, "K_" + k.replace("_d", ""), Key(k))
    S = kb.S
    load_consts(kb)
    phase_mod(kb)
    phase_norm1(kb)
    S.sb.mark(); P = phase_gdn_pre(kb, list(range(cfg.HG))); phase_gdn(kb, list(range(cfg.HG)), P); S.sb.release()
    S.sb.mark(); L = phase_rwkv_pre(kb); phase_rwkv(kb, list(range(cfg.NP)), L); S.sb.release()
    phase_mix(kb, 0)
    S.sb.mark(); R = phase_route(kb); phase_moe(kb, R); phase_final(kb, R); S.sb.release()
    S.wait_all("sp")
    assert S.simulate()
    S.emit()
    return kb


def kernel(**inputs):
    x = np.asarray(inputs["x"])
    B, T, D = x.shape
    DE = np.asarray(inputs["w1"]).shape[-1]
    cfg = Cfg(D, T, DE)
    kb = build_program(cfg)
    ncores = min(NCORES, B) if B < NCORES else NCORES
    in_maps = [host_inputs(cfg, inputs, b) for b in range(B)]
    res = run_bass_kernel_spmd(kb.nc, in_maps, core_ids=list(range(B)))
    out = np.stack([np.asarray(res.results[b]["out"]) for b in range(B)], axis=0)
    return out.astype(np.float32)
```

```python
import numpy as np
from contextlib import ExitStack
import concourse.bass as bass
import concourse.mybir as mybir

F32 = mybir.dt.float32
BF16 = mybir.dt.bfloat16
I32 = mybir.dt.int32
U8 = mybir.dt.uint8
AF = mybir.ActivationFunctionType
ALU = mybir.AluOpType
AX = mybir.AxisListType
DSZ = {F32: 4, BF16: 2, I32: 4, U8: 1}

GRAN = 512
SEM_LIMIT = 30000
DMA_SEM_LIMIT = 60000


class Buf:
    def __init__(self, arena, lo, nbytes, dtype, shape, name):
        self.arena, self.lo, self.hi, self.dtype, self.shape, self.name = arena, lo, lo + nbytes, dtype, shape, name
        ap = arena.t[:, lo:lo + nbytes]
        if dtype != U8:
            ap = ap.bitcast(dtype)
        P = shape[0]
        if P < 128:
            ap = ap[0:P]
        if len(shape) > 2:
            names = " ".join("d%d" % i for i in range(len(shape) - 1))
            kw = {"d%d" % i: shape[i + 1] for i in range(len(shape) - 1)}
            ap = ap.rearrange("p (%s) -> p %s" % (names, names), **kw)
        self.ap = ap

    def __getitem__(self, k):
        return self.ap[k]

    def grans(self):
        gr = 2048 if self.arena.id == "ps" else GRAN
        return [(self.arena.id, g) for g in range(self.lo // gr, (self.hi + gr - 1) // gr)]

    def sub(self, lo_el, n_el, shape=None):
        sz = DSZ[self.dtype]
        return Buf(self.arena, self.lo + lo_el * sz, n_el * sz, self.dtype, shape or [self.shape[0], n_el],
                   self.name + ".s")


class Arena:
    def __init__(self, t, nbytes, aid):
        self.t, self.nbytes, self.id, self.top, self.marks = t, nbytes, aid, 0, []

    def alloc(self, shape, dtype, name="b", align=64):
        if self.id == "ps":
            align = 2048
        n = int(np.prod(shape[1:])) * DSZ[dtype]
        lo = (self.top + align - 1) // align * align
        assert lo + n <= self.nbytes, "arena %s overflow: %s needs %d at %d / %d" % (self.id, name, n, lo, self.nbytes)
        self.top = lo + n
        return Buf(self, lo, n, dtype, list(shape), name)

    def mark(self):
        self.marks.append(self.top)

    def release(self):
        self.top = self.marks.pop()


class Key:
    def __init__(self, name):
        self.name = name

    def grans(self):
        return [("key", self.name)]


ENGS = ["pe", "act", "dve", "pool", "sp"]


class Sched:
    def __init__(self, nc, es, sbuf_bytes=190 * 1024):
        self.nc, self.es = nc, es
        self.sb = Arena(es.enter_context(nc.sbuf_tensor("arena", [128, sbuf_bytes], U8)), sbuf_bytes, "sb")
        self.ps = Arena(es.enter_context(nc.psum_tensor("psarena", [128, 16384], U8)), 16384, "ps")
        self.streams = {e: [] for e in ENGS}
        self.eng_sems = {e: [] for e in ENGS}
        self.eng_count = {e: 0 for e in ENGS}
        self.pending = {e: [] for e in ENGS}
        self.lastw = {}
        self.readers = {}
        self.waited = {e: {} for e in ENGS}
        self.dma_sems = {}
        self.nsem = 0
        self.nops = 0

    def _newsem(self, name):
        self.nsem += 1
        return self.es.enter_context(self.nc.semaphore("%s_%d" % (name, self.nsem)))

    @staticmethod
    def _excl(reads, writes):
        r2 = [r for r in reads if not (isinstance(r, Buf) and r.arena.id == "ps")]
        w2 = list(writes) + [r for r in reads if isinstance(r, Buf) and r.arena.id == "ps"]
        return r2, w2

    def _deps(self, reads, writes):
        reads, writes = self._excl(reads, writes)
        toks = {}

        def add(t):
            if t is None:
                return
            k = id(t[0])
            if k not in toks or toks[k][1] < t[1]:
                toks[k] = t
        for r in reads:
            for g in r.grans():
                add(self.lastw.get(g))
        for w in writes:
            for g in w.grans():
                add(self.lastw.get(g))
                for t in self.readers.get(g, {}).values():
                    add(t)
        return toks

    def _record(self, reads, writes, tok):
        reads, writes = self._excl(reads, writes)
        for r in reads:
            for g in r.grans():
                d = self.readers.setdefault(g, {})
                d[id(tok[0])] = tok
        for w in writes:
            for g in w.grans():
                self.lastw[g] = tok
                self.readers[g] = {}

    def _waits(self, stream, toks):
        out = []
        wd = self.waited[stream]
        for k, t in toks.items():
            if t[1] == 0:
                continue
            if len(t) > 2 and t[2] == stream and t[3] > self.eng_count[stream]:
                continue
            if wd.get(k, 0) >= t[1]:
                continue
            wd[k] = t[1]
            out.append(t)
        return out

    def op(self, eng, fn, reads=(), writes=(), inc=True):
        toks = self._deps(reads, writes)
        waits = self._waits(eng, toks)
        cnt = self.eng_count[eng]
        if cnt % SEM_LIMIT == 0 and (not self.eng_sems[eng] or cnt // SEM_LIMIT >= len(self.eng_sems[eng])):
            self.eng_sems[eng].append(self._newsem(eng))
        sem = self.eng_sems[eng][cnt // SEM_LIMIT]
        if inc:
            self.eng_count[eng] = cnt + 1
            tok = (sem, cnt % SEM_LIMIT + 1, eng, cnt + 1)
        else:
            tok = (sem, cnt % SEM_LIMIT + 1, eng, cnt + 1)
            self.pending[eng].append(1)
        if inc:
            self.pending[eng] = []
        self.streams[eng].append((fn, waits, (sem, 1) if inc else None))
        self._record(reads, writes, tok)
        self.nops += 1
        return tok

    def dma(self, queue, fns, reads=(), writes=(), semkey=None):
        if not isinstance(fns, (list, tuple)):
            fns = [fns]
        toks = self._deps(reads, writes)
        waits = self._waits(queue, toks)
        if semkey is None:
            semkey = (writes[0] if writes else reads[0]).grans()[0]
        if semkey not in self.dma_sems:
            self.dma_sems[semkey] = [self._newsem("dma"), 0]
        rec = self.dma_sems[semkey]
        if rec[1] + 16 * len(fns) > DMA_SEM_LIMIT:
            rec[0], rec[1] = self._newsem("dma"), 0
        for i, fn in enumerate(fns):
            self.streams[queue].append((fn, waits if i == 0 else [], (rec[0], 16)))
        rec[1] += 16 * len(fns)
        tok = (rec[0], rec[1])
        self._record(reads, writes, tok)
        self.nops += 1
        return tok

    def wait_all(self, eng):
        toks = {}
        for g, t in self.lastw.items():
            k = id(t[0])
            if k not in toks or toks[k][1] < t[1]:
                toks[k] = t
        waits = self._waits(eng, toks)
        self.streams[eng].append((None, waits, None))

    def emit(self):
        for e in ENGS:
            assert not self.pending[e], "engine %s has trailing inc=False ops" % e
        nc = self.nc
        with nc.Block() as block:
            def run(engobj, lst):
                for fn, waits, inc in lst:
                    for w in waits:
                        engobj.wait_ge(w[0], w[1])
                    if fn is None:
                        continue
                    ins = fn(engobj)
                    if inc is not None:
                        ins.then_inc(inc[0], inc[1])

            @block.tensor
            def _(t):
                run(t, self.streams["pe"])

            @block.scalar
            def _(t):
                run(t, self.streams["act"])

            @block.vector
            def _(t):
                run(t, self.streams["dve"])

            @block.gpsimd
            def _(t):
                run(t, self.streams["pool"])

            @block.sync
            def _(t):
                run(t, self.streams["sp"])


def simulate(self):
    pos = {e: 0 for e in ENGS}
    val = {}
    progress = True
    while progress:
        progress = False
        for e in ENGS:
            lst = self.streams[e]
            while pos[e] < len(lst):
                fn, waits, inc = lst[pos[e]]
                if all(val.get(id(w[0]), 0) >= w[1] for w in waits):
                    if inc is not None:
                        val[id(inc[0])] = val.get(id(inc[0]), 0) + inc[1]
                    pos[e] += 1
                    progress = True
                else:
                    break
    stuck = {e: (pos[e], len(self.streams[e])) for e in ENGS if pos[e] < len(self.streams[e])}
    for e, (p, n) in stuck.items():
        fn, waits, inc = self.streams[e][p]
        print("STUCK", e, p, n, [(val.get(id(w[0]), 0), w[1], w[2:] if len(w) > 2 else "dma") for w in waits])
    return not stuck


Sched.simulate = simulate

import numpy as np
from contextlib import ExitStack
from concourse.bass_utils import run_bass_kernel_spmd


class Cfg:
    def __init__(s, D, T, DE, pair=False):
        s.D, s.T, s.DE, s.pair = D, T, DE, pair
        s.KC = D // 128
        s.HG = D // 256
        s.GW = s.HG * 128
        s.HR = D // 128
        s.RW = s.HR * 64
        s.NP = s.RW // 128
        s.CONVC = 3 * s.GW
        s.OFF_Z = s.CONVC
        s.OFF_A = s.OFF_Z + s.GW
        s.OFF_B = s.OFF_A + s.HG
        s.OFF_R = s.OFF_B + s.HG
        s.RSC = 3 * s.RW + 448
        s.OFF_G = s.OFF_R + s.RSC
        s.INC = s.OFF_G + 2 * D
        s.NE, s.NG, s.EPG = 64, 8, 8
        s.NT = T // 128
        s.TT = min(512, T)
        s.NTT = T // s.TT
        s.FC = DE // 128
        s.TM = T // 2 if pair else T
        s.NBLK = (2 * s.TM + 64 * 127 + 127) // 128


class KB:
    def __init__(s, cfg, debug=()):
        s.cfg = cfg
        s.nc = bass.Bass("TRN2", target_bir_lowering=False)
        s.es = ExitStack()
        s.S = Sched(s.nc, s.es)
        s.d = {}
        s.debug = debug

    def breg(s, e, val):
        if not hasattr(s, "_bregs"):
            s._bregs = {}
        if val not in s._bregs:
            s._bregs[val] = e.to_reg(val)
        return s._bregs[val]

    def din(s, name, shape, dt=F32):
        s.d[name] = s.nc.dram_tensor(name, list(shape), dt, kind="ExternalInput").ap()
        return s.d[name]

    def dout(s, name, shape, dt=F32):
        s.d[name] = s.nc.dram_tensor(name, list(shape), dt, kind="ExternalOutput").ap()
        return s.d[name]

    def dscr(s, name, shape, dt=F32):
        s.d[name] = s.nc.dram_tensor(name, list(shape), dt, kind="Internal").ap()
        return s.d[name]

    def mm(s, out, lhsT, rhs, R, W, start=True, stop=True, inc=True):
        return s.S.op("pe", lambda e: e.matmul(out, lhsT, rhs, start=start, stop=stop), reads=R, writes=W, inc=inc)

    def tr(s, out, in_, ident, R, W, inc=True):
        return s.S.op("pe", lambda e: e.transpose(out, in_, ident), reads=R, writes=W, inc=inc)

    def act(s, out, in_, func, R, W, scale=1.0, bias=0.0, accum=None, eng="act"):
        if accum is not None:
            return s.S.op(eng, lambda e: e.activation(out=out, in_=in_, func=func, scale=scale, bias=bias, accum_out=accum), reads=R, writes=W)
        return s.S.op(eng, lambda e: e.activation(out=out, in_=in_, func=func, scale=scale, bias=bias), reads=R, writes=W)

    def ts(s, out, in0, s1, s2, op0, op1, R, W, eng="dve", accum=None):
        if op1 is None:
            return s.S.op(eng, lambda e: e.tensor_scalar(out=out, in0=in0, scalar1=s1, scalar2=None, op0=op0), reads=R, writes=W)
        if accum is not None:
            return s.S.op(eng, lambda e: e.tensor_scalar(out=out, in0=in0, scalar1=s1, scalar2=s2, op0=op0, op1=op1, accum_out=accum), reads=R, writes=W)
        return s.S.op(eng, lambda e: e.tensor_scalar(out=out, in0=in0, scalar1=s1, scalar2=s2, op0=op0, op1=op1), reads=R, writes=W)

    def tt(s, out, in0, in1, op, R, W, eng="dve"):
        return s.S.op(eng, lambda e: e.tensor_tensor(out=out, in0=in0, in1=in1, op=op), reads=R, writes=W)

    def stt(s, out, in0, scalar, in1, op0, op1, R, W, eng="dve"):
        return s.S.op(eng, lambda e: e.scalar_tensor_tensor(out=out, in0=in0, scalar=scalar, in1=in1, op0=op0, op1=op1), reads=R, writes=W)

    def cp(s, out, in_, R, W, eng="dve"):
        if eng == "act":
            return s.S.op("act", lambda e: e.activation(out=out, in_=in_, func=AF.Copy), reads=R, writes=W)
        return s.S.op(eng, lambda e: e.tensor_copy(out=out, in_=in_), reads=R, writes=W)

    def red(s, out, in_, op, R, W, eng="dve"):
        return s.S.op(eng, lambda e: e.tensor_reduce(out=out, in_=in_, axis=AX.X, op=op), reads=R, writes=W)

    def recip(s, out, in_, R, W):
        return s.S.op("dve", lambda e: e.reciprocal(out=out, in_=in_), reads=R, writes=W)

    def memset(s, ap, val, W, eng="pool"):
        return s.S.op(eng, lambda e: e.memset(ap, val), writes=W)

    def dma(s, q, out, in_, R, W, semkey=None):
        return s.S.dma(q, lambda e: e.dma_start(out=out, in_=in_), reads=R, writes=W, semkey=semkey)

    def rsqrt_col(s, col, R_W, eps, mul=1.0):
        s.ts(col, col, mul, eps, ALU.mult, ALU.add, [R_W], [R_W])
        s.recip(col, col, [R_W], [R_W])
        s.act(col, col, AF.Sqrt, [R_W], [R_W])


def make_consts():
    i = np.arange(128)
    c = {}
    c["ident"] = np.eye(128, dtype=np.float32)
    c["tril_s"] = (i[:, None] > i[None, :]).astype(np.float32)
    c["tril_i"] = (i[:, None] >= i[None, :]).astype(np.float32)
    c["triu_s"] = (i[:, None] < i[None, :]).astype(np.float32)
    c["triu_i"] = (i[:, None] <= i[None, :]).astype(np.float32)
    c["ones"] = np.ones((128, 128), np.float32)
    c["md16"] = ((i[:, None] // 16) == (i[None, :] // 16)).astype(np.float32)
    for s_ in (16, 32, 64):
        bi, bj = i[:, None] // s_, i[None, :] // s_
        c["m%d" % s_] = ((bi % 2 == 1) & (bj == bi - 1)).astype(np.float32)
    c["bones"] = ((i[:, None] // 64) == (i[None, :] // 64)).astype(np.float32)
    hs = np.zeros((128, 128), np.float32); hs[:64, 0] = 1; hs[64:, 1] = 1
    c["headsel"] = hs
    c["iota_f"] = np.broadcast_to(i[None, :], (128, 128)).astype(np.float32).copy()
    c["iota_p"] = np.broadcast_to(i[:, None], (128, 128)).astype(np.float32).copy()
    return c


def declare_inputs(kb):
    c = kb.cfg
    D, T = c.D, c.T
    kb.din("x", [T, D])
    kb.din("cT", [128, c.KC])
    kb.din("w_ada", [D, 6 * D])
    kb.din("b_ada", [1, 6 * D])
    kb.din("n1g", [128, c.KC])
    kb.din("n2g", [1, D])
    kb.din("nfg", [1, D])
    kb.din("w_in", [D, c.INC])
    kb.din("convT", [128, c.CONVC // 128, 4])
    kb.din("a_log", [1, c.HG])
    kb.din("dt_bias", [1, c.HG])
    kb.din("onorm_g", [1, 128])
    for nm in ["mu_rkv"]:
        kb.din(nm, [128, 3 * c.NP])
    kb.din("mu_wd", [96, 1]); kb.din("mu_ad", [96, 1]); kb.din("mu_gd", [128, 2])
    for nm in ["w0", "a0", "k_k", "k_a", "ln_w", "ln_b", "r_k"]:
        kb.din(nm, [128, c.NP])
    kb.din("w0_row", [1, c.RW]); kb.din("lnw_row", [1, c.RW]); kb.din("lnb_row", [1, c.RW])
    kb.din("w_up", [96, c.RW]); kb.din("a_up", [96, c.RW]); kb.din("g_up", [256, c.RW])
    kb.din("w_gdn_o", [c.GW, D]); kb.din("w_rwkv_o", [c.RW, D]); kb.din("w_out", [D, D])
    kb.din("wr", [D, 72]); kb.din("br", [1, 72])
    kb.din("w1", [64 * 128, (D // 128) * c.DE]); kb.din("w3", [64 * 128, (D // 128) * c.DE])
    kb.din("w2", [64 * 128, c.FC * D])
    for k, v in make_consts().items():
        kb.din(k, v.shape)
    kb.din("tokiota", [128, c.TM // 128], I32)
    kb.dout("out", [c.TM, D])
    kb.dscr("mod_d", [128, 6 * D])
    kb.dscr("hT_d", [D, T], BF16)
    kb.dscr("oy_d", [D, T], BF16)
    kb.dscr("xmid_d", [c.TM, D])
    NWc = min(512, D)
    kb.dscr("wga_c", [c.KC, 128, c.KC * 128], BF16); kb.dscr("wgb_c", [c.KC, 128, c.KC * 128], BF16)
    kb.dscr("wgo_c", [c.KC, 128, (c.KC // 2) * 128], BF16); kb.dscr("wro_c", [c.KC, 128, (c.KC // 2) * 128], BF16)
    kb.dscr("wo_c", [D // NWc, 128, c.KC * NWc], BF16)
    kb.dscr("h2_d", [c.TM + 1, D], BF16)
    kb.dscr("yb_d", [c.NBLK * 128, D])
    kb.dscr("tokid_d", [c.NBLK * 128, 1], I32)


def host_inputs(cfg, inp, b, half=0):
    c = cfg
    D = c.D
    f = lambda a: np.ascontiguousarray(a, dtype=np.float32)
    fm = lambda v: f(np.asarray(v).reshape(-1, 128).T)
    m = {}
    m["x"] = f(inp["x"][b])
    m["cT"] = fm(inp["c"][b])
    m["w_ada"] = f(inp["w_ada"][0]); m["b_ada"] = f(inp["b_ada"][0][None, :])
    m["n1g"] = fm(inp["norm1_g"][0]); m["n2g"] = f(inp["norm2_g"][0][None, :]); m["nfg"] = f(inp["norm_f_g"][None, :])
    m["w_in"] = f(inp["w_in"][0])
    cw = np.asarray(inp["conv_w"][0])
    m["convT"] = f(cw.T.reshape(c.CONVC // 128, 128, 4).transpose(1, 0, 2))
    m["a_log"] = f(inp["gdn_a_log"][0][None, :]); m["dt_bias"] = f(inp["gdn_dt_bias"][0][None, :])
    m["onorm_g"] = f(inp["gdn_onorm_g"][0][None, :])
    mu = np.asarray(inp["rwkv_mu"][0])
    m["mu_rkv"] = fm(mu[:3 * c.RW])
    o = 3 * c.RW
    m["mu_wd"] = f(mu[o:o + 96][:, None]); m["mu_ad"] = f(mu[o + 96:o + 192][:, None]); m["mu_gd"] = fm(mu[o + 192:o + 448])
    m["w0"] = fm(inp["rwkv_w0"][0]); m["a0"] = fm(inp["rwkv_a0"][0]); m["k_k"] = fm(inp["rwkv_k_k"][0]); m["k_a"] = fm(inp["rwkv_k_a"][0])
    m["ln_w"] = fm(inp["rwkv_ln_w"][0]); m["ln_b"] = fm(inp["rwkv_ln_b"][0]); m["r_k"] = fm(np.asarray(inp["rwkv_r_k"][0]).reshape(-1))
    m["w0_row"] = f(inp["rwkv_w0"][0][None, :]); m["lnw_row"] = f(inp["rwkv_ln_w"][0][None, :]); m["lnb_row"] = f(inp["rwkv_ln_b"][0][None, :])
    m["w_up"] = f(inp["rwkv_w_up"][0]); m["a_up"] = f(inp["rwkv_a_up"][0]); m["g_up"] = f(inp["rwkv_g_up"][0])
    m["w_gdn_o"] = f(inp["w_gdn_o"][0]); m["w_rwkv_o"] = f(inp["w_rwkv_o"][0]); m["w_out"] = f(inp["w_out"][0])
    m["wr"] = f(np.concatenate([np.asarray(inp["w_group"][0]), np.asarray(inp["w_expert"][0])], axis=1))
    m["br"] = f(np.concatenate([np.asarray(inp["b_group"][0]), np.asarray(inp["b_expert"][0])])[None, :])
    m["w1"] = f(np.asarray(inp["w1"][0]).reshape(64 * 128, -1))
    m["w3"] = f(np.asarray(inp["w3"][0]).reshape(64 * 128, -1))
    m["w2"] = f(np.asarray(inp["w2"][0]).reshape(64 * 128, -1))
    m.update(make_consts())
    m["tokiota"] = (np.arange(c.TM // 128)[None, :] * 128 + np.arange(128)[:, None]).astype(np.int32)
    return m


def load_consts(kb):
    S = kb.S
    kb.C = {}
    for k in ["ident", "tril_s", "tril_i", "triu_s", "triu_i", "ones", "md16", "m16", "m32", "m64", "bones", "headsel", "iota_f", "iota_p"]:
        b = S.sb.alloc([128, 128], F32, k)
        kb.dma("sp", b[:], kb.d[k][:], [], [b])
        kb.C[k] = b
    for k in ["ident", "ones", "triu_i", "triu_s", "bones", "headsel"]:
        b = S.sb.alloc([128, 128], BF16, k + "_bf")
        kb.cp(b[:], kb.C[k][:], [kb.C[k]], [b], eng="pool")
        kb.C[k + "_bf"] = b


def phase_mod(kb):
    c, S, d = kb.cfg, kb.S, kb.d
    KC, D = c.KC, c.D
    S.sb.mark(); S.ps.mark()
    ct = S.sb.alloc([128, KC], F32, "ct")
    cs = S.sb.alloc([128, KC], F32, "cs")
    scr = S.sb.alloc([128, KC, 128], BF16, "scr")
    kb.dma("sp", ct[:], d["cT"][:], [], [ct])
    kb.act(cs[:], ct[:], AF.Silu, [ct], [cs])
    for kc in range(KC):
        kb.ts(scr[:, kc, :], kb.C["ones"][:], cs[:, kc:kc + 1], None, ALU.mult, None, [cs, kb.C["ones"]], [scr], eng="pool")
    MT = 512
    wa = [S.sb.alloc([128, KC, MT], BF16, "wa%d" % i) for i in range(2)]
    bb = [S.sb.alloc([128, MT], F32, "bb%d" % i) for i in range(2)]
    mo = [S.sb.alloc([128, MT], F32, "mo%d" % i) for i in range(2)]
    ps = [S.ps.alloc([128, MT], F32, "psm%d" % i) for i in range(2)]
    wsrc = d["w_ada"].rearrange("(kc p) n -> p kc n", p=128)
    LV = 9
    for m in range(6 * D // MT):
        i = m % 2
        kb.dma("pool", wa[i][:], wsrc[:, :, m * MT:(m + 1) * MT], [], [wa[i]])
        if LV < 2: continue
        kb.dma("sp", bb[i][:], d["b_ada"][0:1, m * MT:(m + 1) * MT].partition_broadcast(128), [], [bb[i]])
        if LV < 3: continue
        for kc in range(KC):
            kb.mm(ps[i][:], scr[:, kc, :], wa[i][:, kc, :], [scr, wa[i]], [ps[i]], start=(kc == 0), stop=(kc == KC - 1), inc=(kc == KC - 1))
        if LV < 4: continue
        kb.tt(mo[i][:], ps[i][:], bb[i][:], ALU.add, [ps[i], bb[i]], [mo[i]])
        if LV < 5: continue
        kb.dma("sp", d["mod_d"][:, m * MT:(m + 1) * MT], mo[i][:], [mo[i]], [kb.K_mod])
    S.sb.release(); S.ps.release()


def diag_extract(kb, dst, dstbuf, seg, tmpbig, tmp):
    c, d = kb.cfg, kb.d
    kb.dma("sp", tmpbig[:], d["mod_d"][:, seg * c.D:(seg + 1) * c.D], [kb.K_mod], [tmpbig])
    for kc in range(c.KC):
        kb.tt(tmp[:], tmpbig[:, kc * 128:(kc + 1) * 128], kb.C["ident"][:], ALU.mult, [tmpbig, kb.C["ident"]], [tmp])
        kb.red(dst[:, kc:kc + 1], tmp[:], ALU.add, [tmp], [dstbuf])


def phase_norm1(kb):
    c, S, d = kb.cfg, kb.S, kb.d
    KC, D, T, TT = c.KC, c.D, c.T, c.TT
    S.sb.mark(); S.ps.mark()
    A1 = S.sb.alloc([128, KC], F32, "A1"); B1 = S.sb.alloc([128, KC], F32, "B1"); g1n = S.sb.alloc([128, KC], F32, "g1n")
    S.sb.mark()
    big = S.sb.alloc([128, D], F32, "big"); tmp = S.sb.alloc([128, 128], F32, "tmp")
    diag_extract(kb, B1, B1, 0, big, tmp)
    diag_extract(kb, A1, A1, 1, big, tmp)
    kb.dma("sp", g1n[:], d["n1g"][:], [], [g1n])
    kb.stt(A1[:], A1[:], 1.0, g1n[:], ALU.add, ALU.mult, [A1, g1n], [A1])
    S.sb.release()
    xt = [S.sb.alloc([128, D], F32, "xt%d" % i) for i in range(2)]
    junk = S.sb.alloc([128, D], BF16, "junk")
    xn = [S.sb.alloc([128, D], BF16, "xn%d" % i) for i in range(2)]
    st = [S.sb.alloc([128, 2], F32, "st%d" % i) for i in range(2)]
    hTt = [S.sb.alloc([128, KC, TT], BF16, "hTt%d" % i) for i in range(2)]
    pst = [S.ps.alloc([128, 4, 128], BF16, "pst%d" % i) for i in range(4)]
    assert len({p.lo // 2048 for p in pst}) == 4
    hdst = d["hT_d"].rearrange("(kc p) t -> p kc t", p=128)
    G = min(4, KC)
    pi = 0
    for n in range(T // 128):
        i = n % 2
        hb = hTt[(n * 128 // TT) % 2]
        toff = (n * 128) % TT
        kb.dma("sp", xt[i][:], d["x"][n * 128:(n + 1) * 128, :], [], [xt[i]])
        kb.act(junk[:], xt[i][:], AF.Square, [xt[i]], [junk, st[i]], accum=st[i][:, 0:1])
        kb.rsqrt_col(st[i][:, 0:1], st[i], 1e-6, 1.0 / D)
        kb.ts(xn[i][:], xt[i][:], st[i][:, 0:1], None, ALU.mult, None, [xt[i], st[i]], [xn[i]])
        for g in range(KC // G):
            p = pst[pi % 4]; pi += 1
            for j in range(G):
                kc = g * G + j
                kb.tr(p[:, j, :], xn[i][:, kc * 128:(kc + 1) * 128], kb.C["ident_bf"][:], [xn[i], kb.C["ident_bf"]], [p], inc=(j == G - 1))
            for j in range(G):
                kc = g * G + j
                kb.act(hb[:, kc, toff:toff + 128], p[:, j, :], AF.Identity, [p, A1, B1], [hb], scale=A1[:, kc:kc + 1], bias=B1[:, kc:kc + 1],
                       eng="act")
        if toff + 128 == TT:
            t0 = n * 128 + 128 - TT
            kb.dma("sp", hdst[:, :, t0:t0 + TT], hb[:], [hb], [kb.K_hT])
    S.sb.release(); S.ps.release()


class Rot:
    def __init__(self, bufs):
        self.bufs, self.i = bufs, 0

    def get(self):
        b = self.bufs[self.i % len(self.bufs)]
        self.i += 1
        return b


def neumann(kb, grp, pt, NR=3):
    C = kb.C
    for m in grp:
        kb.tt(m["L2"][:], m["L"][:], C["md16"][:], ALU.mult, [m["L"], C["md16"]], [m["L2"]], eng="pool")
        kb.tt(m["U2"][:], m["U"][:], C["md16"][:], ALU.mult, [m["U"], C["md16"]], [m["U2"]], eng="pool")
        kb.tt(m["Y"][:], C["ident"][:], m["U2"][:], ALU.subtract, [C["ident"], m["U2"]], [m["Y"]], eng="pool")
    for r in range(NR):
        for m in grp:
            Lk, Uk = (m["L2"], m["U2"]) if r % 2 == 0 else (m["D"], m["DT"])
            Ln, Un = (m["D"], m["DT"]) if r % 2 == 0 else (m["L2"], m["U2"])
            p1 = pt.get()
            kb.mm(p1[:], Uk[:], Lk[:], [Uk, Lk], [p1])
            kb.cp(Ln[:], p1[:], [p1], [Ln], eng="act")
            if r < NR - 1:
                p2 = pt.get()
                kb.mm(p2[:], Lk[:], Uk[:], [Lk, Uk], [p2])
                kb.cp(Un[:], p2[:], [p2], [Un], eng="dve")
        for m in grp:
            Ln = m["D"] if r % 2 == 0 else m["L2"]
            p3 = pt.get()
            kb.mm(p3[:], Ln[:], m["Y"][:], [Ln, m["Y"]], [p3])
            kb.tt(m["Y"][:], m["Y"][:], p3[:], ALU.add, [m["Y"], p3], [m["Y"]])
    for s_ in (16, 32, 64):
        msk = C["m%d" % s_]
        for m in grp:
            kb.tt(m["L2"][:], m["L"][:], msk[:], ALU.mult, [m["L"], msk], [m["L2"]], eng="pool")
            p = pt.get()
            kb.tr(p[:], m["Y"][:], C["ident"][:], [m["Y"], C["ident"]], [p])
            kb.cp(m["D"][:], p[:], [p], [m["D"]], eng="act")
            p = pt.get()
            kb.mm(p[:], m["L2"][:], m["Y"][:], [m["L2"], m["Y"]], [p])
            kb.cp(m["DT"][:], p[:], [p], [m["DT"]], eng="dve")
        for m in grp:
            p = pt.get()
            kb.mm(p[:], m["D"][:], m["DT"][:], [m["D"], m["DT"]], [p])
            kb.tt(m["Y"][:], m["Y"][:], p[:], ALU.subtract, [m["Y"], p], [m["Y"]])


def phase_gdn_pre(kb, heads):
    c, S, d, C = kb.cfg, kb.S, kb.d, kb.C
    KC, T, TT, HG, NT = c.KC, c.T, c.TT, c.HG, c.NT
    P = {}
    for nm in ["g", "beta", "gc", "egc", "bege", "kdec", "egl"]:
        P[nm] = S.sb.alloc([128, HG, NT], F32, "gp_" + nm)
    S.sb.mark(); S.ps.mark()
    ab = S.sb.alloc([128, 2 * HG, NT], F32, "ab")
    wab = S.sb.alloc([128, KC, 2 * HG], BF16, "wab")
    hT = [S.sb.alloc([128, KC, TT], BF16, "hTs%d" % i) for i in range(2)]
    cb = S.sb.alloc([128, 3, HG], F32, "cb")
    pab = [S.ps.alloc([128, 2 * HG], F32, "pab%d" % i) for i in range(2)]
    pbig = S.ps.alloc([128, 512], F32, "pbig")
    kb.dma("pool", wab[:], d["w_in"].rearrange("(kc p) n -> p kc n", p=128)[:, :, c.OFF_A:c.OFF_A + 2 * HG], [], [wab])
    kb.dma("sp", cb[:, 0, :], d["dt_bias"][0:1, :].partition_broadcast(128), [], [cb])
    kb.dma("sp", cb[:, 1, :], d["a_log"][0:1, :].partition_broadcast(128), [], [cb])
    kb.act(cb[:, 2, :], cb[:, 1, :], AF.Exp, [cb], [cb])
    kb.ts(cb[:, 2, :], cb[:, 2, :], -1.0, None, ALU.mult, None, [cb], [cb])
    hsrc = d["hT_d"].rearrange("(kc p) t -> p kc t", p=128)
    for tt in range(T // TT):
        hb = hT[tt % 2]
        kb.dma("sp", hb[:], hsrc[:, :, tt * TT:(tt + 1) * TT], [kb.K_hT], [hb])
        for j in range(TT // 128):
            n = tt * (TT // 128) + j
            p = pab[n % 2]
            for kc in range(KC):
                kb.mm(p[:], hb[:, kc, j * 128:(j + 1) * 128], wab[:, kc, :], [hb, wab], [p], start=(kc == 0), stop=(kc == KC - 1), inc=(kc == KC - 1))
            kb.cp(ab[:, :, n], p[:], [p], [ab], eng="act")
    g, beta = P["g"], P["beta"]
    for h in range(HG):
        kb.act(g[:, h, :], ab[:, h, :], AF.Exp, [ab, cb], [g], bias=cb[:, 0, h:h + 1])
        kb.act(g[:, h, :], g[:, h, :], AF.Ln, [g], [g], bias=1.0)
        kb.ts(g[:, h, :], g[:, h, :], cb[:, 2, h:h + 1], None, ALU.mult, None, [g, cb], [g])
    kb.act(beta[:].rearrange("p h n -> p (h n)"), ab[:, HG:2 * HG, :].rearrange("p h n -> p (h n)"), AF.Sigmoid, [ab], [beta])
    N = HG * NT
    assert N <= 512
    gf = lambda b: b[:].rearrange("p h n -> p (h n)")
    kb.mm(pbig[:, 0:N], C["triu_i"][:], gf(g), [C["triu_i"], g], [pbig])
    kb.cp(gf(P["gc"]), pbig[:, 0:N], [pbig], [P["gc"]], eng="act")
    kb.mm(pbig[:, 0:N], C["ones"][:], gf(g), [C["ones"], g], [pbig])
    kb.act(gf(P["egl"]), pbig[:, 0:N], AF.Exp, [pbig], [P["egl"]])
    kb.tt(gf(P["kdec"]), pbig[:, 0:N], gf(P["gc"]), ALU.subtract, [pbig, P["gc"]], [P["kdec"]])
    kb.act(gf(P["kdec"]), gf(P["kdec"]), AF.Exp, [P["kdec"]], [P["kdec"]])
    kb.act(gf(P["egc"]), gf(P["gc"]), AF.Exp, [P["gc"]], [P["egc"]])
    kb.tt(gf(P["bege"]), gf(P["egc"]), gf(beta), ALU.mult, [P["egc"], beta], [P["bege"]])
    S.sb.release(); S.ps.release()
    return P


def inproj_groups(kb, cols, hT, wbufs, emit_evac, pbanks):
    c, d = kb.cfg, kb.d
    KC, T, TT = c.KC, c.T, c.TT
    wsrc = d["w_in"].rearrange("(kc p) n -> p kc n", p=128)
    for gi, co in enumerate(cols):
        kb.dma("pool", wbufs[gi][:], wsrc[:, :, co:co + 128], [], [wbufs[gi]])
    hsrc = d["hT_d"].rearrange("(kc p) t -> p kc t", p=128)
    pi = 0
    for tt in range(T // TT):
        hb = hT[tt % 2]
        kb.dma("sp", hb[:], hsrc[:, :, tt * TT:(tt + 1) * TT], [kb.K_hT], [hb])
        for gi in range(len(cols)):
            p = pbanks[pi % len(pbanks)]; pi += 1
            for kc in range(KC):
                kb.mm(p[:, 0:TT], wbufs[gi][:, kc, :], hb[:, kc, :], [wbufs[gi], hb], [p], start=(kc == 0), stop=(kc == KC - 1), inc=(kc == KC - 1))
            emit_evac(gi, tt, p)


def phase_gdn(kb, heads, P):
    c, S, d, C = kb.cfg, kb.S, kb.d, kb.C
    KC, T, TT, HG, NT = c.KC, c.T, c.TT, c.HG, c.NT
    S.sb.mark(); S.ps.mark()
    gon = S.sb.alloc([128, 128], F32, "gon")
    kb.dma("sp", gon[:], d["onorm_g"][0:1, :].partition_broadcast(128), [], [gon])
    qf = S.sb.alloc([128, T], BF16, "qf"); kf = S.sb.alloc([128, T], BF16, "kf")
    vf = S.sb.alloc([128, T], BF16, "vf"); gzf = S.sb.alloc([128, T], BF16, "gzf")
    of = S.sb.alloc([128, T], BF16, "of")
    Sst = S.sb.alloc([128, 128], F32, "Sst"); Sbf = S.sb.alloc([128, 128], BF16, "Sbf")
    for hi, h in enumerate(heads):
        S.sb.mark(); S.ps.mark()
        wb = [S.sb.alloc([128, KC, 128], BF16, "wg%d" % i) for i in range(4)]
        hT = [S.sb.alloc([128, KC, TT], BF16, "hTg%d" % i) for i in range(2)]
        zc = [S.sb.alloc([128, 3 + TT], F32, "zc%d" % i) for i in range(3)]
        cw = S.sb.alloc([128, 3, 4], F32, "cw")
        acc = [S.sb.alloc([128, TT], F32, "acc%d" % i) for i in range(2)]
        sq = S.sb.alloc([128, TT], BF16, "sq"); rn = S.sb.alloc([128, TT], F32, "rn")
        pb = [S.ps.alloc([128, 512], F32, "pg%d" % i) for i in range(5)]
        pn = [S.ps.alloc([128, 512], F32, "pn%d" % i) for i in range(2)]
        cols = [h * 128, c.GW + h * 128, 2 * c.GW + h * 128, c.OFF_Z + h * 128]
        for i in range(3):
            kb.dma("sp", cw[:, i, :], d["convT"][:, cols[i] // 128, :], [], [cw])
            kb.memset(zc[i][:, 0:3], 0.0, [zc[i]])
        dst = [qf, kf, vf]
        scale_q = 128.0 ** -0.5

        def evac(gi, tt, p):
            t0 = tt * TT
            if gi == 3:
                kb.act(gzf[:, t0:t0 + TT], p[:, 0:TT], AF.Silu, [p], [gzf])
                return
            z = zc[gi]
            kb.cp(z[:, 3:3 + TT], p[:, 0:TT], [p], [z], eng="act")
            a = acc[gi % 2]
            kb.ts(a[:], z[:, 3:3 + TT], cw[:, gi, 3:4], None, ALU.mult, None, [z, cw], [a])
            for i in range(3):
                kb.stt(a[:], z[:, i:i + TT], cw[:, gi, i:i + 1], a[:], ALU.mult, ALU.add, [z, cw, a], [a])
            kb.cp(z[:, 0:3], z[:, TT:TT + 3], [z], [z], eng="pool")
            if gi == 2:
                kb.act(vf[:, t0:t0 + TT], a[:], AF.Silu, [a], [vf])
                return
            kb.act(a[:], a[:], AF.Silu, [a], [a])
            kb.act(sq[:], a[:], AF.Square, [a], [sq])
            pp = pn[gi % 2]
            kb.mm(pp[:, 0:TT], C["ones_bf"][:], sq[:], [C["ones_bf"], sq], [pp])
            kb.ts(rn[:], pp[:, 0:TT], 1.0, 1e-6, ALU.mult, ALU.add, [pp], [rn])
            kb.recip(rn[:], rn[:], [rn], [rn])
            kb.act(rn[:], rn[:], AF.Sqrt, [rn], [rn])
            if gi == 0:
                kb.stt(dst[gi][:, t0:t0 + TT], a[:], scale_q, rn[:], ALU.mult, ALU.mult, [a, rn], [dst[gi]])
            else:
                kb.tt(dst[gi][:, t0:t0 + TT], a[:], rn[:], ALU.mult, [a, rn], [dst[gi]])
        GL = 9
        if GL >= 1: inproj_groups(kb, cols, hT, wb, evac, pb)
        S.sb.release(); S.ps.release()
        if GL < 2: return
        S.sb.mark(); S.ps.mark()
        ktm = S.sb.alloc([128, NT, 128], BF16, "ktm"); vbt = S.sb.alloc([128, NT, 128], BF16, "vbt")
        kbe = S.sb.alloc([128, NT, 128], BF16, "kbe")
        wT = S.sb.alloc([128, NT, 128], BF16, "wT"); u = S.sb.alloc([128, NT, 128], F32, "u")
        aT = S.sb.alloc([128, NT, 128], BF16, "aT")
        G = min(8, NT)
        grp = []
        for i in range(G):
            m = {k: S.sb.alloc([128, 128], F32, "%s%d" % (k, i)) for k in ["L", "U", "Y", "L2", "U2", "gb", "D", "DT"]}
            m["Ybf"] = S.sb.alloc([128, 128], BF16, "Ybf%d" % i)
            grp.append(m)
        pt = Rot([S.ps.alloc([128, 128], F32, "pt%d" % i) for i in range(6)])
        ptb = Rot([S.ps.alloc([128, 128], BF16, "ptb%d" % i) for i in range(2)])
        gcol = lambda nm, n: P[nm][:, h, n:n + 1]
        for n0 in range(0, NT, G):
            ns = list(range(n0, min(NT, n0 + G)))
            for i, n in enumerate(ns):
                m = grp[i]
                cs = slice(n * 128, (n + 1) * 128)
                p = ptb.get()
                kb.tr(p[:], kf[:, cs], C["ident_bf"][:], [kf, C["ident_bf"]], [p])
                kb.cp(ktm[:, n, :], p[:], [p], [ktm], eng="act")
                kb.ts(kbe[:, n, :], p[:], gcol("bege", n), None, ALU.mult, None, [p, P["bege"]], [kbe])
                p = ptb.get()
                kb.tr(p[:], vf[:, cs], C["ident_bf"][:], [vf, C["ident_bf"]], [p])
                kb.ts(vbt[:, n, :], p[:], gcol("beta", n), None, ALU.mult, None, [p, P["beta"]], [vbt])
                kb.ts(m["gb"][:], C["ones"][:], gcol("g", n), None, ALU.mult, None, [C["ones"], P["g"]], [m["gb"]], eng="pool")
                p = pt.get()
                kb.mm(p[:], m["gb"][:], C["triu_i"][:], [m["gb"], C["triu_i"]], [p])
                kb.ts(m["D"][:], p[:], gcol("gc", n), 0.0, ALU.subtract, ALU.max, [p, P["gc"]], [m["D"]])
                kb.ts(m["DT"][:], p[:], gcol("gc", n), 0.0, ALU.subtract, ALU.min, [p, P["gc"]], [m["DT"]])
                kb.act(m["D"][:], m["D"][:], AF.Exp, [m["D"]], [m["D"]], scale=-1.0)
                kb.act(m["DT"][:], m["DT"][:], AF.Exp, [m["DT"]], [m["DT"]])
                kb.tt(m["D"][:], m["D"][:], C["tril_s"][:], ALU.mult, [m["D"], C["tril_s"]], [m["D"]], eng="pool")
                kb.tt(m["DT"][:], m["DT"][:], C["triu_i"][:], ALU.mult, [m["DT"], C["triu_i"]], [m["DT"]], eng="pool")
                p = pt.get()
                kb.mm(p[:], kf[:, cs], kf[:, cs], [kf], [p])
                kb.stt(m["L"][:], p[:], gcol("beta", n), m["D"][:], ALU.mult, ALU.mult, [p, P["beta"], m["D"]], [m["L"]])
                p = pt.get()
                kb.tr(p[:], m["L"][:], C["ident"][:], [m["L"], C["ident"]], [p])
                kb.cp(m["U"][:], p[:], [p], [m["U"]], eng="act")
                p = pt.get()
                kb.mm(p[:], kf[:, cs], qf[:, cs], [kf, qf], [p])
                kb.tt(aT[:, n, :], p[:], m["DT"][:], ALU.mult, [p, m["DT"]], [aT])
            if GL < 3: continue
            neumann(kb, grp[:len(ns)], pt)
            if GL < 4: continue
            for i, n in enumerate(ns):
                m = grp[i]
                kb.cp(m["Ybf"][:], m["Y"][:], [m["Y"]], [m["Ybf"]], eng="pool")
                p = pt.get()
                kb.mm(p[:], kbe[:, n, :], m["Ybf"][:], [kbe, m["Ybf"]], [p])
                kb.cp(wT[:, n, :], p[:], [p], [wT], eng="act")
                p = pt.get()
                kb.mm(p[:], m["Ybf"][:], vbt[:, n, :], [m["Ybf"], vbt], [p])
                kb.cp(u[:, n, :], p[:], [p], [u], eng="dve")
        if GL < 5: return
        vn = S.sb.alloc([128, 128], F32, "vn"); vnb = S.sb.alloc([128, 128], BF16, "vnb"); vns = S.sb.alloc([128, 128], BF16, "vns")
        o1 = S.sb.alloc([128, 128], F32, "o1"); o = S.sb.alloc([128, 128], F32, "o"); onb = S.sb.alloc([128, 128], BF16, "onb")
        junk = S.sb.alloc([128, 128], BF16, "junkg"); st = S.sb.alloc([128, 2], F32, "stg")
        kb.memset(Sst[:], 0.0, [Sst]); kb.memset(Sbf[:], 0.0, [Sbf])
        for n in range(NT):
            cs = slice(n * 128, (n + 1) * 128)
            pw = pt.get(); pq = pt.get()
            kb.mm(pw[:], wT[:, n, :], Sbf[:], [wT, Sbf], [pw])
            kb.mm(pq[:], qf[:, cs], Sbf[:], [qf, Sbf], [pq])
            kb.tt(vn[:], u[:, n, :], pw[:], ALU.subtract, [u, pw], [vn])
            kb.cp(vnb[:], vn[:], [vn], [vnb], eng="act")
            kb.ts(vns[:], vn[:], gcol("kdec", n), None, ALU.mult, None, [vn, P["kdec"]], [vns])
            pa = pt.get(); psu = pt.get()
            kb.mm(pa[:], aT[:, n, :], vnb[:], [aT, vnb], [pa])
            kb.mm(psu[:], ktm[:, n, :], vns[:], [ktm, vns], [psu])
            kb.stt(Sst[:], Sst[:], gcol("egl", n), psu[:], ALU.mult, ALU.add, [Sst, P["egl"], psu], [Sst])
            kb.cp(Sbf[:], Sst[:], [Sst], [Sbf], eng="act")
            kb.cp(o1[:], pa[:], [pa], [o1], eng="act")
            kb.stt(o[:], pq[:], gcol("egc", n), o1[:], ALU.mult, ALU.add, [pq, P["egc"], o1], [o])
            kb.act(junk[:], o[:], AF.Square, [o], [junk, st], accum=st[:, 0:1])
            kb.rsqrt_col(st[:, 0:1], st, 1e-6, 1.0 / 128)
            kb.stt(onb[:], o[:], st[:, 0:1], gon[:], ALU.mult, ALU.mult, [o, st, gon], [onb])
            p = ptb.get()
            kb.tr(p[:], onb[:], C["ident_bf"][:], [onb, C["ident_bf"]], [p])
            kb.tt(of[:, cs], p[:], gzf[:, cs], ALU.mult, [p, gzf], [of])
        kb.dma("sp", d["oy_d"][h * 128:(h + 1) * 128, :], of[:], [of], [kb.K_oy])
        S.sb.release(); S.ps.release()
    S.sb.release(); S.ps.release()


def phase_rwkv_pre(kb):
    c, S, d, C = kb.cfg, kb.S, kb.d, kb.C
    KC, T, TT = c.KC, c.T, c.TT
    L = {}
    L["twd"] = S.sb.alloc([128, T], BF16, "twd"); L["ads"] = S.sb.alloc([128, T], BF16, "ads")
    L["sgd"] = S.sb.alloc([128, 2, T], BF16, "sgd")
    S.sb.mark(); S.ps.mark()
    base = c.OFF_R + 3 * c.RW
    groups = [(base, 96), (base + 96, 96), (base + 192, 128), (base + 320, 128)]
    wb = [S.sb.alloc([128, KC, 128], BF16, "wl%d" % i) for i in range(4)]
    hT = [S.sb.alloc([128, KC, TT], BF16, "hTl%d" % i) for i in range(2)]
    zr = [S.sb.alloc([128, 1 + TT], F32, "zl%d" % i) for i in range(4)]
    tmp = S.sb.alloc([128, TT], F32, "tl")
    mu = S.sb.alloc([128, 4], F32, "mul")
    pb = [S.ps.alloc([128, 512], F32, "pl%d" % i) for i in range(4)]
    kb.dma("sp", mu[0:96, 0:1], d["mu_wd"][:, :], [], [mu]); kb.dma("sp", mu[0:96, 1:2], d["mu_ad"][:, :], [], [mu])
    kb.dma("sp", mu[:, 2:4], d["mu_gd"][:, :], [], [mu])
    wsrc = d["w_in"].rearrange("(kc p) n -> p kc n", p=128)
    for gi, (co, w) in enumerate(groups):
        kb.dma("pool", wb[gi][:, :, 0:w], wsrc[:, :, co:co + w], [], [wb[gi]])
        kb.memset(zr[gi][:, 0:1], 0.0, [zr[gi]])
    hsrc = d["hT_d"].rearrange("(kc p) t -> p kc t", p=128)
    for tt in range(T // TT):
        hb = hT[tt % 2]
        t0 = tt * TT
        kb.dma("sp", hb[:], hsrc[:, :, t0:t0 + TT], [kb.K_hT], [hb])
        for gi, (co, w) in enumerate(groups):
            p = pb[gi]
            for kc in range(KC):
                kb.mm(p[0:w, 0:TT], wb[gi][:, kc, 0:w], hb[:, kc, :], [wb[gi], hb], [p], start=(kc == 0), stop=(kc == KC - 1), inc=(kc == KC - 1))
            z = zr[gi]
            kb.cp(z[0:w, 1:1 + TT], p[0:w, 0:TT], [p], [z], eng="act")
            kb.tt(tmp[0:w, :], z[0:w, 0:TT], z[0:w, 1:1 + TT], ALU.subtract, [z], [tmp])
            kb.stt(tmp[0:w, :], tmp[0:w, :], mu[0:w, gi:gi + 1], z[0:w, 1:1 + TT], ALU.mult, ALU.add, [tmp, mu, z], [tmp])
            kb.cp(z[0:w, 0:1], z[0:w, TT:TT + 1], [z], [z], eng="pool")
            if gi == 0:
                kb.act(L["twd"][0:96, t0:t0 + TT], tmp[0:96, :], AF.Tanh, [tmp], [L["twd"]])
            elif gi == 1:
                kb.cp(L["ads"][0:96, t0:t0 + TT], tmp[0:96, :], [tmp], [L["ads"]], eng="act")
            else:
                kb.act(L["sgd"][:, gi - 2, t0:t0 + TT], tmp[:], AF.Sigmoid, [tmp], [L["sgd"]])
    S.sb.release(); S.ps.release()
    return L


def phase_rwkv(kb, pairs, L):
    c, S, d, C = kb.cfg, kb.S, kb.d, kb.C
    KC, T, TT, NT, RW = c.KC, c.T, c.TT, c.NT, c.RW
    S.sb.mark(); S.ps.mark()
    rf = S.sb.alloc([128, T], F32, "rf"); kf = S.sb.alloc([128, T], F32, "kfr"); vf = S.sb.alloc([128, T], F32, "vfr")
    yf = S.sb.alloc([128, T], BF16, "yf")
    prmall = S.sb.alloc([128, 8, c.NP], F32, "prmall")
    for j, nm in enumerate(["w0", "a0", "k_k", "k_a", "r_k"]):
        kb.dma("sp", prmall[:, j, :], d[nm][:, :], [], [prmall])
    kb.dma("sp", prmall[:, 5:8, :], d["mu_rkv"].rearrange("p (j n) -> p j n", j=3), [], [prmall])
    prm_ = prmall
    bc = S.sb.alloc([128, 3, 128], F32, "bcr")
    wup = S.sb.alloc([128, 128], BF16, "wup"); aup = S.sb.alloc([128, 128], BF16, "aup"); gup = S.sb.alloc([128, 2, 128], BF16, "gup")
    H = S.sb.alloc([128, 64], F32, "Hst"); Hbf = S.sb.alloc([128, 64], BF16, "Hbf")
    for pr in pairs:
        ch0 = pr * 128
        class _P:
            def __getitem__(self, k):
                rows, cols = k
                return prmall[rows, cols.start, pr:pr + 1]
        prm = _P()
        for j, nm in enumerate(["w0_row", "lnw_row", "lnb_row"]):
            kb.dma("sp", bc[:, j, :], d[nm][0:1, ch0:ch0 + 128].partition_broadcast(128), [], [bc])
        kb.dma("pool", wup[0:96, :], d["w_up"][:, ch0:ch0 + 128], [], [wup])
        kb.dma("pool", aup[0:96, :], d["a_up"][:, ch0:ch0 + 128], [], [aup])
        kb.dma("pool", gup[:], d["g_up"].rearrange("(kc p) n -> p kc n", p=128)[:, :, ch0:ch0 + 128], [], [gup])
        S.sb.mark(); S.ps.mark()
        wb = [S.sb.alloc([128, KC, 128], BF16, "wr%d" % i) for i in range(3)]
        _h = S.sb.alloc([128, KC, TT], BF16, "hTr0")
        hT = [_h, _h]
        zr = [S.sb.alloc([128, 1 + TT], F32, "zr%d" % i) for i in range(3)]
        tmp = S.sb.alloc([128, TT], F32, "tr")
        pb = [S.ps.alloc([128, 512], F32, "pr%d" % i) for i in range(6)]
        cols = [c.OFF_R + j * RW + ch0 for j in range(3)]
        for i in range(3):
            kb.memset(zr[i][:, 0:1], 0.0, [zr[i]])
        dst = [rf, kf, vf]

        def evac(gi, tt, p):
            t0 = tt * TT
            z = zr[gi]
            kb.cp(z[:, 1:1 + TT], p[:, 0:TT], [p], [z], eng="act")
            kb.tt(tmp[:], z[:, 0:TT], z[:, 1:1 + TT], ALU.subtract, [z], [tmp])
            kb.stt(dst[gi][:, t0:t0 + TT], tmp[:], prm[:, 5 + gi:6 + gi], z[:, 1:1 + TT], ALU.mult, ALU.add, [tmp, prmall, z], [dst[gi]])
            kb.cp(z[:, 0:1], z[:, TT:TT + 1], [z], [z], eng="pool")
        inproj_groups(kb, cols, hT, wb, evac, pb)
        S.sb.release(); S.ps.release()
        S.sb.mark(); S.ps.mark()
        fA = {k: S.sb.alloc([128, TT], F32, "f_" + k) for k in ["alr", "G", "Gx", "t1", "t2", "kk", "ke"]}
        bA = {k: S.sb.alloc([128, TT], BF16, "b_" + k) for k in ["Rh", "Ah", "Kh", "Bh", "Kt", "Bt", "Pb", "vb"]}
        lwt = S.sb.alloc([128, 128], F32, "lwt")
        CPT = TT // 128
        members = []
        for j_ in range(CPT):
            row = []
            for i in range(2):
                m = {k: S.sb.alloc([128, 128], F32, "r%s%d_%d" % (k, i, j_)) for k in ["L", "U", "Y", "L2", "U2", "D", "DT"]}
                for k in ["Ybf", "AakT", "ArbT", "ArkT"]:
                    m[k] = S.sb.alloc([128, 128], BF16, "r%s%d_%d" % (k, i, j_))
                m["AKV"] = S.sb.alloc([128, 64], BF16, "rAKV%d_%d" % (i, j_)); m["U2v"] = S.sb.alloc([128, 64], F32, "rU2v%d_%d" % (i, j_))
                m["Ub"] = S.sb.alloc([128, 64], BF16, "rUb%d_%d" % (i, j_))
                row.append(m)
            members.append(row)
        VtmL = [S.sb.alloc([128, 128], BF16, "Vtm%d" % j_) for j_ in range(CPT)]
        AtmL = [S.sb.alloc([128, 128], BF16, "Atm%d" % j_) for j_ in range(CPT)]
        BttmL = [S.sb.alloc([128, 128], BF16, "Bttm%d" % j_) for j_ in range(CPT)]
        KttmL = [S.sb.alloc([128, 128], BF16, "Kttm%d" % j_) for j_ in range(CPT)]
        WTL = [S.sb.alloc([128, 128], BF16, "WTr%d" % j_) for j_ in range(CPT)]
        Yp = S.sb.alloc([128, 128], F32, "Yp"); Yo = S.sb.alloc([128, 128], BF16, "Yo")
        st = S.sb.alloc([128, 8], F32, "str"); junk = S.sb.alloc([128, 128], BF16, "junkr")
        eGl = S.sb.alloc([128, NT], F32, "eGl")
        pt = Rot([S.ps.alloc([128, 128], F32, "qt%d" % i) for i in range(6)])
        ptb = Rot([S.ps.alloc([128, 128], BF16, "qtb%d" % i) for i in range(2)])
        kb.memset(H[:], 0.0, [H]); kb.memset(Hbf[:], 0.0, [Hbf])
        CPT = TT // 128
        for tt in range(T // TT):
            t0 = tt * TT
            ts_ = slice(t0, t0 + TT)
            p = pt.get()
            pbig = p
            for j in range(CPT):
                cs = slice(t0 + j * 128, t0 + (j + 1) * 128)
                js = slice(j * 128, (j + 1) * 128)
                n = tt * CPT + j
                p = pt.get()
                kb.mm(p[:], aup[0:96, :], L["ads"][0:96, cs], [aup, L["ads"]], [p])
                kb.act(fA["alr"][:, js], p[:], AF.Sigmoid, [p, prmall], [fA["alr"]], bias=prm[:, 1:2])
                p = pt.get()
                kb.mm(p[:], L["twd"][0:96, cs], wup[0:96, :], [L["twd"], wup], [p])
                kb.tt(lwt[:], p[:], bc[:, 0, :], ALU.add, [p, bc], [lwt])
                kb.act(lwt[:], lwt[:], AF.Sigmoid, [lwt], [lwt])
                kb.ts(lwt[:], lwt[:], -0.6065306597126334, None, ALU.mult, None, [lwt], [lwt])
                p = pt.get()
                kb.mm(p[:], lwt[:], C["triu_i"][:], [lwt, C["triu_i"]], [p])
                kb.cp(fA["G"][:, js], p[:], [p], [fA["G"]], eng="act")
                p = pt.get()
                kb.mm(p[:], lwt[:], C["triu_s"][:], [lwt, C["triu_s"]], [p])
                kb.cp(fA["Gx"][:, js], p[:], [p], [fA["Gx"]], eng="act")
                kb.act(fA["t2"][:, js], fA["G"][:, js], AF.Exp, [fA["G"]], [fA["t2"]], scale=-1.0, bias=fA["G"][:, j * 128 + 127:j * 128 + 128])
                kb.act(eGl[:, n:n + 1], fA["G"][:, j * 128 + 127:j * 128 + 128], AF.Exp, [fA["G"]], [eGl])
            alr, G, Gx, t1, t2, kk, ke = [fA[k] for k in ["alr", "G", "Gx", "t1", "t2", "kk", "ke"]]
            kb.ts(kk[:], kf[:, ts_], prm[:, 2:3], None, ALU.mult, None, [kf, prmall], [kk])
            kb.act(bA["Pb"][:], kk[:], AF.Square, [kk], [bA["Pb"]])
            for j in range(CPT):
                js = slice(j * 128, (j + 1) * 128)
                p = pt.get()
                kb.mm(p[:], C["bones_bf"][:], bA["Pb"][:, js], [C["bones_bf"], bA["Pb"]], [p])
                kb.ts(t1[:, js], p[:], 1.0, 1e-6, ALU.mult, ALU.add, [p], [t1])
            kb.recip(t1[:], t1[:], [t1], [t1])
            kb.act(t1[:], t1[:], AF.Sqrt, [t1], [t1])
            kb.tt(kk[:], kk[:], t1[:], ALU.mult, [kk, t1], [kk])
            kb.ts(ke[:], alr[:], -1.0, prm[:, 3:4], ALU.add, ALU.mult, [alr, prmall], [ke])
            kb.stt(ke[:], ke[:], 1.0, kf[:, ts_], ALU.add, ALU.mult, [ke, kf], [ke])
            kb.tt(bA["Kt"][:], ke[:], t2[:], ALU.mult, [ke, t2], [bA["Kt"]])
            kb.tt(t1[:], kk[:], alr[:], ALU.mult, [kk, alr], [t1])
            kb.tt(bA["Bt"][:], t1[:], t2[:], ALU.mult, [t1, t2], [bA["Bt"]])
            kb.stt(bA["Pb"][:], rf[:, ts_], prm[:, 4:5], ke[:], ALU.mult, ALU.mult, [rf, prmall, ke], [bA["Pb"]])
            kb.act(t2[:], G[:], AF.Exp, [G], [t2], scale=-1.0)
            kb.tt(bA["Kh"][:], ke[:], t2[:], ALU.mult, [ke, t2], [bA["Kh"]])
            kb.tt(bA["Bh"][:], t1[:], t2[:], ALU.mult, [t1, t2], [bA["Bh"]])
            kb.act(t2[:], G[:], AF.Exp, [G], [t2])
            kb.tt(bA["Rh"][:], rf[:, ts_], t2[:], ALU.mult, [rf, t2], [bA["Rh"]])
            kb.act(t2[:], Gx[:], AF.Exp, [Gx], [t2])
            kb.stt(bA["Ah"][:], kk[:], -1.0, t2[:], ALU.mult, ALU.mult, [kk, t2], [bA["Ah"]])
            kb.cp(bA["vb"][:], vf[:, ts_], [vf], [bA["vb"]], eng="pool")
            for j in range(CPT):
                n = tt * CPT + j
                js = slice(j * 128, (j + 1) * 128)
                cs = slice(t0 + j * 128, t0 + (j + 1) * 128)
                Rh, Ah, Kh, Bh, Kt, Bt, Pb, vb = [bA[k] for k in ["Rh", "Ah", "Kh", "Bh", "Kt", "Bt", "Pb", "vb"]]
                Vtm, Atm, Bttm, Kttm, WT = VtmL[j], AtmL[j], BttmL[j], KttmL[j], WTL[j]
                mem_j = members[j]
                for (src, dstb) in [(vb, Vtm), (Ah, Atm), (Bt, Bttm), (Kt, Kttm)]:
                    p = ptb.get()
                    kb.tr(p[:], src[:, js], C["ident_bf"][:], [src, C["ident_bf"]], [p])
                    kb.cp(dstb[:], p[:], [p], [dstb], eng="act")
                for hh in range(2):
                    m = mem_j[hh]
                    ps_ = slice(hh * 64, hh * 64 + 64)
                    p = pt.get()
                    kb.mm(p[:], Bh[ps_, js], Ah[ps_, js], [Bh, Ah], [p])
                    kb.stt(m["U"][:], p[:], -1.0, C["triu_s"][:], ALU.mult, ALU.mult, [p, C["triu_s"]], [m["U"]])
                    p = pt.get()
                    kb.mm(p[:], Ah[ps_, js], Bh[ps_, js], [Bh, Ah], [p])
                    kb.stt(m["L"][:], p[:], -1.0, C["tril_s"][:], ALU.mult, ALU.mult, [p, C["tril_s"]], [m["L"]])
                    p = pt.get()
                    kb.mm(p[:], Kh[ps_, js], Ah[ps_, js], [Kh, Ah], [p])
                    kb.tt(m["AakT"][:], p[:], C["triu_s"][:], ALU.mult, [p, C["triu_s"]], [m["AakT"]])
                    p = pt.get()
                    kb.mm(p[:], Bh[ps_, js], Rh[ps_, js], [Bh, Rh], [p])
                    kb.tt(m["ArbT"][:], p[:], C["triu_i"][:], ALU.mult, [p, C["triu_i"]], [m["ArbT"]])
                    p = pt.get()
                    kb.mm(p[:], Kh[ps_, js], Rh[ps_, js], [Kh, Rh], [p])
                    kb.tt(m["ArkT"][:], p[:], C["triu_i"][:], ALU.mult, [p, C["triu_i"]], [m["ArkT"]])
            neumann(kb, [m_ for row_ in members for m_ in row_], pt)
            for j in range(CPT):
                n = tt * CPT + j
                js = slice(j * 128, (j + 1) * 128)
                cs = slice(t0 + j * 128, t0 + (j + 1) * 128)
                Rh, Ah, Kh, Bh, Kt, Bt, Pb, vb = [bA[k] for k in ["Rh", "Ah", "Kh", "Bh", "Kt", "Bt", "Pb", "vb"]]
                Vtm, Atm, Bttm, Kttm, WT = VtmL[j], AtmL[j], BttmL[j], KttmL[j], WTL[j]
                mem_j = members[j]
                for hh in range(2):
                    m = mem_j[hh]
                    hs = slice(hh * 64, hh * 64 + 64)
                    kb.cp(m["Ybf"][:], m["Y"][:], [m["Y"]], [m["Ybf"]], eng="pool")
                    p = pt.get()
                    kb.mm(p[:, 0:64], m["AakT"][:], Vtm[:, hs], [m["AakT"], Vtm], [p])
                    kb.cp(m["AKV"][:], p[:, 0:64], [p], [m["AKV"]], eng="act")
                    p = pt.get()
                    kb.mm(p[:, 0:64], m["Ybf"][:], m["AKV"][:], [m["Ybf"], m["AKV"]], [p])
                    kb.cp(m["U2v"][:], p[:, 0:64], [p], [m["U2v"]], eng="dve")
                    p = pt.get()
                    kb.mm(p[:], Atm[:], m["Ybf"][:], [Atm, m["Ybf"]], [p])
                    kb.cp(WT[hs, :], p[hs, :], [p], [WT], eng="act")
            for j in range(CPT):
                n = tt * CPT + j
                js = slice(j * 128, (j + 1) * 128)
                cs = slice(t0 + j * 128, t0 + (j + 1) * 128)
                Rh, Ah, Kh, Bh, Kt, Bt, Pb, vb = [bA[k] for k in ["Rh", "Ah", "Kh", "Bh", "Kt", "Bt", "Pb", "vb"]]
                Vtm, Atm, Bttm, Kttm, WT = VtmL[j], AtmL[j], BttmL[j], KttmL[j], WTL[j]
                mem_j = members[j]
                HS = [slice(0, 64), slice(64, 128)]
                pus, pys, phs = [None, None], [None, None], [None, None]
                for hh in range(2):
                    m = mem_j[hh]; hs = HS[hh]
                    pus[hh] = pt.get()
                    kb.mm(pus[hh][:, 0:64], WT[hs, :], Hbf[hs, :], [WT, Hbf], [pus[hh]])
                for hh in range(2):
                    m = mem_j[hh]; hs = HS[hh]
                    kb.tt(m["Ub"][:], pus[hh][:, 0:64], m["U2v"][:], ALU.add, [pus[hh], m["U2v"]], [m["Ub"]])
                for hh in range(2):
                    m = mem_j[hh]; hs = HS[hh]
                    py = pt.get(); pys[hh] = py
                    kb.mm(py[:, 0:64], Rh[hs, js], Hbf[hs, :], [Rh, Hbf], [py], start=True, stop=False, inc=False)
                    kb.mm(py[:, 0:64], m["ArkT"][:], Vtm[:, hs], [m["ArkT"], Vtm], [py], start=False, stop=False, inc=False)
                    kb.mm(py[:, 0:64], m["ArbT"][:], m["Ub"][:], [m["ArbT"], m["Ub"]], [py], start=False, stop=True)
                    ph = pt.get(); phs[hh] = ph
                    kb.mm(ph[:, 0:64], Kttm[:], Vtm[:, hs], [Kttm, Vtm], [ph], start=True, stop=False, inc=False)
                    kb.mm(ph[:, 0:64], Bttm[:], m["Ub"][:], [Bttm, m["Ub"]], [ph], start=False, stop=True)
                for hh in range(2):
                    hs = HS[hh]
                    kb.stt(H[hs, :], H[hs, :], eGl[hs, n:n + 1], phs[hh][hs, 0:64], ALU.mult, ALU.add, [H, eGl, phs[hh]], [H])
                    kb.cp(Hbf[hs, :], H[hs, :], [H], [Hbf], eng="act")
                    kb.cp(Yp[:, hs], pys[hh][:, 0:64], [pys[hh]], [Yp], eng="act")
                for hh in range(2):
                    hs = slice(hh * 64, hh * 64 + 64)
                    kb.red(st[:, 0:1], Yp[:, hs], ALU.add, [Yp], [st])
                    kb.act(junk[:, 0:64], Yp[:, hs], AF.Square, [Yp], [junk, st], accum=st[:, 1:2])
                    kb.ts(st[:, 0:2], st[:, 0:2], 1.0 / 64, None, ALU.mult, None, [st], [st])
                    kb.tt(st[:, 2:3], st[:, 0:1], st[:, 0:1], ALU.mult, [st], [st])
                    kb.tt(st[:, 2:3], st[:, 1:2], st[:, 2:3], ALU.subtract, [st], [st])
                    kb.rsqrt_col(st[:, 2:3], st, 64e-5, 1.0)
                    kb.ts(Yp[:, hs], Yp[:, hs], st[:, 0:1], st[:, 2:3], ALU.subtract, ALU.mult, [Yp, st], [Yp])
                kb.tt(Yp[:], Yp[:], bc[:, 1, :], ALU.mult, [Yp, bc], [Yp])
                kb.tt(Yp[:], Yp[:], bc[:, 2, :], ALU.add, [Yp, bc], [Yp])
                p = pt.get()
                kb.mm(p[:, 0:2], Pb[:, js], C["headsel_bf"][:, 0:2], [Pb, C["headsel_bf"]], [p])
                kb.cp(st[:, 4:6], p[:, 0:2], [p], [st], eng="act")
                for hh in range(2):
                    hs = slice(hh * 64, hh * 64 + 64)
                    kb.stt(Yp[:, hs], Vtm[:, hs], st[:, 4 + hh:5 + hh], Yp[:, hs], ALU.mult, ALU.add, [Vtm, st, Yp], [Yp])
                p = pt.get()
                kb.mm(p[:], L["sgd"][:, 0, cs], gup[:, 0, :], [L["sgd"], gup], [p], start=True, stop=False, inc=False)
                kb.mm(p[:], L["sgd"][:, 1, cs], gup[:, 1, :], [L["sgd"], gup], [p], start=False, stop=True)
                kb.tt(Yo[:], Yp[:], p[:], ALU.mult, [Yp, p], [Yo])
                p = ptb.get()
                kb.tr(p[:], Yo[:], C["ident_bf"][:], [Yo, C["ident_bf"]], [p])
                kb.cp(yf[:, cs], p[:], [p], [yf], eng="act")
        kb.dma("sp", d["oy_d"][c.GW + ch0:c.GW + ch0 + 128, :], yf[:], [yf], [kb.K_oy])
        S.sb.release(); S.ps.release()
    S.sb.release(); S.ps.release()


def phase_mix(kb, tok0):
    c, S, d, C = kb.cfg, kb.S, kb.d, kb.C
    KC, D, T, TT, TM = c.KC, c.D, c.T, c.TT, c.TM
    KH = KC // 2
    S.sb.mark(); S.ps.mark()
    mT = S.sb.alloc([128, KC, TT], BF16, "mT")
    g1bc = S.sb.alloc([128, D], F32, "g1bc")
    kb.dma("sp", g1bc[:], d["mod_d"][:, 2 * D:3 * D], [kb.K_mod], [g1bc])
    wsrc = d["w_in"].rearrange("(kc p) n -> p kc n", p=128)
    gosrc = d["w_gdn_o"].rearrange("(kc p) n -> p kc n", p=128)
    rosrc = d["w_rwkv_o"].rearrange("(kc p) n -> p kc n", p=128)
    wosrc = d["w_out"].rearrange("(kc p) n -> p kc n", p=128)
    hsrc = d["hT_d"].rearrange("(kc p) t -> p kc t", p=128)
    osrc = d["oy_d"].rearrange("(kc p) t -> p kc t", p=128)
    K_wc = Key("wcache")
    NW = min(512, D)
    for j in range(KC):
        kb.S.dma("pool", [
            (lambda e, j=j: e.dma_start(out=d["wga_c"][j].rearrange("p (kc n) -> p kc n", kc=KC), in_=wsrc[:, :, c.OFF_G + j * 128:c.OFF_G + (j + 1) * 128])),
            (lambda e, j=j: e.dma_start(out=d["wgb_c"][j].rearrange("p (kc n) -> p kc n", kc=KC), in_=wsrc[:, :, c.OFF_G + D + j * 128:c.OFF_G + D + (j + 1) * 128])),
            (lambda e, j=j: e.dma_start(out=d["wgo_c"][j].rearrange("p (kc n) -> p kc n", kc=KH), in_=gosrc[:, :, j * 128:(j + 1) * 128])),
            (lambda e, j=j: e.dma_start(out=d["wro_c"][j].rearrange("p (kc n) -> p kc n", kc=KH), in_=rosrc[:, :, j * 128:(j + 1) * 128])),
        ], reads=[], writes=[K_wc], semkey="wcache")
    for n in range(D // NW):
        kb.S.dma("pool", (lambda e, n=n: e.dma_start(out=d["wo_c"][n].rearrange("p (kc n) -> p kc n", kc=KC), in_=wosrc[:, :, n * NW:(n + 1) * NW])),
                 reads=[], writes=[K_wc], semkey="wcache")
    for tt in range(TM // TT):
        t0 = tok0 + tt * TT
        S.sb.mark(); S.ps.mark()
        hb = S.sb.alloc([128, KC, TT], BF16, "hTm"); ob = S.sb.alloc([128, KC, TT], BF16, "oTm")
        wga = [S.sb.alloc([128, KC, 128], BF16, "wga%d" % i) for i in range(2)]
        wgb = [S.sb.alloc([128, KC, 128], BF16, "wgb%d" % i) for i in range(2)]
        wgo = [S.sb.alloc([128, KH, 128], BF16, "wgo%d" % i) for i in range(2)]
        wro = [S.sb.alloc([128, KH, 128], BF16, "wro%d" % i) for i in range(2)]
        sa = S.sb.alloc([128, TT], F32, "sa"); sbb = S.sb.alloc([128, TT], F32, "sbb"); ta = S.sb.alloc([128, TT], F32, "ta")
        pp = [S.ps.alloc([128, 512], F32, "pm%d" % i) for i in range(8)]
        kb.dma("sp", hb[:], hsrc[:, :, t0:t0 + TT], [kb.K_hT], [hb])
        kb.dma("sp", ob[:], osrc[:, :, t0:t0 + TT], [kb.K_oy], [ob])
        for j in range(KC):
            i = j % 2
            kb.dma("sp", wga[i][:], d["wga_c"][j].rearrange("p (kc n) -> p kc n", kc=KC), [K_wc], [wga[i]])
            kb.dma("sp", wgb[i][:], d["wgb_c"][j].rearrange("p (kc n) -> p kc n", kc=KC), [K_wc], [wgb[i]])
            kb.dma("sp", wgo[i][:], d["wgo_c"][j].rearrange("p (kc n) -> p kc n", kc=KH), [K_wc], [wgo[i]])
            kb.dma("sp", wro[i][:], d["wro_c"][j].rearrange("p (kc n) -> p kc n", kc=KH), [K_wc], [wro[i]])
            pa, pb_, pc, pd = [pp[(4 * j + q) % 8] for q in range(4)]
            for kc in range(KC):
                kb.mm(pa[:, 0:TT], wga[i][:, kc, :], hb[:, kc, :], [wga[i], hb], [pa], start=(kc == 0), stop=(kc == KC - 1), inc=(kc == KC - 1))
            for kc in range(KC):
                kb.mm(pb_[:, 0:TT], wgb[i][:, kc, :], hb[:, kc, :], [wgb[i], hb], [pb_], start=(kc == 0), stop=(kc == KC - 1), inc=(kc == KC - 1))
            for kc in range(KH):
                kb.mm(pc[:, 0:TT], wgo[i][:, kc, :], ob[:, kc, :], [wgo[i], ob], [pc], start=(kc == 0), stop=(kc == KH - 1), inc=(kc == KH - 1))
            for kc in range(KH):
                kb.mm(pd[:, 0:TT], wro[i][:, kc, :], ob[:, KH + kc, :], [wro[i], ob], [pd], start=(kc == 0), stop=(kc == KH - 1), inc=(kc == KH - 1))
            kb.act(sa[:], pa[:, 0:TT], AF.Sigmoid, [pa], [sa])
            kb.act(sbb[:], pb_[:, 0:TT], AF.Sigmoid, [pb_], [sbb])
            kb.tt(ta[:], sa[:], pc[:, 0:TT], ALU.mult, [sa, pc], [ta])
            kb.tt(sbb[:], sbb[:], pd[:, 0:TT], ALU.mult, [sbb, pd], [sbb])
            kb.tt(mT[:, j, :], ta[:], sbb[:], ALU.add, [ta, sbb], [mT], eng="pool")
        S.sb.release(); S.ps.release()
        S.sb.mark(); S.ps.mark()
        NW = min(512, D)
        wo = [S.sb.alloc([128, KC, NW], BF16, "wo%d" % i) for i in range(2)]
        xt = [S.sb.alloc([128, D], F32, "xtm%d" % i) for i in range(TT // 128)]
        po = [S.ps.alloc([128, 512], F32, "po%d" % i) for i in range(4)]
        for i in range(TT // 128):
            kb.dma("sp", xt[i][:], d["x"][t0 + i * 128:t0 + (i + 1) * 128, :], [], [xt[i]])
        pi = 0
        for n in range(D // NW):
            w = wo[n % 2]
            kb.dma("sp", w[:], d["wo_c"][n].rearrange("p (kc n) -> p kc n", kc=KC), [K_wc], [w])
            for i in range(TT // 128):
                p = po[pi % 4]; pi += 1
                for kc in range(KC):
                    kb.mm(p[:, 0:NW], mT[:, kc, i * 128:(i + 1) * 128], w[:, kc, :], [mT, w], [p], start=(kc == 0), stop=(kc == KC - 1), inc=(kc == KC - 1))
                cs = slice(n * NW, (n + 1) * NW)
                kb.tt(ta_ := None, None, None, None, [], []) if False else None
                kb.S.op("dve", (lambda e, i=i, p=p, cs=cs: e.tensor_tensor(out=p[:, 0:NW], in0=p[:, 0:NW], in1=g1bc[:, cs], op=ALU.mult)), reads=[g1bc], writes=[p])
                kb.tt(xt[i][:, cs], xt[i][:, cs], p[:, 0:NW], ALU.add, [xt[i], p], [xt[i]])
        for i in range(TT // 128):
            r0 = tt * TT + i * 128
            kb.dma("sp", d["xmid_d"][r0:r0 + 128, :], xt[i][:], [xt[i]], [kb.K_xmid])
        S.sb.release(); S.ps.release()
    S.sb.release(); S.ps.release()


def phase_route(kb):
    c, S, d, C = kb.cfg, kb.S, kb.d, kb.C
    KC, D, TM, NBLK = c.KC, c.D, c.TM, c.NBLK
    NTm = TM // 128
    R = {}
    R["wts"] = S.sb.alloc([128, NTm, 2], F32, "wts")
    R["desti"] = S.sb.alloc([128, NTm, 2], I32, "desti")
    R["idxW"] = S.sb.alloc([128, NBLK], I32, "idxW")
    S.sb.mark(); S.ps.mark()
    M1 = S.sb.alloc([128, NTm, 64], F32, "M1"); M2 = S.sb.alloc([128, NTm, 64], F32, "M2")
    Msb = S.sb.alloc([128, NTm, 64], BF16, "Msb")
    destf = S.sb.alloc([128, NTm, 2], F32, "destf")
    A2 = S.sb.alloc([128, D], F32, "A2"); B2 = S.sb.alloc([128, D], F32, "B2")
    wr = S.sb.alloc([128, KC, 72], F32, "wr"); brb = S.sb.alloc([128, 72], F32, "brb")
    xm = [S.sb.alloc([128, D], F32, "xmr%d" % i) for i in range(2)]
    h2b = [S.sb.alloc([128, D], BF16, "h2b%d" % i) for i in range(2)]
    junk = S.sb.alloc([128, D], BF16, "junkq")
    hTc = [S.sb.alloc([128, 128], F32, "hTc%d" % i) for i in range(4)]
    lg = S.sb.alloc([128, 72], F32, "lg"); sm = S.sb.alloc([128, 64], F32, "sm"); st = S.sb.alloc([128, 16], F32, "stq")
    ptr = Rot([S.ps.alloc([128, 128], F32, "ptq%d" % i) for i in range(5)])
    pl = S.ps.alloc([128, 128], F32, "plq")
    kb.dma("sp", A2[:], d["mod_d"][:, 4 * D:5 * D], [kb.K_mod], [A2])
    kb.dma("sp", B2[:], d["n2g"][0:1, :].partition_broadcast(128), [], [B2])
    kb.stt(A2[:], A2[:], 1.0, B2[:], ALU.add, ALU.mult, [A2, B2], [A2])
    kb.dma("sp", B2[:], d["mod_d"][:, 3 * D:4 * D], [kb.K_mod], [B2])
    kb.dma("sp", wr[:], d["wr"].rearrange("(kc p) n -> p kc n", p=128), [], [wr])
    kb.dma("sp", brb[:], d["br"][0:1, :].partition_broadcast(128), [], [brb])
    for n in range(NTm):
        x_ = xm[n % 2]; hb = h2b[n % 2]
        kb.dma("sp", x_[:], d["xmid_d"][n * 128:(n + 1) * 128, :], [kb.K_xmid], [x_])
        kb.act(junk[:], x_[:], AF.Square, [x_], [junk, st], accum=st[:, 0:1])
        kb.rsqrt_col(st[:, 0:1], st, 1e-6, 1.0 / D)
        kb.stt(x_[:], x_[:], st[:, 0:1], A2[:], ALU.mult, ALU.mult, [x_, st, A2], [x_])
        kb.tt(x_[:], x_[:], B2[:], ALU.add, [x_, B2], [x_])
        kb.cp(hb[:], x_[:], [x_], [hb], eng="pool")
        kb.dma("sp", d["h2_d"][n * 128:(n + 1) * 128, :], hb[:], [hb], [kb.K_h2])
        for kc in range(KC):
            p = ptr.get(); hc = hTc[kc % 4]
            kb.tr(p[:], x_[:, kc * 128:(kc + 1) * 128], C["ident"][:], [x_, C["ident"]], [p])
            kb.cp(hc[:], p[:], [p], [hc], eng=("act" if kc % 2 == 0 else "dve"))
            kb.mm(pl[:, 0:72], hc[:], wr[:, kc, :], [hc, wr], [pl], start=(kc == 0), stop=(kc == KC - 1), inc=(kc == KC - 1))
        kb.tt(lg[:], pl[:, 0:72], brb[:], ALU.add, [pl, brb], [lg])
        kb.red(st[:, 1:2], lg[:, 0:8], ALU.max, [lg], [st])
        ohg = sm[:, 0:8]
        kb.ts(ohg, lg[:, 0:8], st[:, 1:2], None, ALU.is_equal, None, [lg, st], [sm])
        kb.ts(st[:, 2:3], st[:, 1:2], -1.0, None, ALU.mult, None, [st], [st])
        kb.act(sm[:, 8:16], lg[:, 0:8], AF.Exp, [lg, st], [sm, st], bias=st[:, 2:3], accum=st[:, 3:4])
        kb.recip(st[:, 3:4], st[:, 3:4], [st], [st])
        esel = sm[:, 16:24]
        kb.ts(esel, lg[:, 8:16], sm[:, 0:1], None, ALU.mult, None, [lg, sm], [sm])
        for g in range(1, 8):
            kb.stt(esel, lg[:, 8 + 8 * g:16 + 8 * g], sm[:, g:g + 1], esel, ALU.mult, ALU.add, [lg, sm], [sm])
        kb.red(st[:, 4:5], esel, ALU.max, [sm], [st])
        mk1 = sm[:, 24:32]; es2 = sm[:, 32:40]; mk2 = sm[:, 40:48]
        kb.ts(mk1, esel, st[:, 4:5], None, ALU.is_equal, None, [sm, st], [sm])
        kb.stt(es2, mk1, -1e30, esel, ALU.mult, ALU.add, [sm], [sm])
        kb.red(st[:, 5:6], es2, ALU.max, [sm], [st])
        kb.ts(mk2, es2, st[:, 5:6], None, ALU.is_equal, None, [sm, st], [sm])
        kb.tt(st[:, 6:7], st[:, 4:5], st[:, 5:6], ALU.subtract, [st], [st])
        kb.act(st[:, 6:7], st[:, 6:7], AF.Sigmoid, [st], [st])
        kb.ts(st[:, 7:8], st[:, 6:7], -1.0, 1.0, ALU.mult, ALU.add, [st], [st])
        kb.ts(R["wts"][:, n, :], st[:, 6:8], st[:, 3:4], None, ALU.mult, None, [st], [R["wts"]])
        for g in range(8):
            kb.ts(M1[:, n, 8 * g:8 * g + 8], mk1, sm[:, g:g + 1], None, ALU.mult, None, [sm], [M1], eng="pool")
            kb.ts(M2[:, n, 8 * g:8 * g + 8], mk2, sm[:, g:g + 1], None, ALU.mult, None, [sm], [M2], eng="pool")
        kb.tt(Msb[:, n, :], M1[:, n, :], M2[:, n, :], ALU.add, [M1, M2], [Msb], eng="pool")
    cnt = S.sb.alloc([128, 8], F32, "cnt")
    pc = ptr.get()
    for n in range(NTm):
        kb.mm(pc[0:64, 0:1], Msb[:, n, :], C["ones_bf"][:, 0:1], [Msb, C["ones_bf"]], [pc], start=(n == 0), stop=(n == NTm - 1), inc=(n == NTm - 1))
    kb.cp(cnt[0:64, 0:1], pc[0:64, 0:1], [pc], [cnt], eng="act")
    thr = S.sb.alloc([128, 128], F32, "thr")
    assert 2 * TM // 128 <= 128
    kb.ts(thr[0:64, :], C["iota_f"][0:64, :], 128.0, None, ALU.mult, None, [C["iota_f"]], [thr])
    kb.ts(thr[0:64, :], thr[0:64, :], cnt[0:64, 0:1], None, ALU.is_lt, None, [thr, cnt], [thr])
    kb.red(cnt[0:64, 2:3], thr[0:64, :], ALU.add, [thr], [cnt])
    nbrep = S.sb.alloc([128, 128], F32, "nbrep")
    kb.ts(nbrep[0:64, :], C["ones"][0:64, :], cnt[0:64, 2:3], None, ALU.mult, None, [C["ones"], cnt], [nbrep])
    p = ptr.get()
    kb.mm(p[:, 0:64], nbrep[0:64, :], C["triu_s"][0:64, 0:64], [nbrep, C["triu_s"]], [p])
    startrow = S.sb.alloc([128, 64], F32, "startrow")
    kb.ts(startrow[:], p[:, 0:64], 128.0, None, ALU.mult, None, [p], [startrow])
    p = ptr.get()
    kb.mm(p[0:64, 0:1], C["triu_i"][0:64, 0:64], cnt[0:64, 2:3], [C["triu_i"], cnt], [p])
    kb.cp(cnt[0:64, 3:4], p[0:64, 0:1], [p], [cnt], eng="act")
    Bm = S.sb.alloc([128, 128], F32, "Bm")
    kb.ts(Bm[0:64, 0:NBLK], C["iota_f"][0:64, 0:NBLK], cnt[0:64, 3:4], None, ALU.is_ge, None, [C["iota_f"], cnt], [Bm])
    p = ptr.get()
    kb.mm(p[:, 0:NBLK], C["ones"][0:64, :], Bm[0:64, 0:NBLK], [C["ones"], Bm], [p])
    idxf = S.sb.alloc([128, 128], F32, "idxf")
    kb.stt(idxf[:, 0:NBLK], p[:, 0:NBLK], 128.0, C["iota_p"][:, 0:NBLK], ALU.mult, ALU.add, [p, C["iota_p"]], [idxf])
    kb.cp(R["idxW"][:], idxf[:, 0:NBLK], [idxf], [R["idxW"]], eng="dve")
    carry = S.sb.alloc([128, 64], F32, "carry"); df = S.sb.alloc([128, 64], F32, "df"); tmp = S.sb.alloc([128, 64], F32, "tmpq")
    kb.cp(carry[:], startrow[:], [startrow], [carry], eng="pool")
    for n in range(NTm):
        p = ptr.get()
        kb.mm(p[:, 0:64], C["triu_s_bf"][:], Msb[:, n, :], [C["triu_s_bf"], Msb], [p])
        kb.tt(df[:], p[:, 0:64], carry[:], ALU.add, [p, carry], [df])
        kb.tt(tmp[:], df[:], M1[:, n, :], ALU.mult, [df, M1], [tmp])
        kb.red(destf[:, n, 0:1], tmp[:], ALU.add, [tmp], [destf])
        kb.tt(tmp[:], df[:], M2[:, n, :], ALU.mult, [df, M2], [tmp])
        kb.red(destf[:, n, 1:2], tmp[:], ALU.add, [tmp], [destf])
        p = ptr.get()
        kb.mm(p[:, 0:64], C["ones_bf"][:], Msb[:, n, :], [C["ones_bf"], Msb], [p])
        kb.tt(carry[:], carry[:], p[:, 0:64], ALU.add, [carry, p], [carry])
    kb.cp(R["desti"][:].rearrange("p n j -> p (n j)"), destf[:].rearrange("p n j -> p (n j)"), [destf], [R["desti"]], eng="dve")
    fill = S.sb.alloc([128, NBLK], I32, "fill"); tokv = S.sb.alloc([128, NTm], I32, "tokv")
    zrow = S.sb.alloc([128, D], BF16, "zrow")
    kb.memset(fill[:], TM, [fill]); kb.memset(zrow[0:1, :], 0.0, [zrow])
    kb.dma("sp", tokv[:], d["tokiota"][:, :], [], [tokv])
    kb.dma("sp", d["h2_d"][TM:TM + 1, :], zrow[0:1, :], [zrow], [kb.K_h2])
    kb.dma("sp", d["tokid_d"].rearrange("(p b) o -> p (b o)", p=128), fill[:], [fill], [kb.K_tokid])
    for n in range(NTm):
        for j in range(2):
            kb.S.dma("pool", (lambda e, n=n, j=j: e.indirect_dma_start(
                out=d["tokid_d"][:, :], out_offset=bass.IndirectOffsetOnAxis(ap=R["desti"][:, n, j:j + 1], axis=0),
                in_=tokv[:, n:n + 1], in_offset=None)),
                reads=[R["desti"], tokv, kb.K_tokid], writes=[kb.K_tokid2], semkey="tokscat")
    S.sb.release(); S.ps.release()
    return R


def phase_moe(kb, R):
    c, S, d, C = kb.cfg, kb.S, kb.d, kb.C
    KC, D, TM, NBLK, DE, FC = c.KC, c.D, c.TM, c.NBLK, c.DE, c.FC
    S.sb.mark(); S.ps.mark()
    w1t = S.sb.alloc([128, KC * DE], BF16, "w1t"); w3t = S.sb.alloc([128, KC * DE], BF16, "w3t"); w2t = S.sb.alloc([128, FC * D], BF16, "w2t")
    xb = [S.sb.alloc([128, D], BF16, "xb%d" % i) for i in range(2)]
    idb = [S.sb.alloc([128, 1], I32, "idb%d" % i) for i in range(2)]
    xbT = S.sb.alloc([128, KC, 128], BF16, "xbT"); aT = S.sb.alloc([128, FC, 128], BF16, "aT")
    s1 = S.sb.alloc([128, DE], F32, "s1"); ab = S.sb.alloc([128, DE], BF16, "ab_")
    yb = [S.sb.alloc([128, D], F32, "yb%d" % i) for i in range(2)]
    pst = Rot([S.ps.alloc([128, 4, 128], BF16, "pe%d" % i) for i in range(2)])
    ph = [S.ps.alloc([128, 512], F32, "ph%d" % i) for i in range(2)]
    py = Rot([S.ps.alloc([128, 512], F32, "py%d" % i) for i in range(4)])
    NW = min(512, D)
    for b in range(NBLK):
        i = b % 2
        kb.dma("sp", idb[i][:], d["tokid_d"][b * 128:(b + 1) * 128, :], [kb.K_tokid2], [idb[i]])
        kb.S.dma("pool", (lambda e, i=i: e.indirect_dma_start(out=xb[i][:], out_offset=None, in_=d["h2_d"][:, :],
                 in_offset=bass.IndirectOffsetOnAxis(ap=idb[i][:, 0:1], axis=0))),
                 reads=[idb[i], kb.K_h2], writes=[xb[i]])
        for (wt, nm) in [(w1t, "w1"), (w3t, "w3"), (w2t, "w2")]:
            kb.S.dma("pool", (lambda e, wt=wt, nm=nm, b=b: e.indirect_dma_start(out=wt[:], out_offset=None, in_=d[nm][:, :],
                     in_offset=bass.IndirectOffsetOnAxis(ap=R["idxW"][:, b:b + 1], axis=0), bounds_check=kb.breg(e, 64 * 128 - 1), oob_is_err=False)),
                     reads=[R["idxW"]], writes=[wt])
        G = min(4, KC)
        for g in range(KC // G):
            p = pst.get()
            for j in range(G):
                kc = g * G + j
                kb.tr(p[:, j, :], xb[i][:, kc:D:KC], C["ident_bf"][:], [xb[i], C["ident_bf"]], [p], inc=(j == G - 1))
            kb.cp(xbT[:, g * G:(g + 1) * G, :], p[:, 0:G, :], [p], [xbT], eng=("act" if g % 2 == 0 else "dve"))
        for kc in range(KC):
            kb.mm(ph[0][:, 0:DE], xbT[:, kc, :], w1t[:, kc * DE:(kc + 1) * DE], [xbT, w1t], [ph[0]], start=(kc == 0), stop=(kc == KC - 1), inc=(kc == KC - 1))
        for kc in range(KC):
            kb.mm(ph[1][:, 0:DE], xbT[:, kc, :], w3t[:, kc * DE:(kc + 1) * DE], [xbT, w3t], [ph[1]], start=(kc == 0), stop=(kc == KC - 1), inc=(kc == KC - 1))
        kb.act(s1[:], ph[0][:, 0:DE], AF.Silu, [ph[0]], [s1])
        kb.tt(ab[:], s1[:], ph[1][:, 0:DE], ALU.mult, [s1, ph[1]], [ab])
        p = pst.get()
        for fc in range(FC):
            kb.tr(p[:, fc, :], ab[:, fc:DE:FC], C["ident_bf"][:], [ab, C["ident_bf"]], [p], inc=(fc == FC - 1))
        kb.cp(aT[:], p[:, 0:FC, :], [p], [aT], eng="act")
        for n in range(D // NW):
            pq = py.get()
            for fc in range(FC):
                kb.mm(pq[:, 0:NW], aT[:, fc, :], w2t[:, fc * D + n * NW:fc * D + (n + 1) * NW], [aT, w2t], [pq], start=(fc == 0), stop=(fc == FC - 1), inc=(fc == FC - 1))
            kb.cp(yb[i][:, n * NW:(n + 1) * NW], pq[:, 0:NW], [pq], [yb[i]], eng=("act" if n % 2 == 0 else "dve"))
        kb.dma("sp", d["yb_d"][b * 128:(b + 1) * 128, :], yb[i][:], [yb[i]], [kb.K_yb])
    S.sb.release(); S.ps.release()


def phase_final(kb, R):
    c, S, d, C = kb.cfg, kb.S, kb.d, kb.C
    D, TM, NBLK = c.D, c.TM, c.NBLK
    S.sb.mark()
    g2 = S.sb.alloc([128, D], F32, "g2bc"); nf = S.sb.alloc([128, D], F32, "nfbc")
    r1 = [S.sb.alloc([128, D], F32, "r1_%d" % i) for i in range(2)]; r2 = [S.sb.alloc([128, D], F32, "r2_%d" % i) for i in range(2)]
    xm = [S.sb.alloc([128, D], F32, "xmf%d" % i) for i in range(2)]
    junk = S.sb.alloc([128, D], BF16, "junkf"); st = S.sb.alloc([128, 2], F32, "stf")
    kb.dma("sp", g2[:], d["mod_d"][:, 5 * D:6 * D], [kb.K_mod], [g2])
    kb.dma("sp", nf[:], d["nfg"][0:1, :].partition_broadcast(128), [], [nf])
    for n in range(TM // 128):
        i = n % 2
        for j, rb in enumerate([r1[i], r2[i]]):
            kb.S.dma("pool", (lambda e, rb=rb, n=n, j=j: e.indirect_dma_start(out=rb[:], out_offset=None, in_=d["yb_d"][:, :],
                     in_offset=bass.IndirectOffsetOnAxis(ap=R["desti"][:, n, j:j + 1], axis=0))),
                     reads=[R["desti"], kb.K_yb], writes=[rb])
        kb.dma("sp", xm[i][:], d["xmid_d"][n * 128:(n + 1) * 128, :], [kb.K_xmid], [xm[i]])
        kb.ts(r1[i][:], r1[i][:], R["wts"][:, n, 0:1], None, ALU.mult, None, [r1[i], R["wts"]], [r1[i]])
        kb.stt(r1[i][:], r2[i][:], R["wts"][:, n, 1:2], r1[i][:], ALU.mult, ALU.add, [r2[i], R["wts"], r1[i]], [r1[i]])
        kb.tt(r1[i][:], r1[i][:], g2[:], ALU.mult, [r1[i], g2], [r1[i]], eng="pool")
        kb.tt(xm[i][:], xm[i][:], r1[i][:], ALU.add, [xm[i], r1[i]], [xm[i]])
        kb.act(junk[:], xm[i][:], AF.Square, [xm[i]], [junk, st], accum=st[:, 0:1])
        kb.rsqrt_col(st[:, 0:1], st, 1e-6, 1.0 / D)
        kb.stt(xm[i][:], xm[i][:], st[:, 0:1], nf[:], ALU.mult, ALU.mult, [xm[i], st, nf], [xm[i]])
        kb.dma("sp", d["out"][n * 128:(n + 1) * 128, :], xm[i][:], [xm[i]], [kb.K_out])
    S.sb.release()


NCORES = 4


def build_program(cfg):
    kb = KB(cfg)
    declare_inputs(kb)
    for k in ["mod_d", "hT_d", "oy_d", "xmid_d", "h2", "tokid", "tokid2", "yb", "out"]:
        setattr(kb, "K_" + k.replace("_d", ""), Key(k))
    S = kb.S
    load_consts(kb)
    phase_mod(kb)
    phase_norm1(kb)
    S.sb.mark(); P = phase_gdn_pre(kb, list(range(cfg.HG))); phase_gdn(kb, list(range(cfg.HG)), P); S.sb.release()
    S.sb.mark(); L = phase_rwkv_pre(kb); phase_rwkv(kb, list(range(cfg.NP)), L); S.sb.release()
    phase_mix(kb, 0)
    S.sb.mark(); R = phase_route(kb); phase_moe(kb, R); phase_final(kb, R); S.sb.release()
    S.wait_all("sp")
    assert S.simulate()
    S.emit()
    return kb


def kernel(**inputs):
    x = np.asarray(inputs["x"])
    B, T, D = x.shape
    DE = np.asarray(inputs["w1"]).shape[-1]
    cfg = Cfg(D, T, DE)
    kb = build_program(cfg)
    ncores = min(NCORES, B) if B < NCORES else NCORES
    in_maps = [host_inputs(cfg, inputs, b) for b in range(B)]
    res = run_bass_kernel_spmd(kb.nc, in_maps, core_ids=list(range(B)))
    out = np.stack([np.asarray(res.results[b]["out"]) for b in range(B)], axis=0)
    return out.astype(np.float32)
```
